# Optimizing a Trainium2 kernel written in Bass

```python
import jax, jax.numpy as jnp
from jax import lax
import numpy as np

D_MODEL = 2048
BATCH = 2
SEQ = 4096
DEPTH = 1

CONV_CH = D_MODEL // 2
CONV_K = 31
HEAD_DIM = 128
HEADS_PER_GROUP = 4
DILATED_GROUPS = ((128, 1), (512, 4), (2048, 16))
N_GROUPS = len(DILATED_GROUPS)
N_ATTN_HEADS = HEADS_PER_GROUP * N_GROUPS
ATTN_WIDTH = N_ATTN_HEADS * HEAD_DIM
ATTN_OUT_WIDTH = HEADS_PER_GROUP * HEAD_DIM
BAND_BLOCK = 128
OFF_Q = 2 * CONV_CH
OFF_K = OFF_Q + ATTN_WIDTH
OFF_V = OFF_K + ATTN_WIDTH
OFF_G = OFF_V + ATTN_WIDTH
IN_COLS = OFF_G + 2 * D_MODEL
PEER_HEADS = 8
PEER_NKEYS = 128
PEER_EXPERTS = PEER_NKEYS * PEER_NKEYS
PEER_TOPK = 16
PEER_QDIM = 256
PEER_HALF = PEER_QDIM // 2
PEER_CHUNK = 128
EPS = 1e-6

kernel_name = "hybrid_conv_dilatedattn_peer_adaln"


def rmsnorm(x, g):
    xf = x.astype(jnp.float32)
    y = xf * lax.rsqrt(jnp.mean(xf * xf, axis=-1, keepdims=True) + EPS)
    return (y * g.astype(jnp.float32)).astype(x.dtype)


def layernorm(x, g, b):
    xf = x.astype(jnp.float32)
    mu = jnp.mean(xf, axis=-1, keepdims=True)
    var = jnp.mean(jnp.square(xf - mu), axis=-1, keepdims=True)
    y = (xf - mu) * lax.rsqrt(var + EPS) * g.astype(jnp.float32) + b.astype(jnp.float32)
    return y.astype(x.dtype)


def dilated_group_attention(q, k, v, window, dil):
    B, S, Hg, E = q.shape
    w_sub = window // dil
    blk = BAND_BLOCK
    assert w_sub <= blk
    L = S // dil
    nb = -(-L // blk)
    Lp = nb * blk

    def split(t):
        t = t.reshape(B, L, dil, Hg, E)
        return jnp.pad(t, ((0, 0), (0, Lp - L), (0, 0), (0, 0), (0, 0)))

    def band(t):
        t = jnp.pad(split(t), ((0, 0), (blk, 0), (0, 0), (0, 0), (0, 0)))
        t = t.reshape(B, nb + 1, blk, dil, Hg, E)
        return jnp.concatenate([t[:, :-1], t[:, 1:]], axis=2)

    qs = split(q).reshape(B, nb, blk, dil, Hg, E)
    kb, vb = band(k), band(v)
    s = jnp.einsum('bnqrhe,bnkrhe->bnrhqk', qs, kb) * (E ** -0.5)
    qi = jnp.arange(blk)[:, None]
    ki = jnp.arange(2 * blk)[None, :]
    dist = qi + blk - ki
    key_pos = jnp.arange(nb)[:, None, None] * blk + ki[None] - blk
    valid = (dist >= 0)[None] & (dist <= w_sub)[None] & (key_pos >= 0)
    s = jnp.where(valid[None, :, None, None], s, -jnp.inf)
    m = jnp.max(s, axis=-1, keepdims=True)
    p = jnp.exp(s - m)
    den = jnp.sum(p, axis=-1, keepdims=True)
    o = jnp.einsum('bnrhqk,bnkrhe->bnqrhe', p / den, vb)
    lse = (m + jnp.log(den))[..., 0]
    o = o.reshape(B, Lp, dil, Hg, E)[:, :L].reshape(B, S, Hg, E)
    lse = lse.transpose(0, 1, 4, 2, 3).reshape(B, Lp, dil, Hg)[:, :L].reshape(B, S, Hg)
    return o, lse


def token_mixer(h, w_in, conv_dw, conv_db, conv_ln_g, conv_ln_b, w_conv_out, b_conv_out,
                q_norm_g, k_norm_g, w_attn_o, w_out):
    B, S, D = h.shape
    z = h @ w_in
    glu = z[..., :OFF_Q]
    u = glu[..., :CONV_CH] * jax.nn.sigmoid(glu[..., CONV_CH:])
    u = lax.conv_general_dilated(u, conv_dw[:, None, :].astype(u.dtype), window_strides=(1,),
                                 padding=[(CONV_K - 1, 0)], dimension_numbers=('NWC', 'WIO', 'NWC'),
                                 feature_group_count=CONV_CH) + conv_db
    u = jax.nn.silu(layernorm(u, conv_ln_g, conv_ln_b))
    y_conv = u @ w_conv_out + b_conv_out
    def heads(t, g):
        t = t.reshape(B, S, N_ATTN_HEADS, HEAD_DIM).astype(jnp.float32)
        if g is None:
            return t
        return t * lax.rsqrt(jnp.mean(t * t, axis=-1, keepdims=True) + EPS) * g.astype(jnp.float32)
    q = heads(z[..., OFF_Q:OFF_K], q_norm_g)
    k = heads(z[..., OFF_K:OFF_V], k_norm_g)
    v = heads(z[..., OFF_V:OFF_G], None)
    outs, lses = [], []
    for gi, (window, dil) in enumerate(DILATED_GROUPS):
        hs = slice(gi * HEADS_PER_GROUP, (gi + 1) * HEADS_PER_GROUP)
        o, l = dilated_group_attention(q[:, :, hs], k[:, :, hs], v[:, :, hs], window, dil)
        outs.append(o)
        lses.append(l)
    wts = jax.nn.softmax(jnp.stack(lses, axis=0), axis=0)
    o = jnp.sum(wts[..., None] * jnp.stack(outs, axis=0), axis=0)
    y_attn = o.reshape(B, S, ATTN_OUT_WIDTH).astype(h.dtype) @ w_attn_o
    gates = jax.nn.sigmoid(z[..., OFF_G:])
    merged = gates[..., :D] * y_conv + gates[..., D:] * y_attn
    return merged @ w_out


def peer(h, w_q, sub_keys, w_up, w_down):
    B, S, D = h.shape
    qp = (h @ w_q).reshape(B, S, PEER_HEADS, 2, PEER_HALF).astype(jnp.float32)
    s = jnp.einsum('bshpe,hpne->bshpn', qp, sub_keys.astype(jnp.float32))
    v1, i1 = lax.top_k(s[..., 0, :], PEER_TOPK)
    v2, i2 = lax.top_k(s[..., 1, :], PEER_TOPK)
    cand = (v1[..., :, None] + v2[..., None, :]).reshape(B, S, PEER_HEADS, PEER_TOPK * PEER_TOPK)
    top, ci = lax.top_k(cand, PEER_TOPK)
    e1 = jnp.take_along_axis(i1, ci // PEER_TOPK, axis=-1)
    e2 = jnp.take_along_axis(i2, ci % PEER_TOPK, axis=-1)
    idx = e1 * PEER_NKEYS + e2
    g = jax.nn.softmax(top, axis=-1).astype(h.dtype)
    nc = (B * S) // PEER_CHUNK
    hk = PEER_HEADS * PEER_TOPK
    xs = (h.reshape(nc, PEER_CHUNK, D), idx.reshape(nc, PEER_CHUNK, hk), g.reshape(nc, PEER_CHUNK, hk))

    def chunk(args):
        xc, ic, gc = args
        u = jnp.take(w_up, ic, axis=0)
        a = jax.nn.gelu(jnp.einsum('td,tkd->tk', xc, u), approximate=False)
        return jnp.einsum('tk,tkd->td', gc * a, jnp.take(w_down, ic, axis=0))

    return lax.map(chunk, xs).reshape(B, S, D)


def setup_inputs(seed: int = 0) -> dict:
    key = jax.random.key(seed)
    ks = jax.random.split(key, 24)
    D, L = D_MODEL, DEPTH
    nrm = lambda k, shape, s: jax.random.normal(k, shape, jnp.float32) * s
    return {
        "x": nrm(ks[0], (BATCH, SEQ, D), 1.0),
        "c": nrm(ks[1], (BATCH, D), 1.0),
        "norm1_g": 1.0 + nrm(ks[2], (L, D), 0.02),
        "norm2_g": 1.0 + nrm(ks[3], (L, D), 0.02),
        "w_ada": nrm(ks[4], (L, D, 6 * D), 0.5 * D ** -0.5),
        "b_ada": nrm(ks[5], (L, 6 * D), 0.01),
        "w_in": nrm(ks[6], (L, D, IN_COLS), D ** -0.5),
        "conv_dw": nrm(ks[7], (L, CONV_K, CONV_CH), CONV_K ** -0.5),
        "conv_db": nrm(ks[8], (L, CONV_CH), 0.01),
        "conv_ln_g": 1.0 + nrm(ks[9], (L, CONV_CH), 0.02),
        "conv_ln_b": nrm(ks[10], (L, CONV_CH), 0.01),
        "w_conv_out": nrm(ks[11], (L, CONV_CH, D), CONV_CH ** -0.5),
        "b_conv_out": nrm(ks[12], (L, D), 0.01),
        "q_norm_g": 1.0 + nrm(ks[13], (L, N_ATTN_HEADS, HEAD_DIM), 0.02),
        "k_norm_g": 1.0 + nrm(ks[14], (L, N_ATTN_HEADS, HEAD_DIM), 0.02),
        "w_attn_o": nrm(ks[15], (L, ATTN_OUT_WIDTH, D), ATTN_OUT_WIDTH ** -0.5),
        "w_out": nrm(ks[16], (L, D, D), D ** -0.5),
        "peer_w_q": nrm(ks[17], (L, D, PEER_HEADS * PEER_QDIM), D ** -0.5),
        "peer_sub_keys": nrm(ks[18], (L, PEER_HEADS, 2, PEER_NKEYS, PEER_HALF), PEER_HALF ** -0.5),
        "peer_w_up": nrm(ks[19], (L, PEER_EXPERTS, D), D ** -0.5),
        "peer_w_down": nrm(ks[20], (L, PEER_EXPERTS, D), PEER_HEADS ** -0.5),
    }


def reference(x, c, norm1_g, norm2_g, w_ada, b_ada, w_in, conv_dw, conv_db, conv_ln_g, conv_ln_b,
              w_conv_out, b_conv_out, q_norm_g, k_norm_g, w_attn_o, w_out,
              peer_w_q, peer_sub_keys, peer_w_up, peer_w_down):
    B, S, D = x.shape
    for l in range(DEPTH):
        mod = (jax.nn.silu(c) @ w_ada[l] + b_ada[l]).reshape(B, 6, D)
        shift1, scale1, gate1 = mod[:, 0, None], mod[:, 1, None], mod[:, 2, None]
        shift2, scale2, gate2 = mod[:, 3, None], mod[:, 4, None], mod[:, 5, None]
        h = rmsnorm(x, norm1_g[l]) * (1.0 + scale1) + shift1
        x = x + gate1 * token_mixer(h, w_in[l], conv_dw[l], conv_db[l], conv_ln_g[l], conv_ln_b[l],
                                    w_conv_out[l], b_conv_out[l], q_norm_g[l], k_norm_g[l],
                                    w_attn_o[l], w_out[l])
        h = rmsnorm(x, norm2_g[l]) * (1.0 + scale2) + shift2
        x = x + gate2 * peer(h, peer_w_q[l], peer_sub_keys[l], peer_w_up[l], peer_w_down[l])
    return x
```

```python
import contextlib
import numpy as np
import concourse.bass as bass
import concourse.mybir as mybir
from concourse.bass_utils import run_bass_kernel_spmd

F32 = mybir.dt.float32
BF16 = mybir.dt.bfloat16
U32 = mybir.dt.uint32
AF = mybir.ActivationFunctionType
ALU = mybir.AluOpType
AX = mybir.AxisListType

D = 2048
KC = 16
NT = 1024
LT = 3072
EPS = 1e-6
NEG = -30000.0
IN_COLS = 10752
CH_Q, CH_K, CH_V, CH_G = 16, 28, 40, 52

FM = {}
_o = 0
for _n, _w in [("cT", 16), ("bada", 96), ("g1", 16), ("g2", 16), ("dw", 248), ("db", 8), ("lng", 8), ("lnb", 8),
               ("bco", 16), ("qg", 12), ("kg", 12), ("hval", 1), ("kb1", 9), ("kb2", 12), ("kb3", 16)]:
    FM[_n] = (_o, _o + _w)
    _o += _w
NFM = _o
CS = {"ident": (0, 128), "mcur": (128, 256), "mprev": (256, 384), "iota": (384, 512), "iota16": (512, 528)}
NCS = 528

NO_SELF_SYNC = ("pe",)
STAGE = "full"


class Sched:
    def __init__(self, nc, es):
        self.nc = nc
        self.es = es
        self.eng = {'pe': nc.tensor, 'act': nc.scalar, 'dve': nc.vector, 'pool': nc.gpsimd, 'sp': nc.sync}
        self.sem = {k: es.enter_context(nc.semaphore('sem_' + k)) for k in self.eng}
        self.cnt = {k: 0 for k in self.eng}
        self.seen = {k: {} for k in self.eng}
        self.last_w = {}
        self.readers = {}
        self.dsem = {}
        self.dcnt = {}
        self.bank_i = 0

    def _semof(self, key):
        return self.sem[key] if key in self.sem else self.dsem[key]

    def _deps(self, reads, writes):
        deps = {}

        def add(k, c):
            if deps.get(k, 0) < c:
                deps[k] = c
        for r in reads:
            ev = self.last_w.get(r)
            if ev is not None:
                add(*ev)
        for w in writes:
            ev = self.last_w.get(w)
            if ev is not None:
                add(*ev)
            for k, c in self.readers.get(w, {}).items():
                add(k, c)
        return deps

    def _wait(self, e, deps):
        for k, c in deps.items():
            if k == e and e in NO_SELF_SYNC:
                continue
            if self.seen[e].get(k, 0) >= c:
                continue
            self.eng[e].wait_ge(self._semof(k), c)
            self.seen[e][k] = c

    def _record(self, ev, reads, writes):
        k, c = ev
        for r in reads:
            self.readers.setdefault(r, {})[k] = c
        for w in writes:
            self.last_w[w] = ev
            self.readers[w] = {}

    def op(self, e, fn, reads=(), writes=()):
        self._wait(e, self._deps(reads, writes))
        ins = fn(self.eng[e])
        self.cnt[e] += 1
        ins.then_inc(self.sem[e], 1)
        self._record((e, self.cnt[e]), reads, writes)
        return ins

    def dma(self, q, semname, reads=(), writes=(), out=None, in_=None, fn=None, **kw):
        if semname not in self.dsem:
            self.dsem[semname] = self.es.enter_context(self.nc.semaphore('d_' + semname))
            self.dcnt[semname] = 0
        self._wait(q, self._deps(reads, writes))
        if fn is not None:
            ins = fn(self.eng[q])
        else:
            ins = self.eng[q].dma_start(out=out, in_=in_, **kw)
        self.dcnt[semname] += 16
        ins.then_inc(self.dsem[semname], 16)
        self._record((semname, self.dcnt[semname]), reads, writes)
        return ins

    def barrier(self):
        evs = {k: c for k, c in self.cnt.items() if c > 0}
        evs.update({k: c for k, c in self.dcnt.items() if c > 0})
        for e in self.eng:
            self._wait(e, dict(evs))

    def finish(self, q='sp'):
        evs = {k: c for k, c in self.dcnt.items() if c > 0}
        evs.update({k: c for k, c in self.cnt.items() if c > 0 and k != q})
        self._wait(q, evs)


class Arena:
    def __init__(self, base_ap, lo, hi):
        self.base = base_ap
        self.lo0, self.hi0 = lo, hi
        self.lo, self.hi = lo, hi

    def reset(self):
        self.lo, self.hi = self.lo0, self.hi0

    def alloc(self, shape, dtype, top=False):
        n = int(np.prod(shape))
        isz = 4 if dtype in (F32, U32) else 2
        words = (n * isz + 3) // 4
        if top:
            self.hi -= words
            off = self.hi
        else:
            off = self.lo
            self.lo += words
        assert self.lo <= self.hi, ("arena overflow", self.lo, self.hi)
        v = self.base[:, off:off + words]
        if dtype != F32:
            v = v.bitcast(dtype)
        v = v[:, 0:n]
        if len(shape) == 2:
            v = v.rearrange("p (a b) -> p a b", a=shape[0], b=shape[1])
        elif len(shape) == 3:
            v = v.rearrange("p (a b c) -> p a b c", a=shape[0], b=shape[1], c=shape[2])
        return v


def bc(ap2, reps_axis, n):
    pat = [list(x) for x in ap2.ap]
    pat.insert(reps_axis, [0, n])
    return bass.AP(tensor=ap2.tensor, offset=ap2.offset, ap=pat)


def build_program(stage=None):
    stage = stage or STAGE
    nc = bass.Bass("TRN2", target_bir_lowering=False)
    early = stage in ("A", "H", "M1")
    def dram(n, s, d=F32, kind="ExternalInput"):
        if early and n in ("w_co", "w_ao", "w_out", "w_q", "w_upP", "w_dnP") or (stage == "A" and n in ("w_in", "xh")):
            return None
        if (stage == "x1" and n in ("w_q", "w_upP", "w_dnP")) or (stage == "R" and n in ("w_upP", "w_dnP")):
            return None
        return nc.dram_tensor(n, s, d, kind=kind).ap()
    dbg = nc.dram_tensor("dbg", [128, 8192], F32, kind="ExternalOutput").ap() if stage != "full" else None
    xh = dram("xh", [LT, D])
    fm_d = dram("fm", [128, NFM])
    cst_d = dram("cst", [128, NCS])
    skT_d = dram("skT", [128, 2048])
    w_ada = dram("w_ada", [D, 6 * D])
    w_in = dram("w_in", [D, IN_COLS])
    w_co = dram("w_co", [1024, D])
    w_ao = dram("w_ao", [512, D])
    w_out = dram("w_out", [D, D])
    w_q = dram("w_q", [D, D])
    w_upP = dram("w_upP", [128, 128, KC * 128])
    w_dnP = dram("w_dnP", [128, 128, D])
    y = dram("y", [NT, D], F32, "ExternalOutput")
    gscr = nc.dram_tensor("gscr", [128, 128, NT], BF16, kind="Internal").ap()

    with contextlib.ExitStack() as es:
        S = Sched(nc, es)
        TOT = 53100
        big = es.enter_context(nc.sbuf_tensor("arena", [128, TOT], F32))
        pbs = [es.enter_context(nc.psum_tensor(f"pb{i}", [128, 512], F32)) for i in range(8)]
        CONST_W = 7900
        X1_W = 16384
        AC = Arena(big, 0, CONST_W)
        AX1 = Arena(big, CONST_W, CONST_W + X1_W)
        AM = Arena(big, CONST_W + X1_W, TOT)

        dbgc = {'c': 0}

        def dump(ap, n, res):
            c0 = dbgc['c']
            dbgc['c'] += n
            S.dma('pool', 'dbg', out=dbg[:, c0:c0 + n], in_=ap, reads=[res])
            return c0

        bank_set = {'s': list(range(8))}

        def bank():
            bs = bank_set['s']
            i = bs[S.bank_i % len(bs)]
            S.bank_i += 1
            return pbs[i], f"pb{i}"

        fm = AC.alloc([NFM], F32)
        cst = AC.alloc([NCS], F32)
        skT = AC.alloc([16, 128], F32)
        idb = AC.alloc([128], BF16)
        mcur_b = AC.alloc([128], BF16)
        mprev_b = AC.alloc([128], BF16)
        ones_f = AC.alloc([128], F32)
        ones_b = AC.alloc([128], BF16)
        sc = AC.alloc([16], F32)
        scb = AC.alloc([16], BF16)
        modT = AC.alloc([96], F32)
        modT2 = modT
        A1T = AC.alloc([16], F32)
        A2T = AC.alloc([16], F32)
        gate1B = AC.alloc([D], F32)
        gate2B = AC.alloc([D], F32)
        nsc = [(AC.alloc([1], F32), AC.alloc([1], F32), AC.alloc([1], F32)) for _ in range(2)]
        nstate = {'i': 0, 'q': 0}
        epsc = AC.alloc([2], F32)
        eps1 = epsc[:, 0:1]
        eps128 = epsc[:, 1:2]
        diagf = AC.alloc([128], F32)

        def fmc(name, a=None, b=None):
            o0, o1 = FM[name]
            if a is None:
                return fm[:, o0:o1]
            return fm[:, o0 + a:o0 + (b if b is not None else a + 1)]
        ident_f = cst[:, 0:128]

        S.dma('sp', 'c0', out=fm, in_=fm_d[:, :], writes=['fm'])
        S.dma('sp', 'c1', out=cst, in_=cst_d[:, :], writes=['cst'])
        S.dma('sp', 'c2', out=skT.rearrange("p a b -> p (a b)"), in_=skT_d[:, :], writes=['skT'])
        S.op('dve', lambda e: e.tensor_copy(out=idb, in_=ident_f), reads=['cst'], writes=['idb'])
        S.op('dve', lambda e: e.tensor_copy(out=mcur_b, in_=cst[:, 128:256]), reads=['cst'], writes=['mcur'])
        S.op('dve', lambda e: e.tensor_copy(out=mprev_b, in_=cst[:, 256:384]), reads=['cst'], writes=['mprev'])
        S.op('dve', lambda e: e.memset(ones_f, 1.0), writes=['ones_f'])
        S.op('dve', lambda e: e.memset(ones_b, 1.0), writes=['ones_b'])
        S.op('dve', lambda e: e.memset(eps1, EPS), writes=['epsc'])
        S.op('dve', lambda e: e.memset(eps128, 128.0 * EPS), writes=['epsc'])

        S.op('act', lambda e: e.activation(out=sc, in_=fmc("cT"), func=AF.Silu), reads=['fm'], writes=['sc'])
        S.op('dve', lambda e: e.tensor_copy(out=scb, in_=sc), reads=['sc'], writes=['scb'])
        wa = [AX1.alloc([16, 512], BF16) for _ in range(3)]
        w_ada_v = w_ada.rearrange("(kc p) n -> p kc n", p=128)
        for nb in range(8):
            s_ = nb % 3
            S.dma('pool', f'wa{s_}', out=wa[s_], in_=w_ada_v[:, :, nb * 512:(nb + 1) * 512], writes=[f'wa{s_}'])
            pb, pn = bank()
            for sub in range(4):
                for kc in range(KC):
                    S.op('pe', lambda e: e.matmul(out=pb[:, sub:sub + 1], lhsT=wa[s_][:, kc, sub * 128:(sub + 1) * 128],
                                                  rhs=scb[:, kc:kc + 1], start=(kc == 0), stop=(kc == KC - 1)),
                         reads=[f'wa{s_}', 'scb'], writes=[pn])
            S.op('dve', lambda e: e.tensor_tensor(out=modT[:, nb * 4:nb * 4 + 4], in0=pb[:, 0:4],
                                                  in1=fmc("bada", nb * 4, nb * 4 + 4), op=ALU.add),
                 reads=[pn, 'fm'], writes=['modT'])
        ada_state = {'col': 32, 'i': 0}

        def ada_deferred(wad, nblk):
            for _ in range(nblk):
                col = ada_state['col']
                if col >= 96:
                    return
                ada_state['col'] += 1
                s_ = ada_state['i'] % 2
                ada_state['i'] += 1
                S.dma('pool', f'wad{s_}', out=wad[s_], in_=w_ada_v[:, :, col * 128:(col + 1) * 128], writes=[f'wad{s_}'])
                pb, pn = bank()
                for kc in range(KC):
                    S.op('pe', lambda e: e.matmul(out=pb[:, 0:1], lhsT=wad[s_][:, kc, :], rhs=scb[:, kc:kc + 1],
                                                  start=(kc == 0), stop=(kc == KC - 1)), reads=[f'wad{s_}', 'scb'], writes=[pn])
                S.op('dve', lambda e: e.tensor_tensor(out=modT2[:, col:col + 1], in0=pb[:, 0:1], in1=fmc("bada", col), op=ALU.add),
                     reads=[pn, 'fm'], writes=['modT2'])
        S.op('dve', lambda e: e.scalar_tensor_tensor(out=A1T, in0=modT[:, 16:32], scalar=1.0, in1=fmc("g1"),
                                                     op0=ALU.add, op1=ALU.mult), reads=['modT', 'fm'], writes=['A1T'])
        sh1T = modT[:, 0:16]
        sh2T = modT[:, 48:64]

        def bcast_cols(colsT, dst, dname):
            for g4 in range(4):
                pb, pn = bank()
                for k4 in range(4):
                    kc = g4 * 4 + k4
                    S.op('dve', lambda e: e.tensor_scalar(out=diagf, in0=ident_f, scalar1=colsT[:, kc:kc + 1], scalar2=None,
                                                          op0=ALU.mult), reads=['cst', 'modT2'], writes=['diagf'])
                    S.op('pe', lambda e: e.matmul(out=pb[:, k4 * 128:(k4 + 1) * 128], lhsT=ones_f, rhs=diagf,
                                                  start=True, stop=True), reads=['ones_f', 'diagf'], writes=[pn])
                S.op('act', lambda e: e.activation(out=dst[:, g4 * 512:(g4 + 1) * 512], in_=pb[:, :], func=AF.Copy),
                     reads=[pn], writes=[dname])
        S.barrier()
        AX1.reset()
        if stage == "A":
            dump(modT, 96, 'modT'); dump(A1T, 16, 'A1T')
            S.finish()
            return nc

        def norm_T(src, src_res, dst3, dst_res, AT, ares, shT, junks, xss, shres='modT'):
            ni = nstate['i'] % 2
            nstate['i'] += 1
            ss, rs, rstd = nsc[ni]
            junk = junks[ni % len(junks)]
            xs = xss[ni % len(xss)]
            jn, xn_, sn, rn, rdn = f'junk{ni}', f'xs{ni}', f'ss{ni}', f'rs{ni}', f'rstd{ni}'
            S.op('act', lambda e: e.activation(out=junk, in_=src, func=AF.Square, accum_out=ss),
                 reads=[src_res], writes=[jn, sn])
            S.op('act', lambda e: e.activation(out=rs, in_=ss, func=AF.Sqrt, scale=1.0 / D, bias=EPS),
                 reads=[sn], writes=[rn])
            S.op('dve', lambda e: e.reciprocal(out=rstd, in_=rs), reads=[rn], writes=[rdn])
            S.op('act', lambda e: e.activation(out=xs, in_=src, func=AF.Identity, scale=rstd),
                 reads=[src_res, rdn], writes=[xn_])
            for half in range(2):
                pb, pn = bank()
                pv = pb[:, :].bitcast(BF16)
                for k in range(8):
                    kc = half * 8 + k
                    S.op('pe', lambda e: e.transpose(out=pv[:, k * 128:(k + 1) * 128], in_=xs[:, kc * 128:(kc + 1) * 128],
                                                     identity=idb), reads=[xn_, 'idb'], writes=[pn])
                for k in range(8):
                    kc = half * 8 + k
                    if k % 2 == 0:
                        S.op('dve', lambda e: e.tensor_scalar(out=dst3[:, kc, :], in0=pv[:, k * 128:(k + 1) * 128],
                                                              scalar1=AT[:, kc:kc + 1], scalar2=shT[:, kc:kc + 1],
                                                              op0=ALU.mult, op1=ALU.add),
                             reads=[pn, ares, shres], writes=[dst_res])
                    else:
                        S.op('act', lambda e: e.activation(out=dst3[:, kc, :], in_=pv[:, k * 128:(k + 1) * 128],
                                                           func=AF.Identity, scale=AT[:, kc:kc + 1], bias=shT[:, kc:kc + 1]),
                             reads=[pn, ares, shres], writes=[dst_res])

        w_in_v = w_in.rearrange("(kc p) n -> p kc n", p=128)
        wstate = {'i': 0}

        def load_w(ws, cc):
            s_ = wstate['i'] % len(ws)
            wstate['i'] += 1
            S.dma('pool', f'ws{s_}', out=ws[s_], in_=w_in_v[:, :, cc * 128:(cc + 1) * 128], writes=[f'ws{s_}'])
            return ws[s_], f'ws{s_}'

        def projT(wslot, wres, hT, hres, t0, n, pb, pn, c0=0):
            for kc in range(KC):
                S.op('pe', lambda e: e.matmul(out=pb[:, c0:c0 + n], lhsT=wslot[:, kc, :], rhs=hT[:, kc, t0:t0 + n],
                                              start=(kc == 0), stop=(kc == KC - 1)), reads=[wres, hres], writes=[pn])

        pend = {'f': None}

        def flush_norm():
            if pend['f'] is not None:
                f_ = pend['f']
                pend['f'] = None
                f_()

        def qk_norm(pb, pn, n, gcol, dst, dres, tmp, is_q):
            sqs, rks = tmp
            i = nstate['q'] % 2
            nstate['q'] += 1
            sq, rk = sqs[i], rks[i]
            flush_norm()
            S.op('act', lambda e: e.activation(out=sq[:, 0:n], in_=pb[:, 0:n], func=AF.Square), reads=[pn], writes=[f'sq{i}'])

            def rest():
                pb2, pn2 = bank()
                S.op('pe', lambda e: e.matmul(out=pb2[:, 0:n], lhsT=ones_f, rhs=sq[:, 0:n], start=True, stop=True),
                     reads=['ones_f', f'sq{i}'], writes=[pn2])
                if is_q:
                    S.op('act', lambda e: e.activation(out=rk[:, 0:n], in_=pb2[:, 0:n], func=AF.Sqrt, scale=1.0, bias=128.0 * EPS),
                         reads=[pn2], writes=[f'rk{i}'])
                else:
                    S.op('act', lambda e: e.activation(out=rk[:, 0:n], in_=pb2[:, 0:n], func=AF.Sqrt, scale=1.0 / 128, bias=EPS),
                         reads=[pn2], writes=[f'rk{i}'])
                S.op('dve', lambda e: e.reciprocal(out=rk[:, 0:n], in_=rk[:, 0:n]), reads=[f'rk{i}'], writes=[f'rk{i}'])
                S.op('dve', lambda e: e.scalar_tensor_tensor(out=dst, in0=pb[:, 0:n], scalar=gcol, in1=rk[:, 0:n],
                                                             op0=ALU.mult, op1=ALU.mult), reads=[pn, f'rk{i}', 'fm'], writes=[dres])
            pend['f'] = rest

        hTm = AM.alloc([KC, 1536], BF16)
        K3T = AM.alloc([4, LT], BF16)
        V3T = AM.alloc([4, LT], BF16)
        ws = [AM.alloc([KC, 128], BF16) for _ in range(3)]
        am_mark = (AM.lo, AM.hi)

        xin = [AX1.alloc([D], F32) for _ in range(3)]
        junk = [AX1.alloc([D], BF16)]
        xsb = [AX1.alloc([D], BF16) for _ in range(2)]
        hTh = AX1.alloc([KC, 512], BF16)
        sq = [AX1.alloc([512], F32) for _ in range(2)]
        rk = [AX1.alloc([512], F32) for _ in range(2)]
        xi = 0
        for tile in range(24):
            s_ = xi % 3
            xi += 1
            S.dma('sp', f'xin{s_}', out=xin[s_], in_=xh[tile * 128:(tile + 1) * 128, :], writes=[f'xin{s_}'])
            if tile < 12:
                blk, tt = tile // 4, tile % 4
                norm_T(xin[s_], f'xin{s_}', hTh[:, :, tt * 128:(tt + 1) * 128], 'hTh', A1T, 'A1T', sh1T, junk, xsb)
                if tt == 3:
                    for hd in range(4):
                        for kind in range(2):
                            cc = (CH_K if kind == 0 else CH_V) + 8 + hd
                            wsl, wr = load_w(ws, cc)
                            pb, pn = bank()
                            projT(wsl, wr, hTh, 'hTh', 0, 512, pb, pn)
                            if kind == 0:
                                qk_norm(pb, pn, 512, fmc("kg", 8 + hd), K3T[:, hd, blk * 512:(blk + 1) * 512], 'K3T', (sq, rk), False)
                            else:
                                S.op('act', lambda e: e.activation(out=V3T[:, hd, blk * 512:(blk + 1) * 512], in_=pb[:, :], func=AF.Copy),
                                     reads=[pn], writes=['V3T'])
                    flush_norm()
            else:
                flush_norm()
                m0 = (tile - 12) * 128
                norm_T(xin[s_], f'xin{s_}', hTm[:, :, m0:m0 + 128], 'hTm', A1T, 'A1T', sh1T, junk, xsb)
        S.barrier()
        if stage == "H":
            dump(hTm[:, 0, 0:512], 512, 'hTm'); dump(hTm[:, 5, 512:1024], 512, 'hTm'); dump(K3T[:, 1, 0:512], 512, 'K3T')
            dump(V3T[:, 2, 512:1024], 512, 'V3T'); dump(hTh[:, 3, :], 512, 'hTh')
            S.finish()
            return nc
        AX1.reset()

        attnT = AX1.alloc([4, NT], BF16)
        x1_mark = AX1.lo
        Qs = [AX1.alloc([NT], BF16) for _ in range(3)]
        K2T = AX1.alloc([1536], BF16)
        V2T = AX1.alloc([1536], BF16)
        V2 = AX1.alloc([12, 128], BF16)
        K1T = AX1.alloc([1152], BF16)
        V1T = AX1.alloc([1152], BF16)
        V1 = AX1.alloc([9, 128], BF16)
        V3 = AX1.alloc([32, 128], BF16)
        sq = [AX1.alloc([512], F32) for _ in range(2)]
        rk = [AX1.alloc([512], F32) for _ in range(2)]
        PTs = [AX1.alloc([256], BF16) for _ in range(4)]
        oacc = AX1.alloc([NT], F32)
        dacc = AX1.alloc([NT], F32)
        pti = {'i': 0}
        wad = [AX1.alloc([16, 128], BF16) for _ in range(2)]

        def kv_proj(cc, hT, hres, spans, dstK, dres, gcol, is_k):
            wsl, wr = load_w(ws, cc)
            for (t0, n, d0) in spans:
                pb, pn = bank()
                projT(wsl, wr, hT, hres, t0, n, pb, pn)
                if is_k:
                    qk_norm(pb, pn, n, gcol, dstK[:, d0:d0 + n], dres, (sq, rk), False)
                else:
                    S.op('act', lambda e: e.activation(out=dstK[:, d0:d0 + n], in_=pb[:, 0:n], func=AF.Copy),
                         reads=[pn], writes=[dres])

        def transpose_tiles(srcs, dst, dres, sres):
            for i0 in range(0, len(srcs), 8):
                pb, pn = bank()
                pv = pb[:, :].bitcast(BF16)
                grp = srcs[i0:i0 + 8]
                for k, (sap, n) in enumerate(grp):
                    S.op('pe', lambda e: e.transpose(out=pv[0:n, k * 128:(k + 1) * 128], in_=sap, identity=idb),
                         reads=[sres, 'idb'], writes=[pn])
                nmin = min(n for _, n in grp)
                S.op('dve', lambda e: e.tensor_copy(out=dst[0:nmin, i0:i0 + len(grp), :],
                                                    in_=pv[0:nmin, 0:len(grp) * 128].rearrange("p (k e) -> p k e", k=len(grp))),
                     reads=[pn], writes=[dres])

        def attend(pairs, qap, qn, ob, on, db, dn, c0, first_unused=None):
            pts = []
            for (kap, nk, vap, mask, kb) in pairs:
                pb, pn = bank()
                S.op('pe', lambda e: e.matmul(out=pb[0:nk, 0:qn], lhsT=kap, rhs=qap, start=True, stop=False),
                     reads=['KQ', 'K3T'], writes=[pn])
                S.op('pe', lambda e: e.matmul(out=pb[0:nk, 0:qn], lhsT=idb[0:nk, 0:nk], rhs=mask, start=False, stop=True),
                     reads=['idb', 'mcur', 'mprev'], writes=[pn])
                i = pti['i'] % 4
                pti['i'] += 1
                pt = PTs[i]
                if kb is not None:
                    S.op('act', lambda e: e.activation(out=pt[0:nk, 0:qn], in_=pb[0:nk, 0:qn], func=AF.Exp, bias=kb),
                         reads=[pn, 'fm'], writes=[f'pt{i}'])
                else:
                    S.op('act', lambda e: e.activation(out=pt[0:nk, 0:qn], in_=pb[0:nk, 0:qn], func=AF.Exp),
                         reads=[pn], writes=[f'pt{i}'])
                pts.append((pt, i, nk, vap))
            for j, (pt, i, nk, vap) in enumerate(pts):
                S.op('pe', lambda e: e.matmul(out=ob[:, c0:c0 + qn], lhsT=vap, rhs=pt[0:nk, 0:qn],
                                              start=(j == 0), stop=(j == len(pts) - 1)), reads=[f'pt{i}', 'Vt'], writes=[on])
            for j, (pt, i, nk, vap) in enumerate(pts):
                S.op('pe', lambda e: e.matmul(out=db[:, c0:c0 + qn], lhsT=ones_b[0:nk, :], rhs=pt[0:nk, 0:qn],
                                              start=(j == 0), stop=(j == len(pts) - 1)), reads=[f'pt{i}', 'ones_b'], writes=[dn])

        for j in range(4):
            h1, h2, h3 = j, 4 + j, 8 + j
            ada_deferred(wad, 16)
            for gi, hd in enumerate((h1, h2, h3)):
                wsl, wr = load_w(ws, CH_Q + hd)
                for half in range(2):
                    pb, pn = bank()
                    projT(wsl, wr, hTm, 'hTm', 512 + half * 512, 512, pb, pn)
                    qk_norm(pb, pn, 512, fmc("qg", hd), Qs[gi][:, half * 512:(half + 1) * 512], 'KQ', (sq, rk), True)
            sp1 = [(384, 512, 0), (896, 512, 512), (1408, 128, 1024)]
            sp2 = [(0, 512, 0), (512, 512, 512), (1024, 512, 1024)]
            sp3 = [(0, 512, 1536), (512, 512, 2048), (1024, 512, 2560)]
            kv_proj(CH_K + h1, hTm, 'hTm', sp1, K1T, 'KQ', fmc("kg", h1), True)
            kv_proj(CH_V + h1, hTm, 'hTm', sp1, V1T, 'VT', None, False)
            kv_proj(CH_K + h2, hTm, 'hTm', sp2, K2T, 'KQ', fmc("kg", h2), True)
            kv_proj(CH_V + h2, hTm, 'hTm', sp2, V2T, 'VT', None, False)
            kv_proj(CH_K + h3, hTm, 'hTm', sp3, K3T[:, j, :], 'K3T', fmc("kg", h3), True)
            kv_proj(CH_V + h3, hTm, 'hTm', sp3, V3T[:, j, :], 'V3T', None, False)
            flush_norm()
            transpose_tiles([(V1T[:, i * 128:(i + 1) * 128], 128) for i in range(9)], V1, 'Vt', 'VT')
            transpose_tiles([(V2T[:, 512 * kt + r:512 * kt + 512:4], 128) for r in range(4) for kt in range(3)], V2, 'Vt', 'VT')
            transpose_tiles([(V3T[:, j, r:2048:16], 128) for r in range(16)], V3[:, 0:16, :], 'Vt', 'V3T')
            transpose_tiles([(V3T[:, j, 2048 + r:LT:16], 64) for r in range(16)], V3[:, 16:32, :], 'Vt', 'V3T')
            bank_set['s'] = list(range(6))
            for hb in range(2):
                ob, on = pbs[6], 'pb6'
                db, dn = pbs[7], 'pb7'
                for q4 in range(4):
                    qb = hb * 4 + q4
                    pairs = [(K1T[:, 128 * (qb + 1):128 * (qb + 2)], 128, V1[:, qb + 1, :], mcur_b, fmc("kb1", qb + 1)),
                             (K1T[:, 128 * qb:128 * (qb + 1)], 128, V1[:, qb, :], mprev_b, fmc("kb1", qb))]
                    attend(pairs, Qs[0][:, 128 * qb:128 * (qb + 1)], 128, ob, on, db, dn, q4 * 128)
                S.op('act', lambda e: e.activation(out=oacc[:, hb * 512:(hb + 1) * 512], in_=ob[:, :], func=AF.Copy),
                     reads=[on], writes=['oacc'])
                S.op('dve', lambda e: e.tensor_copy(out=dacc[:, hb * 512:(hb + 1) * 512], in_=db[:, :]),
                     reads=[dn], writes=['dacc'])
            for qb in range(2):
                ob, on = pbs[6], 'pb6'
                db, dn = pbs[7], 'pb7'
                for r in range(4):
                    pairs = [(K2T[:, 512 * (qb + 1) + r:512 * (qb + 2):4], 128, V2[:, r * 3 + qb + 1, :], mcur_b, fmc("kb2", r * 3 + qb + 1)),
                             (K2T[:, 512 * qb + r:512 * (qb + 1):4], 128, V2[:, r * 3 + qb, :], mprev_b, fmc("kb2", r * 3 + qb))]
                    attend(pairs, Qs[1][:, 512 * qb + r:512 * (qb + 1):4], 128, ob, on, db, dn, r * 128)
                ov = oacc[:, 512 * qb:512 * (qb + 1)].rearrange("p (m r) -> p r m", r=4)
                dv = dacc[:, 512 * qb:512 * (qb + 1)].rearrange("p (m r) -> p r m", r=4)
                S.op('dve', lambda e: e.tensor_tensor(out=ov, in0=ob[:, :].rearrange("p (r m) -> p r m", r=4), in1=ov, op=ALU.add),
                     reads=[on, 'oacc'], writes=['oacc'])
                S.op('dve', lambda e: e.tensor_tensor(out=dv, in0=db[:, :].rearrange("p (r m) -> p r m", r=4), in1=dv, op=ALU.add),
                     reads=[dn, 'dacc'], writes=['dacc'])
            for hb in range(2):
                ob, on = pbs[6], 'pb6'
                db, dn = pbs[7], 'pb7'
                for r8 in range(8):
                    r = hb * 8 + r8
                    pairs = [(K3T[:, j, r:2048:16], 128, V3[:, r, :], mprev_b[:, 0:64], fmc("kb3", r)),
                             (K3T[:, j, 2048 + r:LT:16], 64, V3[0:64, 16 + r, :], mcur_b[0:64, 0:64], None)]
                    attend(pairs, Qs[2][:, r:NT:16], 64, ob, on, db, dn, r8 * 64)
                ov = oacc[:, :].rearrange("p (m r) -> p r m", r=16)[:, hb * 8:(hb + 1) * 8, :]
                dv = dacc[:, :].rearrange("p (m r) -> p r m", r=16)[:, hb * 8:(hb + 1) * 8, :]
                S.op('dve', lambda e: e.tensor_tensor(out=ov, in0=ob[:, :].rearrange("p (r m) -> p r m", r=8), in1=ov, op=ALU.add),
                     reads=[on, 'oacc'], writes=['oacc'])
                S.op('dve', lambda e: e.tensor_tensor(out=dv, in0=db[:, :].rearrange("p (r m) -> p r m", r=8), in1=dv, op=ALU.add),
                     reads=[dn, 'dacc'], writes=['dacc'])
            bank_set['s'] = list(range(8))
            S.op('dve', lambda e: e.reciprocal(out=dacc, in_=dacc), reads=['dacc'], writes=['dacc'])
            S.op('dve', lambda e: e.tensor_tensor(out=attnT[:, j, :], in0=oacc, in1=dacc, op=ALU.mult),
                 reads=['oacc', 'dacc'], writes=['attnT'])
            if stage == "M1":
                dump(Qs[0][:, 0:512], 512, 'KQ'); dump(K1T[:, 0:512], 512, 'KQ'); dump(V1[:, 1, :], 128, 'Vt')
                dump(attnT[:, 0, :], 1024, 'attnT'); dump(dacc, 1024, 'dacc'); dump(V3[:, 5, :], 128, 'Vt'); dump(V3[:, 21, :], 128, 'Vt')
                dump(V2[:, 4, :], 128, 'Vt')
                S.finish()
                return nc
        assert ada_state['col'] == 96 or stage == "M1"
        S.op('dve', lambda e: e.scalar_tensor_tensor(out=A2T, in0=modT2[:, 64:80], scalar=1.0, in1=fmc("g2"),
                                                     op0=ALU.add, op1=ALU.mult), reads=['modT2', 'fm'], writes=['A2T'])
        bcast_cols(modT2[:, 32:48], gate1B, 'gate1B')
        bcast_cols(modT2[:, 80:96], gate2B, 'gate2B')
        S.barrier()
        AX1.lo = x1_mark
        BM = CONST_W + X1_W

        FA = Arena(big, BM + 12288, BM + 24576)
        FB = Arena(big, BM + 27648, TOT)
        U = AX1.alloc([8, 1152], BF16)
        Vc = AX1.alloc([8, NT], F32)
        uact = FA.alloc([8, NT], BF16)
        sig = FA.alloc([512], F32)
        sqv = FA.alloc([NT], F32)
        mean = FA.alloc([NT], F32)
        rstdv = FA.alloc([NT], F32)
        t1 = FA.alloc([NT], F32)
        diags = [FB.alloc([128], BF16) for _ in range(4)]
        spU = [(384, 512, 0), (896, 512, 512), (1408, 128, 1024)]
        for cc in range(8):
            wa_, wra = load_w(ws, cc)
            wb_, wrb = load_w(ws, 8 + cc)
            for (t0, n, d0) in spU:
                pa, pna = bank()
                pg, png = bank()
                projT(wa_, wra, hTm, 'hTm', t0, n, pa, pna)
                projT(wb_, wrb, hTm, 'hTm', t0, n, pg, png)
                S.op('act', lambda e: e.activation(out=sig[:, 0:n], in_=pg[:, 0:n], func=AF.Sigmoid), reads=[png], writes=['sig'])
                S.op('dve', lambda e: e.tensor_tensor(out=U[:, cc, d0:d0 + n], in0=pa[:, 0:n], in1=sig[:, 0:n], op=ALU.mult),
                     reads=[pna, 'sig'], writes=['U'])
            S.op('dve', lambda e: e.tensor_scalar(out=U[:, cc, 0:128], in0=U[:, cc, 0:128], scalar1=fmc("hval", 0), scalar2=None,
                                                  op0=ALU.mult), reads=['U', 'fm'], writes=['U'])
        di = 0
        for cc in range(8):
            pbs2 = [bank(), bank()]
            for k in range(31):
                dg = diags[di % 4]
                dn_ = f'diag{di % 4}'
                di += 1
                S.op('dve', lambda e: e.tensor_scalar(out=dg, in0=ident_f, scalar1=fmc("dw", cc * 31 + k), scalar2=None, op0=ALU.mult),
                     reads=['cst', 'fm'], writes=[dn_])
                for half in range(2):
                    pb, pn = pbs2[half]
                    o0 = 128 - 30 + k + half * 512
                    S.op('pe', lambda e: e.matmul(out=pb[:, :], lhsT=dg, rhs=U[:, cc, o0:o0 + 512], start=(k == 0), stop=(k == 30)),
                         reads=[dn_, 'U'], writes=[pn])
            for half in range(2):
                pb, pn = pbs2[half]
                S.op('act', lambda e: e.activation(out=Vc[:, cc, half * 512:(half + 1) * 512], in_=pb[:, :], func=AF.Identity,
                                                   bias=fmc("db", cc)), reads=[pn, 'fm'], writes=['Vc'])
        for half in range(2):
            pm, pmn = bank()
            pq, pqn = bank()
            hs = slice(half * 512, (half + 1) * 512)
            for cc in range(8):
                S.op('pe', lambda e: e.matmul(out=pm[:, :], lhsT=ones_f, rhs=Vc[:, cc, hs], start=(cc == 0), stop=(cc == 7)),
                     reads=['ones_f', 'Vc'], writes=[pmn])
            for cc in range(8):
                S.op('act', lambda e: e.activation(out=sqv[:, 0:512], in_=Vc[:, cc, hs], func=AF.Square), reads=['Vc'], writes=['sqv'])
                S.op('pe', lambda e: e.matmul(out=pq[:, :], lhsT=ones_f, rhs=sqv[:, 0:512], start=(cc == 0), stop=(cc == 7)),
                     reads=['ones_f', 'sqv'], writes=[pqn])
            S.op('dve', lambda e: e.tensor_scalar(out=mean[:, hs], in0=pm[:, :], scalar1=1.0 / 1024, scalar2=None, op0=ALU.mult),
                 reads=[pmn], writes=['mean'])
            S.op('dve', lambda e: e.tensor_tensor(out=t1[:, hs], in0=mean[:, hs], in1=mean[:, hs], op=ALU.mult),
                 reads=['mean'], writes=['t1'])
            S.op('dve', lambda e: e.scalar_tensor_tensor(out=rstdv[:, hs], in0=pq[:, :], scalar=1.0 / 1024, in1=t1[:, hs],
                                                         op0=ALU.mult, op1=ALU.subtract), reads=[pqn, 't1'], writes=['rstdv'])
            S.op('act', lambda e: e.activation(out=rstdv[:, hs], in_=rstdv[:, hs], func=AF.Sqrt, scale=1.0, bias=EPS),
                 reads=['rstdv'], writes=['rstdv'])
            S.op('dve', lambda e: e.reciprocal(out=rstdv[:, hs], in_=rstdv[:, hs]), reads=['rstdv'], writes=['rstdv'])
        for cc in range(8):
            S.op('dve', lambda e: e.tensor_tensor(out=t1, in0=Vc[:, cc, :], in1=mean, op=ALU.subtract), reads=['Vc', 'mean'], writes=['t1'])
            S.op('dve', lambda e: e.tensor_tensor(out=t1, in0=t1, in1=rstdv, op=ALU.mult), reads=['t1', 'rstdv'], writes=['t1'])
            S.op('act', lambda e: e.activation(out=uact[:, cc, :], in_=t1, func=AF.Silu, scale=fmc("lng", cc), bias=fmc("lnb", cc)),
                 reads=['t1', 'fm'], writes=['uact'])
        S.barrier()
        AX1.lo = x1_mark

        mergedT = Arena(big, BM + 16384, BM + 24576).alloc([KC, NT], BF16)
        wco = [AX1.alloc([8, 128], BF16) for _ in range(2)]
        wao = [AX1.alloc([4, 128], BF16) for _ in range(2)]
        sA = [AX1.alloc([512], F32) for _ in range(2)]
        sB = [AX1.alloc([512], F32) for _ in range(2)]
        tA = [AX1.alloc([512], F32) for _ in range(2)]
        tB = [AX1.alloc([512], F32) for _ in range(2)]
        w_co_v = w_co.rearrange("(cc p) n -> p cc n", p=128)
        w_ao_v = w_ao.rearrange("(j p) n -> p j n", p=128)
        it = 0
        for dc in range(KC):
            s_ = dc % 2
            S.dma('pool', f'wco{s_}', out=wco[s_], in_=w_co_v[:, :, dc * 128:(dc + 1) * 128], writes=[f'wco{s_}'])
            S.dma('pool', f'wao{s_}', out=wao[s_], in_=w_ao_v[:, :, dc * 128:(dc + 1) * 128], writes=[f'wao{s_}'])
            wga, wgar = load_w(ws, CH_G + dc)
            wgb, wgbr = load_w(ws, CH_G + 16 + dc)
            for half in range(2):
                hs = slice(half * 512, (half + 1) * 512)
                b_ = it % 2
                it += 1
                pA, pAn = bank()
                pB, pBn = bank()
                pC, pCn = bank()
                pY, pYn = bank()
                projT(wga, wgar, hTm, 'hTm', 512 + half * 512, 512, pA, pAn)
                projT(wgb, wgbr, hTm, 'hTm', 512 + half * 512, 512, pB, pBn)
                for cc in range(8):
                    S.op('pe', lambda e: e.matmul(out=pC[:, :], lhsT=wco[s_][:, cc, :], rhs=uact[:, cc, hs], start=(cc == 0), stop=(cc == 7)),
                         reads=[f'wco{s_}', 'uact'], writes=[pCn])
                for jj in range(4):
                    S.op('pe', lambda e: e.matmul(out=pY[:, :], lhsT=wao[s_][:, jj, :], rhs=attnT[:, jj, hs], start=(jj == 0), stop=(jj == 3)),
                         reads=[f'wao{s_}', 'attnT'], writes=[pYn])
                S.op('act', lambda e: e.activation(out=sA[b_], in_=pA[:, :], func=AF.Sigmoid), reads=[pAn], writes=[f'sA{b_}'])
                S.op('act', lambda e: e.activation(out=sB[b_], in_=pB[:, :], func=AF.Sigmoid), reads=[pBn], writes=[f'sB{b_}'])
                S.op('dve', lambda e: e.scalar_tensor_tensor(out=tA[b_], in0=pC[:, :], scalar=fmc("bco", dc), in1=sA[b_],
                                                             op0=ALU.add, op1=ALU.mult), reads=[pCn, f'sA{b_}', 'fm'], writes=[f'tA{b_}'])
                S.op('dve', lambda e: e.tensor_tensor(out=tB[b_], in0=pY[:, :], in1=sB[b_], op=ALU.mult),
                     reads=[pYn, f'sB{b_}'], writes=[f'tB{b_}'])
                S.op('dve', lambda e: e.tensor_tensor(out=mergedT[:, dc, hs], in0=tA[b_], in1=tB[b_], op=ALU.add),
                     reads=[f'tA{b_}', f'tB{b_}'], writes=['mergedT'])
        S.barrier()
        AX1.reset()

        x1 = AX1.alloc([8, D], F32)
        AM4 = Arena(big, BM, BM + 16384)
        wf = AM4.alloc([KC, 512], F32)
        wo = AM4.alloc([KC, 512], BF16)
        xr = [AM4.alloc([512], F32) for _ in range(2)]
        w_out_v = w_out.rearrange("(kc p) n -> p kc n", p=128)
        it = 0
        for nb in range(4):
            ns = slice(nb * 512, (nb + 1) * 512)
            S.dma('sp', 'wf', out=wf, in_=w_out_v[:, :, ns], writes=['wf'])
            S.op('dve', lambda e: e.tensor_tensor(out=wo, in0=wf, in1=bc(gate1B[:, ns], 1, KC), op=ALU.mult),
                 reads=['wf', 'gate1B'], writes=['wo'])
            for tt in range(8):
                b_ = it % 2
                it += 1
                S.dma('sp', f'xr{b_}', out=xr[b_], in_=xh[2048 + tt * 128:2048 + (tt + 1) * 128, ns], writes=[f'xr{b_}'])
                pb, pn = bank()
                for kc in range(KC):
                    S.op('pe', lambda e: e.matmul(out=pb[:, :], lhsT=mergedT[:, kc, tt * 128:(tt + 1) * 128], rhs=wo[:, kc, :],
                                                  start=(kc == 0), stop=(kc == KC - 1)), reads=['mergedT', 'wo'], writes=[pn])
                S.op('dve', lambda e: e.tensor_tensor(out=x1[:, tt, ns], in0=pb[:, :], in1=xr[b_], op=ALU.add),
                     reads=[pn, f'xr{b_}'], writes=['x1'])
        S.barrier()

        if stage == "x1":
            for tt in range(8):
                S.dma('sp', 'out', out=y[tt * 128:(tt + 1) * 128, :], in_=x1[:, tt, :], reads=['x1'])
            S.finish()
            return nc

        BM = CONST_W + X1_W
        P = Arena(big, BM, TOT)
        h2T = P.alloc([KC, NT], BF16)
        p_mark = P.lo
        junk = [P.alloc([D], BF16)]
        xsb = [P.alloc([D], BF16) for _ in range(2)]
        for tt in range(8):
            norm_T(x1[:, tt, :], 'x1', h2T[:, :, tt * 128:(tt + 1) * 128], 'h2T', A2T, 'A2T', sh2T, junk, xsb, 'modT2')
        S.barrier()
        P.lo = p_mark
        e1T = P.alloc([NT], F32)
        e2T = P.alloc([NT], F32)
        gT = P.alloc([NT], F32)
        g_mark = P.lo
        wqs = [P.alloc([KC, 128], BF16) for _ in range(2)]
        qpT = P.alloc([16, 512], F32)
        s_sb = P.alloc([16, 128], F32)
        v1 = P.alloc([16, 16], F32)
        i1 = P.alloc([16, 16], U32)
        i1f = P.alloc([16, 16], F32)
        wks = [P.alloc([128], F32) for _ in range(4)]
        cand = P.alloc([8, 256], F32)
        wk2s = [P.alloc([256], F32) for _ in range(2)]
        top = P.alloc([8, 16], F32)
        ci = P.alloc([8, 16], U32)
        cu = P.alloc([8, 16], U32)
        af = P.alloc([8, 16], F32)
        bf_ = P.alloc([8, 16], F32)
        ex = P.alloc([8, 16], F32)
        zs = P.alloc([8], F32)
        gg = P.alloc([8, 16], F32)
        oh = cand
        e1f = P.alloc([8, 16], F32)
        e2f = P.alloc([8, 16], F32)
        w_q_v = w_q.rearrange("(kc p) n -> p kc n", p=128)
        iota16 = cst[:, 512:528]
        iota128 = cst[:, 384:512]
        wqi = 0
        for half in range(2):
            for cc in range(16):
                s_ = wqi % 2
                wqi += 1
                S.dma('pool', f'wq{s_}', out=wqs[s_], in_=w_q_v[:, :, cc * 128:(cc + 1) * 128], writes=[f'wq{s_}'])
                pb, pn = bank()
                projT(wqs[s_], f'wq{s_}', h2T, 'h2T', half * 512, 512, pb, pn)
                S.op('act', lambda e: e.activation(out=qpT[:, cc, :], in_=pb[:, :], func=AF.Copy), reads=[pn], writes=['qpT'])
            for t4 in range(4):
                tt = half * 4 + t4
                for b4 in range(4):
                    pb, pn = bank()
                    for k4 in range(4):
                        hp = b4 * 4 + k4
                        S.op('pe', lambda e: e.matmul(out=pb[:, k4 * 128:(k4 + 1) * 128], lhsT=qpT[:, hp, t4 * 128:(t4 + 1) * 128],
                                                      rhs=skT[:, hp, :], start=True, stop=True), reads=['qpT', 'skT'], writes=[pn])
                    S.op('act', lambda e: e.activation(out=s_sb[:, b4 * 4:(b4 + 1) * 4, :].rearrange("p a b -> p (a b)"), in_=pb[:, :], func=AF.Copy),
                         reads=[pn], writes=['s_sb'])
                NCH = 4
                for hp0 in range(0, 16, NCH):
                    hps = list(range(hp0, hp0 + NCH))
                    for hp in hps:
                        S.op('dve', lambda e: e.max(out=v1[:, hp, 0:8], in_=s_sb[:, hp, :]), reads=['s_sb'], writes=[f'v1a{hp}'])
                    for hp in hps:
                        S.op('dve', lambda e: e.max_index(out=i1[:, hp, 0:8], in_max=v1[:, hp, 0:8], in_values=s_sb[:, hp, :]),
                             reads=['s_sb', f'v1a{hp}'], writes=[f'i1a{hp}'])
                    for hp in hps:
                        S.op('dve', lambda e: e.match_replace(out=wks[hp % NCH], in_to_replace=v1[:, hp, 0:8], in_values=s_sb[:, hp, :], imm_value=-1e30),
                             reads=['s_sb', f'v1a{hp}'], writes=[f'wk{hp % NCH}'])
                    for hp in hps:
                        S.op('dve', lambda e: e.max(out=v1[:, hp, 8:16], in_=wks[hp % NCH]), reads=[f'wk{hp % NCH}'], writes=[f'v1b{hp}'])
                    for hp in hps:
                        S.op('dve', lambda e: e.max_index(out=i1[:, hp, 8:16], in_max=v1[:, hp, 8:16], in_values=wks[hp % NCH]),
                             reads=[f'wk{hp % NCH}', f'v1b{hp}'], writes=[f'i1b{hp}'])
                v1all = [f'v1a{hp}' for hp in range(16)] + [f'v1b{hp}' for hp in range(16)]
                i1all = [f'i1a{hp}' for hp in range(16)] + [f'i1b{hp}' for hp in range(16)]
                v1v = v1.rearrange("p (h q) a -> p h q a", q=2)
                i1fv = i1f.rearrange("p (h q) a -> p h q a", q=2)
                cand4 = cand.rearrange("p h (a b) -> p h a b", a=16)
                S.op('dve', lambda e: e.tensor_tensor(out=cand4, in0=bc(v1v[:, :, 0, :], 3, 16), in1=bc(v1v[:, :, 1, :], 2, 16), op=ALU.add),
                     reads=v1all, writes=['cand'])
                for h0 in range(0, 8, 2):
                    hs2 = (h0, h0 + 1)
                    for h in hs2:
                        S.op('dve', lambda e: e.max(out=top[:, h, 0:8], in_=cand[:, h, :]), reads=['cand'], writes=[f'topa{h}'])
                    for h in hs2:
                        S.op('dve', lambda e: e.max_index(out=ci[:, h, 0:8], in_max=top[:, h, 0:8], in_values=cand[:, h, :]),
                             reads=['cand', f'topa{h}'], writes=[f'cia{h}'])
                    for h in hs2:
                        S.op('dve', lambda e: e.match_replace(out=wk2s[h % 2], in_to_replace=top[:, h, 0:8], in_values=cand[:, h, :], imm_value=-1e30),
                             reads=['cand', f'topa{h}'], writes=[f'wk2{h % 2}'])
                    for h in hs2:
                        S.op('dve', lambda e: e.max(out=top[:, h, 8:16], in_=wk2s[h % 2]), reads=[f'wk2{h % 2}'], writes=[f'topb{h}'])
                    for h in hs2:
                        S.op('dve', lambda e: e.max_index(out=ci[:, h, 8:16], in_max=top[:, h, 8:16], in_values=wk2s[h % 2]),
                             reads=[f'wk2{h % 2}', f'topb{h}'], writes=[f'cib{h}'])
                topall = [f'topa{h}' for h in range(8)] + [f'topb{h}' for h in range(8)]
                ciall = [f'cia{h}' for h in range(8)] + [f'cib{h}' for h in range(8)]
                S.op('dve', lambda e: e.tensor_tensor(out=ex, in0=top, in1=bc(top[:, :, 0], 2, 16), op=ALU.subtract), reads=topall, writes=['ex'])
                S.op('act', lambda e: e.activation(out=ex, in_=ex, func=AF.Exp), reads=['ex'], writes=['ex'])
                S.op('dve', lambda e: e.tensor_reduce(out=zs, in_=ex, axis=AX.X, op=ALU.add), reads=['ex'], writes=['zs'])
                S.op('dve', lambda e: e.reciprocal(out=zs, in_=zs), reads=['zs'], writes=['zs'])
                S.op('dve', lambda e: e.tensor_tensor(out=gg, in0=ex, in1=bc(zs, 2, 16), op=ALU.mult), reads=['ex', 'zs'], writes=['gg'])
                S.op('dve', lambda e: e.tensor_copy(out=i1f, in_=i1), reads=i1all, writes=['i1f'])
                S.op('dve', lambda e: e.tensor_scalar(out=cu, in0=ci, scalar1=4, scalar2=None, op0=ALU.logical_shift_right), reads=ciall, writes=['cu'])
                S.op('dve', lambda e: e.tensor_copy(out=af, in_=cu), reads=['cu'], writes=['af'])
                S.op('dve', lambda e: e.tensor_scalar(out=cu, in0=ci, scalar1=15, scalar2=None, op0=ALU.bitwise_and), reads=ciall, writes=['cu'])
                S.op('dve', lambda e: e.tensor_copy(out=bf_, in_=cu), reads=['cu'], writes=['bf'])
                oh4 = oh.rearrange("p h (k a) -> p h k a", k=16)
                io4 = bc(bc(iota16, 1, 16), 1, 8)
                for (src, q_, dst, dn_) in ((af, 0, e1f, 'e1f'), (bf_, 1, e2f, 'e2f')):
                    S.op('dve', lambda e: e.tensor_tensor(out=oh4, in0=io4, in1=bc(src, 3, 16), op=ALU.is_equal), reads=['af', 'bf', 'cst'], writes=['cand'])
                    S.op('dve', lambda e: e.tensor_tensor(out=oh4, in0=oh4, in1=bc(i1fv[:, :, q_, :], 2, 16), op=ALU.mult), reads=['cand', 'i1f'], writes=['cand'])
                    S.op('dve', lambda e: e.tensor_reduce(out=dst, in_=oh4, axis=AX.X, op=ALU.add), reads=['cand'], writes=[dn_])
                pb, pn = bank()
                for k3, (src, sn) in enumerate(((e1f, 'e1f'), (e2f, 'e2f'), (gg, 'gg'))):
                    S.op('pe', lambda e: e.transpose(out=pb[:, k3 * 128:(k3 + 1) * 128], in_=src.rearrange("p h k -> p (h k)"), identity=ident_f),
                         reads=[sn, 'cst'], writes=[pn])
                for k3, (dst, dn_) in enumerate(((e1T, 'e1T'), (e2T, 'e2T'), (gT, 'gT'))):
                    S.op('act', lambda e: e.activation(out=dst[:, tt * 128:(tt + 1) * 128], in_=pb[:, k3 * 128:(k3 + 1) * 128], func=AF.Copy),
                         reads=[pn], writes=[dn_])
        S.barrier()
        if stage == "R":
            dump(e1T, 1024, 'e1T'); dump(e2T, 1024, 'e2T'); dump(gT, 1024, 'gT')
            S.finish()
            return nc
        P.lo = g_mark
        iob = P.alloc([128], BF16)
        e1b = P.alloc([NT], BF16)
        e2b = P.alloc([NT], BF16)
        gb16 = P.alloc([NT], BF16)
        S.op('dve', lambda e: e.tensor_copy(out=iob, in_=iota128), reads=['cst'], writes=['iob'])
        S.op('dve', lambda e: e.tensor_copy(out=e1b, in_=e1T), reads=['e1T'], writes=['e1b'])
        S.op('dve', lambda e: e.tensor_copy(out=e2b, in_=e2T), reads=['e2T'], writes=['e2b'])
        S.op('dve', lambda e: e.tensor_copy(out=gb16, in_=gT), reads=['gT'], writes=['gb16'])
        P1s = [P.alloc([16, 128], BF16) for _ in range(2)]
        P2s = [P.alloc([16, 128], BF16) for _ in range(2)]
        P2g = [P.alloc([16, 128], BF16) for _ in range(2)]
        Gs = P.alloc([128, 128], BF16)
        gscr_v = gscr.rearrange("j i t -> i j t")
        gi = 0
        ev_i = 0
        for tb in range(8):
            for g16 in range(8):
                t0 = tb * 128 + g16 * 16
                b_ = gi % 2
                gi += 1
                for tl in range(16):
                    S.op('dve', lambda e: e.tensor_scalar(out=P1s[b_][:, tl, :], in0=iob, scalar1=e1T[:, t0 + tl:t0 + tl + 1], scalar2=None,
                                                          op0=ALU.is_equal), reads=['iob', 'e1T'], writes=[f'P1{b_}'])
                    S.op('dve', lambda e: e.tensor_scalar(out=P2g[b_][:, tl, :], in0=iob, scalar1=e2T[:, t0 + tl:t0 + tl + 1],
                                                          scalar2=gT[:, t0 + tl:t0 + tl + 1], op0=ALU.is_equal, op1=ALU.mult),
                         reads=['iob', 'e2T', 'gT'], writes=[f'P2g{b_}'])
                for q4 in range(4):
                    pb, pn = bank()
                    for k4 in range(4):
                        tl = q4 * 4 + k4
                        S.op('pe', lambda e: e.matmul(out=pb[:, k4 * 128:(k4 + 1) * 128], lhsT=P1s[b_][:, tl, :], rhs=P2g[b_][:, tl, :],
                                                      start=True, stop=True), reads=[f'P1{b_}', f'P2g{b_}'], writes=[pn])
                    c0 = g16 * 16 + q4 * 4
                    src = pb[:, :].rearrange("p (t j) -> p j t", t=4)
                    S.op('act', lambda e: e.activation(out=Gs[:, :, c0:c0 + 4], in_=src, func=AF.Copy), reads=[pn], writes=['Gs'])
                    ev_i += 1
            for jq in range(4):
                S.dma('sp', 'gsw', out=gscr_v[:, jq * 32:(jq + 1) * 32, tb * 128:(tb + 1) * 128], in_=Gs[:, jq * 32:(jq + 1) * 32, :],
                      reads=['Gs'], writes=['gscr'])
        S.barrier()
        P.lo = p_mark
        JG = 4
        NG = 128 // JG
        Gt = [P.alloc([JG, NT], BF16) for _ in range(2)]
        NWU = 4
        wup = [P.alloc([KC, 128], BF16) for _ in range(NWU)]
        NWD = 7
        wdn = [P.alloc([D], BF16) for _ in range(NWD)]
        GaT = P.alloc([JG, NT], BF16)
        gel = [P.alloc([512], F32) for _ in range(2)]
        evb = [P.alloc([512], F32) for _ in range(4)]
        st = {'u': 0, 'd': 0}
        uslot = {}
        dslot = {}

        def load_G(g):
            S.dma('sp', f'Gt{g % 2}', out=Gt[g % 2], in_=gscr_v[:, g * JG:(g + 1) * JG, :], reads=['gscr'], writes=[f'Gt{g % 2}'])

        def load_up(j):
            us = st['u'] % NWU
            st['u'] += 1
            uslot[j] = us
            S.dma('pool', f'wup{us}', out=wup[us], in_=w_upP[j].rearrange("p (kc i) -> p kc i", kc=KC), writes=[f'wup{us}'])

        def load_dn(j):
            ds = st['d'] % NWD
            st['d'] += 1
            dslot[j] = ds
            S.dma('pool', f'wdn{ds}', out=wdn[ds].rearrange("p (a b) -> p a b", a=4), in_=w_dnP[j].rearrange("p (a b) -> p a b", a=4),
                  writes=[f'wdn{ds}'])

        load_G(0)
        for jj in range(JG):
            load_up(jj)
            load_dn(jj)
        gl = 0
        ei = 0
        for g in range(NG):
            b_ = g % 2
            j0 = g * JG
            for jj in range(JG):
                us = uslot[j0 + jj]
                for half in range(2):
                    hs = slice(half * 512, (half + 1) * 512)
                    pb, pn = bank()
                    projT(wup[us], f'wup{us}', h2T, 'h2T', half * 512, 512, pb, pn)
                    gb = gl % 2
                    gl += 1
                    S.op('act', lambda e: e.activation(out=gel[gb], in_=pb[:, :], func=AF.Gelu), reads=[pn], writes=[f'gel{gb}'])
                    S.op('dve', lambda e: e.tensor_tensor(out=GaT[:, jj, hs], in0=gel[gb], in1=Gt[b_][:, jj, hs], op=ALU.mult),
                         reads=[f'gel{gb}', f'Gt{b_}'], writes=['GaT'])
            if g + 1 < NG:
                load_G(g + 1)
                for jj in range(JG):
                    load_up(j0 + JG + jj)
                for jj in range(JG - 1):
                    load_dn(j0 + JG + jj)
            for tt in range(8):
                for dq in range(4):
                    ds_ = slice(dq * 512, (dq + 1) * 512)
                    pb, pn = bank()
                    for jj in range(JG):
                        dsl = dslot[j0 + jj]
                        S.op('pe', lambda e: e.matmul(out=pb[:, :], lhsT=GaT[:, jj, tt * 128:(tt + 1) * 128], rhs=wdn[dsl][:, ds_],
                                                      start=(jj == 0), stop=(jj == JG - 1)), reads=['GaT', f'wdn{dsl}'], writes=[pn])
                    eb = ei % 4
                    ei += 1
                    S.op('dve', lambda e: e.tensor_tensor(out=evb[eb], in0=pb[:, :], in1=gate2B[:, ds_], op=ALU.mult),
                         reads=[pn, 'gate2B'], writes=[f'ev{eb}'])
                    xres = f'x1_{tt}_{dq}'
                    if eb % 2 == 0:
                        S.op('pool', lambda e: e.tensor_tensor(out=x1[:, tt, ds_], in0=x1[:, tt, ds_], in1=evb[eb], op=ALU.add),
                             reads=[f'ev{eb}', xres, 'x1'], writes=[xres])
                    else:
                        S.op('dve', lambda e: e.tensor_tensor(out=x1[:, tt, ds_], in0=x1[:, tt, ds_], in1=evb[eb], op=ALU.add),
                             reads=[f'ev{eb}', xres, 'x1'], writes=[xres])
            if g + 1 < NG:
                load_dn(j0 + JG + JG - 1)
        for tt in range(8):
            S.dma('sp', 'out', out=y[tt * 128:(tt + 1) * 128, :], in_=x1[:, tt, :], reads=['x1'] + [f'x1_{tt}_{dq}' for dq in range(4)])
        S.finish()
    return nc


_CACHE = {}


def _host_consts():
    cst = np.zeros((128, NCS), np.float32)
    cst[:, 0:128] = np.eye(128, dtype=np.float32)
    mk = np.arange(128)[:, None]
    mq = np.arange(128)[None, :]
    cst[:, 128:256] = np.where(mq >= mk, 0.0, NEG)
    cst[:, 256:384] = np.where(mq <= mk, 0.0, NEG)
    cst[:, 384:512] = np.arange(128, dtype=np.float32)[None, :]
    cst[:, 512:528] = np.arange(16, dtype=np.float32)[None, :]
    return cst


def _fmT(v, n):
    return np.ascontiguousarray(np.asarray(v, np.float32).reshape(n, 128).T)


def kernel(**inputs):
    f = lambda k: np.asarray(inputs[k], np.float32)
    x, c = f("x"), f("c")
    if "nc" not in _CACHE:
        _CACHE["nc"] = build_program()
    nc = _CACHE["nc"]
    cst = _host_consts()
    sk = f("peer_sub_keys")[0]
    skT = np.ascontiguousarray(sk.reshape(16, 128, 128).transpose(2, 0, 1).reshape(128, 2048))
    w_up = f("peer_w_up")[0]
    w_dn = f("peer_w_down")[0]
    w_upP = np.ascontiguousarray(w_up.reshape(128, 128, KC, 128).transpose(1, 3, 2, 0).reshape(128, 128, KC * 128))
    w_dnP = np.ascontiguousarray(w_dn.reshape(128, 128, D).transpose(1, 0, 2))
    shared = {
        "cst": cst, "skT": skT, "w_ada": f("w_ada")[0], "w_in": f("w_in")[0], "w_co": f("w_conv_out")[0],
        "w_ao": f("w_attn_o")[0], "w_out": f("w_out")[0], "w_q": f("peer_w_q")[0], "w_upP": w_upP, "w_dnP": w_dnP,
    }
    fm_shared = np.zeros((128, NFM), np.float32)

    def put(a, name, arr):
        o0, o1 = FM[name]
        a[:, o0:o1] = arr
    put(fm_shared, "bada", _fmT(f("b_ada")[0], 96))
    put(fm_shared, "g1", _fmT(f("norm1_g")[0], 16))
    put(fm_shared, "g2", _fmT(f("norm2_g")[0], 16))
    dw = f("conv_dw")[0]
    put(fm_shared, "dw", np.ascontiguousarray(dw.reshape(31, 8, 128).transpose(2, 1, 0).reshape(128, 248)))
    put(fm_shared, "db", _fmT(f("conv_db")[0], 8))
    put(fm_shared, "lng", _fmT(f("conv_ln_g")[0], 8))
    put(fm_shared, "lnb", _fmT(f("conv_ln_b")[0], 8))
    put(fm_shared, "bco", _fmT(f("b_conv_out")[0], 16))
    put(fm_shared, "qg", np.ascontiguousarray(f("q_norm_g")[0].T))
    put(fm_shared, "kg", np.ascontiguousarray(f("k_norm_g")[0].T))
    in_maps = []
    for core in range(8):
        b, q = core // 4, core % 4
        lo = 1024 * q - 2048
        xhh = np.zeros((LT, D), np.float32)
        s0 = max(lo, 0)
        xhh[s0 - lo:] = x[b, s0:1024 * q + 1024]
        valid = (lo + np.arange(LT)) >= 0
        kbias = np.where(valid, 0.0, NEG).astype(np.float32)
        fmc_ = fm_shared.copy()
        put(fmc_, "cT", _fmT(c[b], 16))
        put(fmc_, "hval", np.full((128, 1), 1.0 if q > 0 else 0.0, np.float32))
        p = np.arange(128)
        put(fmc_, "kb1", np.stack([kbias[1920 + 128 * i + p] for i in range(9)], axis=1))
        put(fmc_, "kb2", np.stack([kbias[1536 + 4 * (128 * kt + p) + r] for r in range(4) for kt in range(3)], axis=1))
        put(fmc_, "kb3", np.stack([kbias[16 * p + r] for r in range(16)], axis=1))
        m = dict(shared)
        m["xh"] = xhh
        m["fm"] = fmc_
        in_maps.append(m)
    res = run_bass_kernel_spmd(nc, in_maps, core_ids=list(range(8)))
    out = np.zeros((2, 4096, D), np.float32)
    for core in range(8):
        b, q = core // 4, core % 4
        out[b, 1024 * q:1024 * q + 1024] = res.results[core]["y"]
    return out
```

```python
import contextlib
import numpy as np
import concourse.bass as bass
import concourse.mybir as mybir
from concourse.bass_utils import run_bass_kernel_spmd

F32 = mybir.dt.float32
BF16 = mybir.dt.bfloat16
U32 = mybir.dt.uint32
AF = mybir.ActivationFunctionType
ALU = mybir.AluOpType
AX = mybir.AxisListType

D = 2048
KC = 16
NT = 1024
LT = 3072
EPS = 1e-6
NEG = -30000.0
IN_COLS = 10752
CH_Q, CH_K, CH_V, CH_G = 16, 28, 40, 52

FM = {}
_o = 0
for _n, _w in [("cT", 16), ("bada", 96), ("g1", 16), ("g2", 16), ("dw", 248), ("db", 8), ("lng", 8), ("lnb", 8),
               ("bco", 16), ("qg", 12), ("kg", 12), ("hval", 1), ("kb1", 9), ("kb2", 12), ("kb3", 16)]:
    FM[_n] = (_o, _o + _w)
    _o += _w
NFM = _o
CS = {"ident": (0, 128), "mcur": (128, 256), "mprev": (256, 384), "iota": (384, 512), "iota16": (512, 528)}
NCS = 528

NO_SELF_SYNC = ("pe",)
STAGE = "full"


class Sched:
    def __init__(self, nc, es):
        self.nc = nc
        self.es = es
        self.eng = {'pe': nc.tensor, 'act': nc.scalar, 'dve': nc.vector, 'pool': nc.gpsimd, 'sp': nc.sync}
        self.sem = {k: es.enter_context(nc.semaphore('sem_' + k)) for k in self.eng}
        self.cnt = {k: 0 for k in self.eng}
        self.seen = {k: {} for k in self.eng}
        self.last_w = {}
        self.readers = {}
        self.dsem = {}
        self.dcnt = {}
        self.bank_i = 0

    def _semof(self, key):
        return self.sem[key] if key in self.sem else self.dsem[key]

    def _deps(self, reads, writes):
        deps = {}

        def add(k, c):
            if deps.get(k, 0) < c:
                deps[k] = c
        for r in reads:
            ev = self.last_w.get(r)
            if ev is not None:
                add(*ev)
        for w in writes:
            ev = self.last_w.get(w)
            if ev is not None:
                add(*ev)
            for k, c in self.readers.get(w, {}).items():
                add(k, c)
        return deps

    def _wait(self, e, deps):
        for k, c in deps.items():
            if k == e and e in NO_SELF_SYNC:
                continue
            if self.seen[e].get(k, 0) >= c:
                continue
            self.eng[e].wait_ge(self._semof(k), c)
            self.seen[e][k] = c

    def _record(self, ev, reads, writes):
        k, c = ev
        for r in reads:
            self.readers.setdefault(r, {})[k] = c
        for w in writes:
            self.last_w[w] = ev
            self.readers[w] = {}

    def op(self, e, fn, reads=(), writes=()):
        self._wait(e, self._deps(reads, writes))
        ins = fn(self.eng[e])
        self.cnt[e] += 1
        ins.then_inc(self.sem[e], 1)
        self._record((e, self.cnt[e]), reads, writes)
        return ins

    def dma(self, q, semname, reads=(), writes=(), out=None, in_=None, fn=None, **kw):
        if semname not in self.dsem:
            self.dsem[semname] = self.es.enter_context(self.nc.semaphore('d_' + semname))
            self.dcnt[semname] = 0
        self._wait(q, self._deps(reads, writes))
        if fn is not None:
            ins = fn(self.eng[q])
        else:
            ins = self.eng[q].dma_start(out=out, in_=in_, **kw)
        self.dcnt[semname] += 16
        ins.then_inc(self.dsem[semname], 16)
        self._record((semname, self.dcnt[semname]), reads, writes)
        return ins

    def barrier(self):
        evs = {k: c for k, c in self.cnt.items() if c > 0}
        evs.update({k: c for k, c in self.dcnt.items() if c > 0})
        for e in self.eng:
            self._wait(e, dict(evs))

    def finish(self, q='sp'):
        evs = {k: c for k, c in self.dcnt.items() if c > 0}
        evs.update({k: c for k, c in self.cnt.items() if c > 0 and k != q})
        self._wait(q, evs)


class Arena:
    def __init__(self, base_ap, lo, hi):
        self.base = base_ap
        self.lo0, self.hi0 = lo, hi
        self.lo, self.hi = lo, hi

    def reset(self):
        self.lo, self.hi = self.lo0, self.hi0

    def alloc(self, shape, dtype, top=False):
        n = int(np.prod(shape))
        isz = 4 if dtype in (F32, U32) else 2
        words = (n * isz + 3) // 4
        if top:
            self.hi -= words
            off = self.hi
        else:
            off = self.lo
            self.lo += words
        assert self.lo <= self.hi, ("arena overflow", self.lo, self.hi)
        v = self.base[:, off:off + words]
        if dtype != F32:
            v = v.bitcast(dtype)
        v = v[:, 0:n]
        if len(shape) == 2:
            v = v.rearrange("p (a b) -> p a b", a=shape[0], b=shape[1])
        elif len(shape) == 3:
            v = v.rearrange("p (a b c) -> p a b c", a=shape[0], b=shape[1], c=shape[2])
        return v


def bc(ap2, reps_axis, n):
    pat = [list(x) for x in ap2.ap]
    pat.insert(reps_axis, [0, n])
    return bass.AP(tensor=ap2.tensor, offset=ap2.offset, ap=pat)


def build_program(stage=None):
    stage = stage or STAGE
    nc = bass.Bass("TRN2", target_bir_lowering=False)
    early = stage in ("A", "H", "M1")
    def dram(n, s, d=F32, kind="ExternalInput"):
        if early and n in ("w_co", "w_ao", "w_out", "w_q", "w_upP", "w_dnP") or (stage == "A" and n in ("w_in", "xh")):
            return None
        if (stage == "x1" and n in ("w_q", "w_upP", "w_dnP")) or (stage == "R" and n in ("w_upP", "w_dnP")):
            return None
        return nc.dram_tensor(n, s, d, kind=kind).ap()
    dbg = nc.dram_tensor("dbg", [128, 8192], F32, kind="ExternalOutput").ap() if stage != "full" else None
    xh = dram("xh", [LT, D])
    fm_d = dram("fm", [128, NFM])
    cst_d = dram("cst", [128, NCS])
    skT_d = dram("skT", [128, 2048])
    w_ada = dram("w_ada", [D, 6 * D])
    w_in = dram("w_in", [D, IN_COLS])
    w_co = dram("w_co", [1024, D])
    w_ao = dram("w_ao", [512, D])
    w_out = dram("w_out", [D, D])
    w_q = dram("w_q", [D, D])
    w_upP = dram("w_upP", [128, 128, KC * 128])
    w_dnP = dram("w_dnP", [128, 128, D])
    y = dram("y", [NT, D], F32, "ExternalOutput")
    gscr = nc.dram_tensor("gscr", [128, 128, NT], BF16, kind="Internal").ap()

    with contextlib.ExitStack() as es:
        S = Sched(nc, es)
        TOT = 53100
        big = es.enter_context(nc.sbuf_tensor("arena", [128, TOT], F32))
        pbs = [es.enter_context(nc.psum_tensor(f"pb{i}", [128, 512], F32)) for i in range(8)]
        CONST_W = 7900
        X1_W = 16384
        AC = Arena(big, 0, CONST_W)
        AX1 = Arena(big, CONST_W, CONST_W + X1_W)
        AM = Arena(big, CONST_W + X1_W, TOT)

        dbgc = {'c': 0}

        def dump(ap, n, res):
            c0 = dbgc['c']
            dbgc['c'] += n
            S.dma('pool', 'dbg', out=dbg[:, c0:c0 + n], in_=ap, reads=[res])
            return c0

        bank_set = {'s': list(range(8))}

        def bank():
            bs = bank_set['s']
            i = bs[S.bank_i % len(bs)]
            S.bank_i += 1
            return pbs[i], f"pb{i}"

        fm = AC.alloc([NFM], F32)
        cst = AC.alloc([NCS], F32)
        skT = AC.alloc([16, 128], F32)
        idb = AC.alloc([128], BF16)
        mcur_b = AC.alloc([128], BF16)
        mprev_b = AC.alloc([128], BF16)
        ones_f = AC.alloc([128], F32)
        ones_b = AC.alloc([128], BF16)
        sc = AC.alloc([16], F32)
        scb = AC.alloc([16], BF16)
        modT = AC.alloc([96], F32)
        modT2 = modT
        A1T = AC.alloc([16], F32)
        A2T = AC.alloc([16], F32)
        gate1B = AC.alloc([D], F32)
        gate2B = AC.alloc([D], F32)
        nsc = [(AC.alloc([1], F32), AC.alloc([1], F32), AC.alloc([1], F32)) for _ in range(2)]
        nstate = {'i': 0, 'q': 0}
        epsc = AC.alloc([2], F32)
        eps1 = epsc[:, 0:1]
        eps128 = epsc[:, 1:2]
        diagf = AC.alloc([128], F32)

        def fmc(name, a=None, b=None):
            o0, o1 = FM[name]
            if a is None:
                return fm[:, o0:o1]
            return fm[:, o0 + a:o0 + (b if b is not None else a + 1)]
        ident_f = cst[:, 0:128]

        S.dma('sp', 'c0', out=fm, in_=fm_d[:, :], writes=['fm'])
        S.dma('sp', 'c1', out=cst, in_=cst_d[:, :], writes=['cst'])
        S.dma('sp', 'c2', out=skT.rearrange("p a b -> p (a b)"), in_=skT_d[:, :], writes=['skT'])
        S.op('dve', lambda e: e.tensor_copy(out=idb, in_=ident_f), reads=['cst'], writes=['idb'])
        S.op('dve', lambda e: e.tensor_copy(out=mcur_b, in_=cst[:, 128:256]), reads=['cst'], writes=['mcur'])
        S.op('dve', lambda e: e.tensor_copy(out=mprev_b, in_=cst[:, 256:384]), reads=['cst'], writes=['mprev'])
        S.op('dve', lambda e: e.memset(ones_f, 1.0), writes=['ones_f'])
        S.op('dve', lambda e: e.memset(ones_b, 1.0), writes=['ones_b'])
        S.op('dve', lambda e: e.memset(eps1, EPS), writes=['epsc'])
        S.op('dve', lambda e: e.memset(eps128, 128.0 * EPS), writes=['epsc'])

        S.op('act', lambda e: e.activation(out=sc, in_=fmc("cT"), func=AF.Silu), reads=['fm'], writes=['sc'])
        S.op('dve', lambda e: e.tensor_copy(out=scb, in_=sc), reads=['sc'], writes=['scb'])
        wa = [AX1.alloc([16, 512], BF16) for _ in range(3)]
        w_ada_v = w_ada.rearrange("(kc p) n -> p kc n", p=128)
        for nb in range(8):
            s_ = nb % 3
            S.dma('pool', f'wa{s_}', out=wa[s_], in_=w_ada_v[:, :, nb * 512:(nb + 1) * 512], writes=[f'wa{s_}'])
            pb, pn = bank()
            for sub in range(4):
                for kc in range(KC):
                    S.op('pe', lambda e: e.matmul(out=pb[:, sub:sub + 1], lhsT=wa[s_][:, kc, sub * 128:(sub + 1) * 128],
                                                  rhs=scb[:, kc:kc + 1], start=(kc == 0), stop=(kc == KC - 1)),
                         reads=[f'wa{s_}', 'scb'], writes=[pn])
            S.op('dve', lambda e: e.tensor_tensor(out=modT[:, nb * 4:nb * 4 + 4], in0=pb[:, 0:4],
                                                  in1=fmc("bada", nb * 4, nb * 4 + 4), op=ALU.add),
                 reads=[pn, 'fm'], writes=['modT'])
        ada_state = {'col': 32, 'i': 0}

        def ada_deferred(wad, nblk):
            for _ in range(nblk):
                col = ada_state['col']
                if col >= 96:
                    return
                ada_state['col'] += 1
                s_ = ada_state['i'] % 2
                ada_state['i'] += 1
                S.dma('pool', f'wad{s_}', out=wad[s_], in_=w_ada_v[:, :, col * 128:(col + 1) * 128], writes=[f'wad{s_}'])
                pb, pn = bank()
                for kc in range(KC):
                    S.op('pe', lambda e: e.matmul(out=pb[:, 0:1], lhsT=wad[s_][:, kc, :], rhs=scb[:, kc:kc + 1],
                                                  start=(kc == 0), stop=(kc == KC - 1)), reads=[f'wad{s_}', 'scb'], writes=[pn])
                S.op('dve', lambda e: e.tensor_tensor(out=modT2[:, col:col + 1], in0=pb[:, 0:1], in1=fmc("bada", col), op=ALU.add),
                     reads=[pn, 'fm'], writes=['modT2'])
        S.op('dve', lambda e: e.scalar_tensor_tensor(out=A1T, in0=modT[:, 16:32], scalar=1.0, in1=fmc("g1"),
                                                     op0=ALU.add, op1=ALU.mult), reads=['modT', 'fm'], writes=['A1T'])
        sh1T = modT[:, 0:16]
        sh2T = modT[:, 48:64]

        def bcast_cols(colsT, dst, dname):
            for g4 in range(4):
                pb, pn = bank()
                for k4 in range(4):
                    kc = g4 * 4 + k4
                    S.op('dve', lambda e: e.tensor_scalar(out=diagf, in0=ident_f, scalar1=colsT[:, kc:kc + 1], scalar2=None,
                                                          op0=ALU.mult), reads=['cst', 'modT2'], writes=['diagf'])
                    S.op('pe', lambda e: e.matmul(out=pb[:, k4 * 128:(k4 + 1) * 128], lhsT=ones_f, rhs=diagf,
                                                  start=True, stop=True), reads=['ones_f', 'diagf'], writes=[pn])
                S.op('act', lambda e: e.activation(out=dst[:, g4 * 512:(g4 + 1) * 512], in_=pb[:, :], func=AF.Copy),
                     reads=[pn], writes=[dname])
        S.barrier()
        AX1.reset()
        if stage == "A":
            dump(modT, 96, 'modT'); dump(A1T, 16, 'A1T')
            S.finish()
            return nc

        def norm_T(src, src_res, dst3, dst_res, AT, ares, shT, junks, xss, shres='modT'):
            ni = nstate['i'] % 2
            nstate['i'] += 1
            ss, rs, rstd = nsc[ni]
            junk = junks[ni % len(junks)]
            xs = xss[ni % len(xss)]
            jn, xn_, sn, rn, rdn = f'junk{ni}', f'xs{ni}', f'ss{ni}', f'rs{ni}', f'rstd{ni}'
            S.op('act', lambda e: e.activation(out=junk, in_=src, func=AF.Square, accum_out=ss),
                 reads=[src_res], writes=[jn, sn])
            S.op('act', lambda e: e.activation(out=rs, in_=ss, func=AF.Sqrt, scale=1.0 / D, bias=EPS),
                 reads=[sn], writes=[rn])
            S.op('dve', lambda e: e.reciprocal(out=rstd, in_=rs), reads=[rn], writes=[rdn])
            S.op('act', lambda e: e.activation(out=xs, in_=src, func=AF.Identity, scale=rstd),
                 reads=[src_res, rdn], writes=[xn_])
            for half in range(2):
                pb, pn = bank()
                pv = pb[:, :].bitcast(BF16)
                for k in range(8):
                    kc = half * 8 + k
                    S.op('pe', lambda e: e.transpose(out=pv[:, k * 128:(k + 1) * 128], in_=xs[:, kc * 128:(kc + 1) * 128],
                                                     identity=idb), reads=[xn_, 'idb'], writes=[pn])
                for k in range(8):
                    kc = half * 8 + k
                    if k % 2 == 0:
                        S.op('dve', lambda e: e.tensor_scalar(out=dst3[:, kc, :], in0=pv[:, k * 128:(k + 1) * 128],
                                                              scalar1=AT[:, kc:kc + 1], scalar2=shT[:, kc:kc + 1],
                                                              op0=ALU.mult, op1=ALU.add),
                             reads=[pn, ares, shres], writes=[dst_res])
                    else:
                        S.op('act', lambda e: e.activation(out=dst3[:, kc, :], in_=pv[:, k * 128:(k + 1) * 128],
                                                           func=AF.Identity, scale=AT[:, kc:kc + 1], bias=shT[:, kc:kc + 1]),
                             reads=[pn, ares, shres], writes=[dst_res])

        w_in_v = w_in.rearrange("(kc p) n -> p kc n", p=128)
        wstate = {'i': 0}

        def load_w(ws, cc):
            s_ = wstate['i'] % len(ws)
            wstate['i'] += 1
            S.dma('pool', f'ws{s_}', out=ws[s_], in_=w_in_v[:, :, cc * 128:(cc + 1) * 128], writes=[f'ws{s_}'])
            return ws[s_], f'ws{s_}'

        def projT(wslot, wres, hT, hres, t0, n, pb, pn, c0=0):
            for kc in range(KC):
                S.op('pe', lambda e: e.matmul(out=pb[:, c0:c0 + n], lhsT=wslot[:, kc, :], rhs=hT[:, kc, t0:t0 + n],
                                              start=(kc == 0), stop=(kc == KC - 1)), reads=[wres, hres], writes=[pn])

        pend = {'f': None}

        def flush_norm():
            if pend['f'] is not None:
                f_ = pend['f']
                pend['f'] = None
                f_()

        def qk_norm(pb, pn, n, gcol, dst, dres, tmp, is_q):
            sqs, rks = tmp
            i = nstate['q'] % 2
            nstate['q'] += 1
            sq, rk = sqs[i], rks[i]
            flush_norm()
            S.op('act', lambda e: e.activation(out=sq[:, 0:n], in_=pb[:, 0:n], func=AF.Square), reads=[pn], writes=[f'sq{i}'])

            def rest():
                pb2, pn2 = bank()
                S.op('pe', lambda e: e.matmul(out=pb2[:, 0:n], lhsT=ones_f, rhs=sq[:, 0:n], start=True, stop=True),
                     reads=['ones_f', f'sq{i}'], writes=[pn2])
                if is_q:
                    S.op('act', lambda e: e.activation(out=rk[:, 0:n], in_=pb2[:, 0:n], func=AF.Sqrt, scale=1.0, bias=128.0 * EPS),
                         reads=[pn2], writes=[f'rk{i}'])
                else:
                    S.op('act', lambda e: e.activation(out=rk[:, 0:n], in_=pb2[:, 0:n], func=AF.Sqrt, scale=1.0 / 128, bias=EPS),
                         reads=[pn2], writes=[f'rk{i}'])
                S.op('dve', lambda e: e.reciprocal(out=rk[:, 0:n], in_=rk[:, 0:n]), reads=[f'rk{i}'], writes=[f'rk{i}'])
                S.op('dve', lambda e: e.scalar_tensor_tensor(out=dst, in0=pb[:, 0:n], scalar=gcol, in1=rk[:, 0:n],
                                                             op0=ALU.mult, op1=ALU.mult), reads=[pn, f'rk{i}', 'fm'], writes=[dres])
            pend['f'] = rest

        hTm = AM.alloc([KC, 1536], BF16)
        K3T = AM.alloc([4, LT], BF16)
        V3T = AM.alloc([4, LT], BF16)
        ws = [AM.alloc([KC, 128], BF16) for _ in range(3)]
        am_mark = (AM.lo, AM.hi)

        xin = [AX1.alloc([D], F32) for _ in range(3)]
        junk = [AX1.alloc([D], BF16)]
        xsb = [AX1.alloc([D], BF16) for _ in range(2)]
        hTh = AX1.alloc([KC, 512], BF16)
        sq = [AX1.alloc([512], F32) for _ in range(2)]
        rk = [AX1.alloc([512], F32) for _ in range(2)]
        xi = 0
        for tile in range(24):
            s_ = xi % 3
            xi += 1
            S.dma('sp', f'xin{s_}', out=xin[s_], in_=xh[tile * 128:(tile + 1) * 128, :], writes=[f'xin{s_}'])
            if tile < 12:
                blk, tt = tile // 4, tile % 4
                norm_T(xin[s_], f'xin{s_}', hTh[:, :, tt * 128:(tt + 1) * 128], 'hTh', A1T, 'A1T', sh1T, junk, xsb)
                if tt == 3:
                    for hd in range(4):
                        for kind in range(2):
                            cc = (CH_K if kind == 0 else CH_V) + 8 + hd
                            wsl, wr = load_w(ws, cc)
                            pb, pn = bank()
                            projT(wsl, wr, hTh, 'hTh', 0, 512, pb, pn)
                            if kind == 0:
                                qk_norm(pb, pn, 512, fmc("kg", 8 + hd), K3T[:, hd, blk * 512:(blk + 1) * 512], 'K3T', (sq, rk), False)
                            else:
                                S.op('act', lambda e: e.activation(out=V3T[:, hd, blk * 512:(blk + 1) * 512], in_=pb[:, :], func=AF.Copy),
                                     reads=[pn], writes=['V3T'])
                    flush_norm()
            else:
                flush_norm()
                m0 = (tile - 12) * 128
                norm_T(xin[s_], f'xin{s_}', hTm[:, :, m0:m0 + 128], 'hTm', A1T, 'A1T', sh1T, junk, xsb)
        S.barrier()
        if stage == "H":
            dump(hTm[:, 0, 0:512], 512, 'hTm'); dump(hTm[:, 5, 512:1024], 512, 'hTm'); dump(K3T[:, 1, 0:512], 512, 'K3T')
            dump(V3T[:, 2, 512:1024], 512, 'V3T'); dump(hTh[:, 3, :], 512, 'hTh')
            S.finish()
            return nc
        AX1.reset()

        attnT = AX1.alloc([4, NT], BF16)
        x1_mark = AX1.lo
        Qs = [AX1.alloc([NT], BF16) for _ in range(3)]
        K2T = AX1.alloc([1536], BF16)
        V2T = AX1.alloc([1536], BF16)
        V2 = AX1.alloc([12, 128], BF16)
        K1T = AX1.alloc([1152], BF16)
        V1T = AX1.alloc([1152], BF16)
        V1 = AX1.alloc([9, 128], BF16)
        V3 = AX1.alloc([32, 128], BF16)
        sq = [AX1.alloc([512], F32) for _ in range(2)]
        rk = [AX1.alloc([512], F32) for _ in range(2)]
        PTs = [AX1.alloc([256], BF16) for _ in range(4)]
        oacc = AX1.alloc([NT], F32)
        dacc = AX1.alloc([NT], F32)
        pti = {'i': 0}
        wad = [AX1.alloc([16, 128], BF16) for _ in range(2)]

        def kv_proj(cc, hT, hres, spans, dstK, dres, gcol, is_k):
            wsl, wr = load_w(ws, cc)
            for (t0, n, d0) in spans:
                pb, pn = bank()
                projT(wsl, wr, hT, hres, t0, n, pb, pn)
                if is_k:
                    qk_norm(pb, pn, n, gcol, dstK[:, d0:d0 + n], dres, (sq, rk), False)
                else:
                    S.op('act', lambda e: e.activation(out=dstK[:, d0:d0 + n], in_=pb[:, 0:n], func=AF.Copy),
                         reads=[pn], writes=[dres])

        def transpose_tiles(srcs, dst, dres, sres):
            for i0 in range(0, len(srcs), 8):
                pb, pn = bank()
                pv = pb[:, :].bitcast(BF16)
                grp = srcs[i0:i0 + 8]
                for k, (sap, n) in enumerate(grp):
                    S.op('pe', lambda e: e.transpose(out=pv[0:n, k * 128:(k + 1) * 128], in_=sap, identity=idb),
                         reads=[sres, 'idb'], writes=[pn])
                nmin = min(n for _, n in grp)
                S.op('dve', lambda e: e.tensor_copy(out=dst[0:nmin, i0:i0 + len(grp), :],
                                                    in_=pv[0:nmin, 0:len(grp) * 128].rearrange("p (k e) -> p k e", k=len(grp))),
                     reads=[pn], writes=[dres])

        def attend(pairs, qap, qn, ob, on, db, dn, c0, first_unused=None):
            pts = []
            for (kap, nk, vap, mask, kb) in pairs:
                pb, pn = bank()
                S.op('pe', lambda e: e.matmul(out=pb[0:nk, 0:qn], lhsT=kap, rhs=qap, start=True, stop=False),
                     reads=['KQ', 'K3T'], writes=[pn])
                S.op('pe', lambda e: e.matmul(out=pb[0:nk, 0:qn], lhsT=idb[0:nk, 0:nk], rhs=mask, start=False, stop=True),
                     reads=['idb', 'mcur', 'mprev'], writes=[pn])
                i = pti['i'] % 4
                pti['i'] += 1
                pt = PTs[i]
                if kb is not None:
                    S.op('act', lambda e: e.activation(out=pt[0:nk, 0:qn], in_=pb[0:nk, 0:qn], func=AF.Exp, bias=kb),
                         reads=[pn, 'fm'], writes=[f'pt{i}'])
                else:
                    S.op('act', lambda e: e.activation(out=pt[0:nk, 0:qn], in_=pb[0:nk, 0:qn], func=AF.Exp),
                         reads=[pn], writes=[f'pt{i}'])
                pts.append((pt, i, nk, vap))
            for j, (pt, i, nk, vap) in enumerate(pts):
                S.op('pe', lambda e: e.matmul(out=ob[:, c0:c0 + qn], lhsT=vap, rhs=pt[0:nk, 0:qn],
                                              start=(j == 0), stop=(j == len(pts) - 1)), reads=[f'pt{i}', 'Vt'], writes=[on])
            for j, (pt, i, nk, vap) in enumerate(pts):
                S.op('pe', lambda e: e.matmul(out=db[:, c0:c0 + qn], lhsT=ones_b[0:nk, :], rhs=pt[0:nk, 0:qn],
                                              start=(j == 0), stop=(j == len(pts) - 1)), reads=[f'pt{i}', 'ones_b'], writes=[dn])

        for j in range(4):
            h1, h2, h3 = j, 4 + j, 8 + j
            ada_deferred(wad, 16)
            for gi, hd in enumerate((h1, h2, h3)):
                wsl, wr = load_w(ws, CH_Q + hd)
                for half in range(2):
                    pb, pn = bank()
                    projT(wsl, wr, hTm, 'hTm', 512 + half * 512, 512, pb, pn)
                    qk_norm(pb, pn, 512, fmc("qg", hd), Qs[gi][:, half * 512:(half + 1) * 512], 'KQ', (sq, rk), True)
            sp1 = [(384, 512, 0), (896, 512, 512), (1408, 128, 1024)]
            sp2 = [(0, 512, 0), (512, 512, 512), (1024, 512, 1024)]
            sp3 = [(0, 512, 1536), (512, 512, 2048), (1024, 512, 2560)]
            kv_proj(CH_K + h1, hTm, 'hTm', sp1, K1T, 'KQ', fmc("kg", h1), True)
            kv_proj(CH_V + h1, hTm, 'hTm', sp1, V1T, 'VT', None, False)
            kv_proj(CH_K + h2, hTm, 'hTm', sp2, K2T, 'KQ', fmc("kg", h2), True)
            kv_proj(CH_V + h2, hTm, 'hTm', sp2, V2T, 'VT', None, False)
            kv_proj(CH_K + h3, hTm, 'hTm', sp3, K3T[:, j, :], 'K3T', fmc("kg", h3), True)
            kv_proj(CH_V + h3, hTm, 'hTm', sp3, V3T[:, j, :], 'V3T', None, False)
            flush_norm()
            transpose_tiles([(V1T[:, i * 128:(i + 1) * 128], 128) for i in range(9)], V1, 'Vt', 'VT')
            transpose_tiles([(V2T[:, 512 * kt + r:512 * kt + 512:4], 128) for r in range(4) for kt in range(3)], V2, 'Vt', 'VT')
            transpose_tiles([(V3T[:, j, r:2048:16], 128) for r in range(16)], V3[:, 0:16, :], 'Vt', 'V3T')
            transpose_tiles([(V3T[:, j, 2048 + r:LT:16], 64) for r in range(16)], V3[:, 16:32, :], 'Vt', 'V3T')
            bank_set['s'] = list(range(6))
            for hb in range(2):
                ob, on = pbs[6], 'pb6'
                db, dn = pbs[7], 'pb7'
                for q4 in range(4):
                    qb = hb * 4 + q4
                    pairs = [(K1T[:, 128 * (qb + 1):128 * (qb + 2)], 128, V1[:, qb + 1, :], mcur_b, fmc("kb1", qb + 1)),
                             (K1T[:, 128 * qb:128 * (qb + 1)], 128, V1[:, qb, :], mprev_b, fmc("kb1", qb))]
                    attend(pairs, Qs[0][:, 128 * qb:128 * (qb + 1)], 128, ob, on, db, dn, q4 * 128)
                S.op('act', lambda e: e.activation(out=oacc[:, hb * 512:(hb + 1) * 512], in_=ob[:, :], func=AF.Copy),
                     reads=[on], writes=['oacc'])
                S.op('dve', lambda e: e.tensor_copy(out=dacc[:, hb * 512:(hb + 1) * 512], in_=db[:, :]),
                     reads=[dn], writes=['dacc'])
            for qb in range(2):
                ob, on = pbs[6], 'pb6'
                db, dn = pbs[7], 'pb7'
                for r in range(4):
                    pairs = [(K2T[:, 512 * (qb + 1) + r:512 * (qb + 2):4], 128, V2[:, r * 3 + qb + 1, :], mcur_b, fmc("kb2", r * 3 + qb + 1)),
                             (K2T[:, 512 * qb + r:512 * (qb + 1):4], 128, V2[:, r * 3 + qb, :], mprev_b, fmc("kb2", r * 3 + qb))]
                    attend(pairs, Qs[1][:, 512 * qb + r:512 * (qb + 1):4], 128, ob, on, db, dn, r * 128)
                ov = oacc[:, 512 * qb:512 * (qb + 1)].rearrange("p (m r) -> p r m", r=4)
                dv = dacc[:, 512 * qb:512 * (qb + 1)].rearrange("p (m r) -> p r m", r=4)
                S.op('dve', lambda e: e.tensor_tensor(out=ov, in0=ob[:, :].rearrange("p (r m) -> p r m", r=4), in1=ov, op=ALU.add),
                     reads=[on, 'oacc'], writes=['oacc'])
                S.op('dve', lambda e: e.tensor_tensor(out=dv, in0=db[:, :].rearrange("p (r m) -> p r m", r=4), in1=dv, op=ALU.add),
                     reads=[dn, 'dacc'], writes=['dacc'])
            for hb in range(2):
                ob, on = pbs[6], 'pb6'
                db, dn = pbs[7], 'pb7'
                for r8 in range(8):
                    r = hb * 8 + r8
                    pairs = [(K3T[:, j, r:2048:16], 128, V3[:, r, :], mprev_b[:, 0:64], fmc("kb3", r)),
                             (K3T[:, j, 2048 + r:LT:16], 64, V3[0:64, 16 + r, :], mcur_b[0:64, 0:64], None)]
                    attend(pairs, Qs[2][:, r:NT:16], 64, ob, on, db, dn, r8 * 64)
                ov = oacc[:, :].rearrange("p (m r) -> p r m", r=16)[:, hb * 8:(hb + 1) * 8, :]
                dv = dacc[:, :].rearrange("p (m r) -> p r m", r=16)[:, hb * 8:(hb + 1) * 8, :]
                S.op('dve', lambda e: e.tensor_tensor(out=ov, in0=ob[:, :].rearrange("p (r m) -> p r m", r=8), in1=ov, op=ALU.add),
                     reads=[on, 'oacc'], writes=['oacc'])
                S.op('dve', lambda e: e.tensor_tensor(out=dv, in0=db[:, :].rearrange("p (r m) -> p r m", r=8), in1=dv, op=ALU.add),
                     reads=[dn, 'dacc'], writes=['dacc'])
            bank_set['s'] = list(range(8))
            S.op('dve', lambda e: e.reciprocal(out=dacc, in_=dacc), reads=['dacc'], writes=['dacc'])
            S.op('dve', lambda e: e.tensor_tensor(out=attnT[:, j, :], in0=oacc, in1=dacc, op=ALU.mult),
                 reads=['oacc', 'dacc'], writes=['attnT'])
            if stage == "M1":
                dump(Qs[0][:, 0:512], 512, 'KQ'); dump(K1T[:, 0:512], 512, 'KQ'); dump(V1[:, 1, :], 128, 'Vt')
                dump(attnT[:, 0, :], 1024, 'attnT'); dump(dacc, 1024, 'dacc'); dump(V3[:, 5, :], 128, 'Vt'); dump(V3[:, 21, :], 128, 'Vt')
                dump(V2[:, 4, :], 128, 'Vt')
                S.finish()
                return nc
        assert ada_state['col'] == 96 or stage == "M1"
        S.op('dve', lambda e: e.scalar_tensor_tensor(out=A2T, in0=modT2[:, 64:80], scalar=1.0, in1=fmc("g2"),
                                                     op0=ALU.add, op1=ALU.mult), reads=['modT2', 'fm'], writes=['A2T'])
        bcast_cols(modT2[:, 32:48], gate1B, 'gate1B')
        bcast_cols(modT2[:, 80:96], gate2B, 'gate2B')
        S.barrier()
        AX1.lo = x1_mark
        BM = CONST_W + X1_W

        FA = Arena(big, BM + 12288, BM + 24576)
        FB = Arena(big, BM + 27648, TOT)
        U = AX1.alloc([8, 1152], BF16)
        Vc = AX1.alloc([8, NT], F32)
        uact = FA.alloc([8, NT], BF16)
        sig = FA.alloc([512], F32)
        sqv = FA.alloc([NT], F32)
        mean = FA.alloc([NT], F32)
        rstdv = FA.alloc([NT], F32)
        t1 = FA.alloc([NT], F32)
        diags = [FB.alloc([128], BF16) for _ in range(4)]
        spU = [(384, 512, 0), (896, 512, 512), (1408, 128, 1024)]
        for cc in range(8):
            wa_, wra = load_w(ws, cc)
            wb_, wrb = load_w(ws, 8 + cc)
            for (t0, n, d0) in spU:
                pa, pna = bank()
                pg, png = bank()
                projT(wa_, wra, hTm, 'hTm', t0, n, pa, pna)
                projT(wb_, wrb, hTm, 'hTm', t0, n, pg, png)
                S.op('act', lambda e: e.activation(out=sig[:, 0:n], in_=pg[:, 0:n], func=AF.Sigmoid), reads=[png], writes=['sig'])
                S.op('dve', lambda e: e.tensor_tensor(out=U[:, cc, d0:d0 + n], in0=pa[:, 0:n], in1=sig[:, 0:n], op=ALU.mult),
                     reads=[pna, 'sig'], writes=['U'])
            S.op('dve', lambda e: e.tensor_scalar(out=U[:, cc, 0:128], in0=U[:, cc, 0:128], scalar1=fmc("hval", 0), scalar2=None,
                                                  op0=ALU.mult), reads=['U', 'fm'], writes=['U'])
        di = 0
        for cc in range(8):
            pbs2 = [bank(), bank()]
            for k in range(31):
                dg = diags[di % 4]
                dn_ = f'diag{di % 4}'
                di += 1
                S.op('dve', lambda e: e.tensor_scalar(out=dg, in0=ident_f, scalar1=fmc("dw", cc * 31 + k), scalar2=None, op0=ALU.mult),
                     reads=['cst', 'fm'], writes=[dn_])
                for half in range(2):
                    pb, pn = pbs2[half]
                    o0 = 128 - 30 + k + half * 512
                    S.op('pe', lambda e: e.matmul(out=pb[:, :], lhsT=dg, rhs=U[:, cc, o0:o0 + 512], start=(k == 0), stop=(k == 30)),
                         reads=[dn_, 'U'], writes=[pn])
            for half in range(2):
                pb, pn = pbs2[half]
                S.op('act', lambda e: e.activation(out=Vc[:, cc, half * 512:(half + 1) * 512], in_=pb[:, :], func=AF.Identity,
                                                   bias=fmc("db", cc)), reads=[pn, 'fm'], writes=['Vc'])
        for half in range(2):
            pm, pmn = bank()
            pq, pqn = bank()
            hs = slice(half * 512, (half + 1) * 512)
            for cc in range(8):
                S.op('pe', lambda e: e.matmul(out=pm[:, :], lhsT=ones_f, rhs=Vc[:, cc, hs], start=(cc == 0), stop=(cc == 7)),
                     reads=['ones_f', 'Vc'], writes=[pmn])
            for cc in range(8):
                S.op('act', lambda e: e.activation(out=sqv[:, 0:512], in_=Vc[:, cc, hs], func=AF.Square), reads=['Vc'], writes=['sqv'])
                S.op('pe', lambda e: e.matmul(out=pq[:, :], lhsT=ones_f, rhs=sqv[:, 0:512], start=(cc == 0), stop=(cc == 7)),
                     reads=['ones_f', 'sqv'], writes=[pqn])
            S.op('dve', lambda e: e.tensor_scalar(out=mean[:, hs], in0=pm[:, :], scalar1=1.0 / 1024, scalar2=None, op0=ALU.mult),
                 reads=[pmn], writes=['mean'])
            S.op('dve', lambda e: e.tensor_tensor(out=t1[:, hs], in0=mean[:, hs], in1=mean[:, hs], op=ALU.mult),
                 reads=['mean'], writes=['t1'])
            S.op('dve', lambda e: e.scalar_tensor_tensor(out=rstdv[:, hs], in0=pq[:, :], scalar=1.0 / 1024, in1=t1[:, hs],
                                                         op0=ALU.mult, op1=ALU.subtract), reads=[pqn, 't1'], writes=['rstdv'])
            S.op('act', lambda e: e.activation(out=rstdv[:, hs], in_=rstdv[:, hs], func=AF.Sqrt, scale=1.0, bias=EPS),
                 reads=['rstdv'], writes=['rstdv'])
            S.op('dve', lambda e: e.reciprocal(out=rstdv[:, hs], in_=rstdv[:, hs]), reads=['rstdv'], writes=['rstdv'])
        for cc in range(8):
            S.op('dve', lambda e: e.tensor_tensor(out=t1, in0=Vc[:, cc, :], in1=mean, op=ALU.subtract), reads=['Vc', 'mean'], writes=['t1'])
            S.op('dve', lambda e: e.tensor_tensor(out=t1, in0=t1, in1=rstdv, op=ALU.mult), reads=['t1', 'rstdv'], writes=['t1'])
            S.op('act', lambda e: e.activation(out=uact[:, cc, :], in_=t1, func=AF.Silu, scale=fmc("lng", cc), bias=fmc("lnb", cc)),
                 reads=['t1', 'fm'], writes=['uact'])
        S.barrier()
        AX1.lo = x1_mark

        mergedT = Arena(big, BM + 16384, BM + 24576).alloc([KC, NT], BF16)
        wco = [AX1.alloc([8, 128], BF16) for _ in range(2)]
        wao = [AX1.alloc([4, 128], BF16) for _ in range(2)]
        sA = [AX1.alloc([512], F32) for _ in range(2)]
        sB = [AX1.alloc([512], F32) for _ in range(2)]
        tA = [AX1.alloc([512], F32) for _ in range(2)]
        tB = [AX1.alloc([512], F32) for _ in range(2)]
        w_co_v = w_co.rearrange("(cc p) n -> p cc n", p=128)
        w_ao_v = w_ao.rearrange("(j p) n -> p j n", p=128)
        it = 0
        for dc in range(KC):
            s_ = dc % 2
            S.dma('pool', f'wco{s_}', out=wco[s_], in_=w_co_v[:, :, dc * 128:(dc + 1) * 128], writes=[f'wco{s_}'])
            S.dma('pool', f'wao{s_}', out=wao[s_], in_=w_ao_v[:, :, dc * 128:(dc + 1) * 128], writes=[f'wao{s_}'])
            wga, wgar = load_w(ws, CH_G + dc)
            wgb, wgbr = load_w(ws, CH_G + 16 + dc)
            for half in range(2):
                hs = slice(half * 512, (half + 1) * 512)
                b_ = it % 2
                it += 1
                pA, pAn = bank()
                pB, pBn = bank()
                pC, pCn = bank()
                pY, pYn = bank()
                projT(wga, wgar, hTm, 'hTm', 512 + half * 512, 512, pA, pAn)
                projT(wgb, wgbr, hTm, 'hTm', 512 + half * 512, 512, pB, pBn)
                for cc in range(8):
                    S.op('pe', lambda e: e.matmul(out=pC[:, :], lhsT=wco[s_][:, cc, :], rhs=uact[:, cc, hs], start=(cc == 0), stop=(cc == 7)),
                         reads=[f'wco{s_}', 'uact'], writes=[pCn])
                for jj in range(4):
                    S.op('pe', lambda e: e.matmul(out=pY[:, :], lhsT=wao[s_][:, jj, :], rhs=attnT[:, jj, hs], start=(jj == 0), stop=(jj == 3)),
                         reads=[f'wao{s_}', 'attnT'], writes=[pYn])
                S.op('act', lambda e: e.activation(out=sA[b_], in_=pA[:, :], func=AF.Sigmoid), reads=[pAn], writes=[f'sA{b_}'])
                S.op('act', lambda e: e.activation(out=sB[b_], in_=pB[:, :], func=AF.Sigmoid), reads=[pBn], writes=[f'sB{b_}'])
                S.op('dve', lambda e: e.scalar_tensor_tensor(out=tA[b_], in0=pC[:, :], scalar=fmc("bco", dc), in1=sA[b_],
                                                             op0=ALU.add, op1=ALU.mult), reads=[pCn, f'sA{b_}', 'fm'], writes=[f'tA{b_}'])
                S.op('dve', lambda e: e.tensor_tensor(out=tB[b_], in0=pY[:, :], in1=sB[b_], op=ALU.mult),
                     reads=[pYn, f'sB{b_}'], writes=[f'tB{b_}'])
                S.op('dve', lambda e: e.tensor_tensor(out=mergedT[:, dc, hs], in0=tA[b_], in1=tB[b_], op=ALU.add),
                     reads=[f'tA{b_}', f'tB{b_}'], writes=['mergedT'])
        S.barrier()
        AX1.reset()

        x1 = AX1.alloc([8, D], F32)
        AM4 = Arena(big, BM, BM + 16384)
        wos = [AM4.alloc([KC, 512], BF16) for _ in range(2)]
        xr = [AM4.alloc([512], F32) for _ in range(2)]
        tg = [AM4.alloc([512], F32) for _ in range(2)]
        w_out_v = w_out.rearrange("(kc p) n -> p kc n", p=128)
        it = 0
        for nb in range(4):
            ns = slice(nb * 512, (nb + 1) * 512)
            wo = wos[nb % 2]
            wn = f'wo{nb % 2}'
            S.dma('pool', wn, out=wo, in_=w_out_v[:, :, ns], writes=[wn])
            for tt in range(8):
                b_ = it % 2
                it += 1
                S.dma('sp', f'xr{b_}', out=xr[b_], in_=xh[2048 + tt * 128:2048 + (tt + 1) * 128, ns], writes=[f'xr{b_}'])
                pb, pn = bank()
                for kc in range(KC):
                    S.op('pe', lambda e: e.matmul(out=pb[:, :], lhsT=mergedT[:, kc, tt * 128:(tt + 1) * 128], rhs=wo[:, kc, :],
                                                  start=(kc == 0), stop=(kc == KC - 1)), reads=['mergedT', wn], writes=[pn])
                S.op('dve', lambda e: e.tensor_tensor(out=tg[b_], in0=pb[:, :], in1=gate1B[:, ns], op=ALU.mult),
                     reads=[pn, 'gate1B'], writes=[f'tg{b_}'])
                S.op('dve', lambda e: e.tensor_tensor(out=x1[:, tt, ns], in0=tg[b_], in1=xr[b_], op=ALU.add),
                     reads=[f'tg{b_}', f'xr{b_}'], writes=['x1'])
        S.barrier()

        if stage == "x1":
            for tt in range(8):
                S.dma('sp', 'out', out=y[tt * 128:(tt + 1) * 128, :], in_=x1[:, tt, :], reads=['x1'])
            S.finish()
            return nc

        BM = CONST_W + X1_W
        P = Arena(big, BM, TOT)
        h2T = P.alloc([KC, NT], BF16)
        p_mark = P.lo
        junk = [P.alloc([D], BF16)]
        xsb = [P.alloc([D], BF16) for _ in range(2)]
        for tt in range(8):
            norm_T(x1[:, tt, :], 'x1', h2T[:, :, tt * 128:(tt + 1) * 128], 'h2T', A2T, 'A2T', sh2T, junk, xsb, 'modT2')
        S.barrier()
        P.lo = p_mark
        e1T = P.alloc([NT], F32)
        e2T = P.alloc([NT], F32)
        gT = P.alloc([NT], F32)
        g_mark = P.lo
        wqs = [P.alloc([KC, 128], BF16) for _ in range(2)]
        qpT = P.alloc([16, 512], F32)
        s_sb = P.alloc([16, 128], F32)
        v1 = P.alloc([16, 16], F32)
        i1 = P.alloc([16, 16], U32)
        i1f = P.alloc([16, 16], F32)
        wks = [P.alloc([128], F32) for _ in range(4)]
        cand = P.alloc([8, 256], F32)
        wk2s = [P.alloc([256], F32) for _ in range(2)]
        top = P.alloc([8, 16], F32)
        ci = P.alloc([8, 16], U32)
        cu = P.alloc([8, 16], U32)
        af = P.alloc([8, 16], F32)
        bf_ = P.alloc([8, 16], F32)
        ex = P.alloc([8, 16], F32)
        zs = P.alloc([8], F32)
        gg = P.alloc([8, 16], F32)
        oh = cand
        e1f = P.alloc([8, 16], F32)
        e2f = P.alloc([8, 16], F32)
        w_q_v = w_q.rearrange("(kc p) n -> p kc n", p=128)
        iota16 = cst[:, 512:528]
        iota128 = cst[:, 384:512]
        wqi = 0
        for half in range(2):
            for cc in range(16):
                s_ = wqi % 2
                wqi += 1
                S.dma('pool', f'wq{s_}', out=wqs[s_], in_=w_q_v[:, :, cc * 128:(cc + 1) * 128], writes=[f'wq{s_}'])
                pb, pn = bank()
                projT(wqs[s_], f'wq{s_}', h2T, 'h2T', half * 512, 512, pb, pn)
                S.op('act', lambda e: e.activation(out=qpT[:, cc, :], in_=pb[:, :], func=AF.Copy), reads=[pn], writes=['qpT'])
            for t4 in range(4):
                tt = half * 4 + t4
                for b4 in range(4):
                    pb, pn = bank()
                    for k4 in range(4):
                        hp = b4 * 4 + k4
                        S.op('pe', lambda e: e.matmul(out=pb[:, k4 * 128:(k4 + 1) * 128], lhsT=qpT[:, hp, t4 * 128:(t4 + 1) * 128],
                                                      rhs=skT[:, hp, :], start=True, stop=True), reads=['qpT', 'skT'], writes=[pn])
                    S.op('act', lambda e: e.activation(out=s_sb[:, b4 * 4:(b4 + 1) * 4, :].rearrange("p a b -> p (a b)"), in_=pb[:, :], func=AF.Copy),
                         reads=[pn], writes=['s_sb'])
                NCH = 4
                for hp0 in range(0, 16, NCH):
                    hps = list(range(hp0, hp0 + NCH))
                    for hp in hps:
                        S.op('dve', lambda e: e.max(out=v1[:, hp, 0:8], in_=s_sb[:, hp, :]), reads=['s_sb'], writes=[f'v1a{hp}'])
                    for hp in hps:
                        S.op('dve', lambda e: e.max_index(out=i1[:, hp, 0:8], in_max=v1[:, hp, 0:8], in_values=s_sb[:, hp, :]),
                             reads=['s_sb', f'v1a{hp}'], writes=[f'i1a{hp}'])
                    for hp in hps:
                        S.op('dve', lambda e: e.match_replace(out=wks[hp % NCH], in_to_replace=v1[:, hp, 0:8], in_values=s_sb[:, hp, :], imm_value=-1e30),
                             reads=['s_sb', f'v1a{hp}'], writes=[f'wk{hp % NCH}'])
                    for hp in hps:
                        S.op('dve', lambda e: e.max(out=v1[:, hp, 8:16], in_=wks[hp % NCH]), reads=[f'wk{hp % NCH}'], writes=[f'v1b{hp}'])
                    for hp in hps:
                        S.op('dve', lambda e: e.max_index(out=i1[:, hp, 8:16], in_max=v1[:, hp, 8:16], in_values=wks[hp % NCH]),
                             reads=[f'wk{hp % NCH}', f'v1b{hp}'], writes=[f'i1b{hp}'])
                v1all = [f'v1a{hp}' for hp in range(16)] + [f'v1b{hp}' for hp in range(16)]
                i1all = [f'i1a{hp}' for hp in range(16)] + [f'i1b{hp}' for hp in range(16)]
                v1v = v1.rearrange("p (h q) a -> p h q a", q=2)
                i1fv = i1f.rearrange("p (h q) a -> p h q a", q=2)
                cand4 = cand.rearrange("p h (a b) -> p h a b", a=16)
                S.op('dve', lambda e: e.tensor_tensor(out=cand4, in0=bc(v1v[:, :, 0, :], 3, 16), in1=bc(v1v[:, :, 1, :], 2, 16), op=ALU.add),
                     reads=v1all, writes=['cand'])
                for h0 in range(0, 8, 2):
                    hs2 = (h0, h0 + 1)
                    for h in hs2:
                        S.op('dve', lambda e: e.max(out=top[:, h, 0:8], in_=cand[:, h, :]), reads=['cand'], writes=[f'topa{h}'])
                    for h in hs2:
                        S.op('dve', lambda e: e.max_index(out=ci[:, h, 0:8], in_max=top[:, h, 0:8], in_values=cand[:, h, :]),
                             reads=['cand', f'topa{h}'], writes=[f'cia{h}'])
                    for h in hs2:
                        S.op('dve', lambda e: e.match_replace(out=wk2s[h % 2], in_to_replace=top[:, h, 0:8], in_values=cand[:, h, :], imm_value=-1e30),
                             reads=['cand', f'topa{h}'], writes=[f'wk2{h % 2}'])
                    for h in hs2:
                        S.op('dve', lambda e: e.max(out=top[:, h, 8:16], in_=wk2s[h % 2]), reads=[f'wk2{h % 2}'], writes=[f'topb{h}'])
                    for h in hs2:
                        S.op('dve', lambda e: e.max_index(out=ci[:, h, 8:16], in_max=top[:, h, 8:16], in_values=wk2s[h % 2]),
                             reads=[f'wk2{h % 2}', f'topb{h}'], writes=[f'cib{h}'])
                topall = [f'topa{h}' for h in range(8)] + [f'topb{h}' for h in range(8)]
                ciall = [f'cia{h}' for h in range(8)] + [f'cib{h}' for h in range(8)]
                S.op('dve', lambda e: e.tensor_tensor(out=ex, in0=top, in1=bc(top[:, :, 0], 2, 16), op=ALU.subtract), reads=topall, writes=['ex'])
                S.op('act', lambda e: e.activation(out=ex, in_=ex, func=AF.Exp), reads=['ex'], writes=['ex'])
                S.op('dve', lambda e: e.tensor_reduce(out=zs, in_=ex, axis=AX.X, op=ALU.add), reads=['ex'], writes=['zs'])
                S.op('dve', lambda e: e.reciprocal(out=zs, in_=zs), reads=['zs'], writes=['zs'])
                S.op('dve', lambda e: e.tensor_tensor(out=gg, in0=ex, in1=bc(zs, 2, 16), op=ALU.mult), reads=['ex', 'zs'], writes=['gg'])
                S.op('dve', lambda e: e.tensor_copy(out=i1f, in_=i1), reads=i1all, writes=['i1f'])
                S.op('dve', lambda e: e.tensor_scalar(out=cu, in0=ci, scalar1=4, scalar2=None, op0=ALU.logical_shift_right), reads=ciall, writes=['cu'])
                S.op('dve', lambda e: e.tensor_copy(out=af, in_=cu), reads=['cu'], writes=['af'])
                S.op('dve', lambda e: e.tensor_scalar(out=cu, in0=ci, scalar1=15, scalar2=None, op0=ALU.bitwise_and), reads=ciall, writes=['cu'])
                S.op('dve', lambda e: e.tensor_copy(out=bf_, in_=cu), reads=['cu'], writes=['bf'])
                oh4 = oh.rearrange("p h (k a) -> p h k a", k=16)
                io4 = bc(bc(iota16, 1, 16), 1, 8)
                for (src, q_, dst, dn_) in ((af, 0, e1f, 'e1f'), (bf_, 1, e2f, 'e2f')):
                    S.op('dve', lambda e: e.tensor_tensor(out=oh4, in0=io4, in1=bc(src, 3, 16), op=ALU.is_equal), reads=['af', 'bf', 'cst'], writes=['cand'])
                    S.op('dve', lambda e: e.tensor_tensor(out=oh4, in0=oh4, in1=bc(i1fv[:, :, q_, :], 2, 16), op=ALU.mult), reads=['cand', 'i1f'], writes=['cand'])
                    S.op('dve', lambda e: e.tensor_reduce(out=dst, in_=oh4, axis=AX.X, op=ALU.add), reads=['cand'], writes=[dn_])
                pb, pn = bank()
                for k3, (src, sn) in enumerate(((e1f, 'e1f'), (e2f, 'e2f'), (gg, 'gg'))):
                    S.op('pe', lambda e: e.transpose(out=pb[:, k3 * 128:(k3 + 1) * 128], in_=src.rearrange("p h k -> p (h k)"), identity=ident_f),
                         reads=[sn, 'cst'], writes=[pn])
                for k3, (dst, dn_) in enumerate(((e1T, 'e1T'), (e2T, 'e2T'), (gT, 'gT'))):
                    S.op('act', lambda e: e.activation(out=dst[:, tt * 128:(tt + 1) * 128], in_=pb[:, k3 * 128:(k3 + 1) * 128], func=AF.Copy),
                         reads=[pn], writes=[dn_])
        S.barrier()
        if stage == "R":
            dump(e1T, 1024, 'e1T'); dump(e2T, 1024, 'e2T'); dump(gT, 1024, 'gT')
            S.finish()
            return nc
        P.lo = g_mark
        iob = P.alloc([128], BF16)
        e1b = P.alloc([NT], BF16)
        e2b = P.alloc([NT], BF16)
        gb16 = P.alloc([NT], BF16)
        S.op('dve', lambda e: e.tensor_copy(out=iob, in_=iota128), reads=['cst'], writes=['iob'])
        S.op('dve', lambda e: e.tensor_copy(out=e1b, in_=e1T), reads=['e1T'], writes=['e1b'])
        S.op('dve', lambda e: e.tensor_copy(out=e2b, in_=e2T), reads=['e2T'], writes=['e2b'])
        S.op('dve', lambda e: e.tensor_copy(out=gb16, in_=gT), reads=['gT'], writes=['gb16'])
        P1s = [P.alloc([16, 128], BF16) for _ in range(2)]
        P2s = [P.alloc([16, 128], BF16) for _ in range(2)]
        P2g = [P.alloc([16, 128], BF16) for _ in range(2)]
        Gs = P.alloc([128, 128], BF16)
        gscr_v = gscr.rearrange("j i t -> i j t")
        gi = 0
        ev_i = 0
        for tb in range(8):
            for g16 in range(8):
                t0 = tb * 128 + g16 * 16
                b_ = gi % 2
                gi += 1
                for tl in range(16):
                    S.op('dve', lambda e: e.tensor_scalar(out=P1s[b_][:, tl, :], in0=iob, scalar1=e1T[:, t0 + tl:t0 + tl + 1], scalar2=None,
                                                          op0=ALU.is_equal), reads=['iob', 'e1T'], writes=[f'P1{b_}'])
                    S.op('dve', lambda e: e.tensor_scalar(out=P2g[b_][:, tl, :], in0=iob, scalar1=e2T[:, t0 + tl:t0 + tl + 1],
                                                          scalar2=gT[:, t0 + tl:t0 + tl + 1], op0=ALU.is_equal, op1=ALU.mult),
                         reads=['iob', 'e2T', 'gT'], writes=[f'P2g{b_}'])
                for q4 in range(4):
                    pb, pn = bank()
                    for k4 in range(4):
                        tl = q4 * 4 + k4
                        S.op('pe', lambda e: e.matmul(out=pb[:, k4 * 128:(k4 + 1) * 128], lhsT=P1s[b_][:, tl, :], rhs=P2g[b_][:, tl, :],
                                                      start=True, stop=True), reads=[f'P1{b_}', f'P2g{b_}'], writes=[pn])
                    c0 = g16 * 16 + q4 * 4
                    src = pb[:, :].rearrange("p (t j) -> p j t", t=4)
                    S.op('act', lambda e: e.activation(out=Gs[:, :, c0:c0 + 4], in_=src, func=AF.Copy), reads=[pn], writes=['Gs'])
                    ev_i += 1
            for jq in range(4):
                S.dma('sp', 'gsw', out=gscr_v[:, jq * 32:(jq + 1) * 32, tb * 128:(tb + 1) * 128], in_=Gs[:, jq * 32:(jq + 1) * 32, :],
                      reads=['Gs'], writes=['gscr'])
        S.barrier()
        P.lo = p_mark
        JG = 4
        NG = 128 // JG
        Gt = [P.alloc([JG, NT], BF16) for _ in range(2)]
        NWU = 4
        wup = [P.alloc([KC, 128], BF16) for _ in range(NWU)]
        NWD = 7
        wdn = [P.alloc([D], BF16) for _ in range(NWD)]
        GaT = P.alloc([JG, NT], BF16)
        gel = [P.alloc([512], F32) for _ in range(2)]
        evb = [P.alloc([512], F32) for _ in range(4)]
        st = {'u': 0, 'd': 0}
        uslot = {}
        dslot = {}

        def load_G(g):
            S.dma('sp', f'Gt{g % 2}', out=Gt[g % 2], in_=gscr_v[:, g * JG:(g + 1) * JG, :], reads=['gscr'], writes=[f'Gt{g % 2}'])

        def load_up(j):
            us = st['u'] % NWU
            st['u'] += 1
            uslot[j] = us
            S.dma('pool', f'wup{us}', out=wup[us], in_=w_upP[j].rearrange("p (kc i) -> p kc i", kc=KC), writes=[f'wup{us}'])

        def load_dn(j):
            ds = st['d'] % NWD
            st['d'] += 1
            dslot[j] = ds
            S.dma('pool', f'wdn{ds}', out=wdn[ds].rearrange("p (a b) -> p a b", a=4), in_=w_dnP[j].rearrange("p (a b) -> p a b", a=4),
                  writes=[f'wdn{ds}'])

        load_G(0)
        for jj in range(JG):
            load_up(jj)
            load_dn(jj)
        gl = 0
        ei = 0
        for g in range(NG):
            b_ = g % 2
            j0 = g * JG
            for jj in range(JG):
                us = uslot[j0 + jj]
                for half in range(2):
                    hs = slice(half * 512, (half + 1) * 512)
                    pb, pn = bank()
                    projT(wup[us], f'wup{us}', h2T, 'h2T', half * 512, 512, pb, pn)
                    gb = gl % 2
                    gl += 1
                    S.op('act', lambda e: e.activation(out=gel[gb], in_=pb[:, :], func=AF.Gelu), reads=[pn], writes=[f'gel{gb}'])
                    S.op('dve', lambda e: e.tensor_tensor(out=GaT[:, jj, hs], in0=gel[gb], in1=Gt[b_][:, jj, hs], op=ALU.mult),
                         reads=[f'gel{gb}', f'Gt{b_}'], writes=['GaT'])
            if g + 1 < NG:
                load_G(g + 1)
                for jj in range(JG):
                    load_up(j0 + JG + jj)
                for jj in range(JG - 1):
                    load_dn(j0 + JG + jj)
            for tt in range(8):
                for dq in range(4):
                    ds_ = slice(dq * 512, (dq + 1) * 512)
                    pb, pn = bank()
                    for jj in range(JG):
                        dsl = dslot[j0 + jj]
                        S.op('pe', lambda e: e.matmul(out=pb[:, :], lhsT=GaT[:, jj, tt * 128:(tt + 1) * 128], rhs=wdn[dsl][:, ds_],
                                                      start=(jj == 0), stop=(jj == JG - 1)), reads=['GaT', f'wdn{dsl}'], writes=[pn])
                    eb = ei % 4
                    ei += 1
                    S.op('dve', lambda e: e.tensor_tensor(out=evb[eb], in0=pb[:, :], in1=gate2B[:, ds_], op=ALU.mult),
                         reads=[pn, 'gate2B'], writes=[f'ev{eb}'])
                    xres = f'x1_{tt}_{dq}'
                    if eb % 2 == 0:
                        S.op('pool', lambda e: e.tensor_tensor(out=x1[:, tt, ds_], in0=x1[:, tt, ds_], in1=evb[eb], op=ALU.add),
                             reads=[f'ev{eb}', xres, 'x1'], writes=[xres])
                    else:
                        S.op('dve', lambda e: e.tensor_tensor(out=x1[:, tt, ds_], in0=x1[:, tt, ds_], in1=evb[eb], op=ALU.add),
                             reads=[f'ev{eb}', xres, 'x1'], writes=[xres])
            if g + 1 < NG:
                load_dn(j0 + JG + JG - 1)
        for tt in range(8):
            S.dma('sp', 'out', out=y[tt * 128:(tt + 1) * 128, :], in_=x1[:, tt, :], reads=['x1'] + [f'x1_{tt}_{dq}' for dq in range(4)])
        S.finish()
    return nc


_CACHE = {}


def _host_consts():
    cst = np.zeros((128, NCS), np.float32)
    cst[:, 0:128] = np.eye(128, dtype=np.float32)
    mk = np.arange(128)[:, None]
    mq = np.arange(128)[None, :]
    cst[:, 128:256] = np.where(mq >= mk, 0.0, NEG)
    cst[:, 256:384] = np.where(mq <= mk, 0.0, NEG)
    cst[:, 384:512] = np.arange(128, dtype=np.float32)[None, :]
    cst[:, 512:528] = np.arange(16, dtype=np.float32)[None, :]
    return cst


def _fmT(v, n):
    return np.ascontiguousarray(np.asarray(v, np.float32).reshape(n, 128).T)


def kernel(**inputs):
    f = lambda k: np.asarray(inputs[k], np.float32)
    x, c = f("x"), f("c")
    if "nc" not in _CACHE:
        _CACHE["nc"] = build_program()
    nc = _CACHE["nc"]
    cst = _host_consts()
    sk = f("peer_sub_keys")[0]
    skT = np.ascontiguousarray(sk.reshape(16, 128, 128).transpose(2, 0, 1).reshape(128, 2048))
    w_up = f("peer_w_up")[0]
    w_dn = f("peer_w_down")[0]
    w_upP = np.ascontiguousarray(w_up.reshape(128, 128, KC, 128).transpose(1, 3, 2, 0).reshape(128, 128, KC * 128))
    w_dnP = np.ascontiguousarray(w_dn.reshape(128, 128, D).transpose(1, 0, 2))
    shared = {
        "cst": cst, "skT": skT, "w_ada": f("w_ada")[0], "w_in": f("w_in")[0], "w_co": f("w_conv_out")[0],
        "w_ao": f("w_attn_o")[0], "w_out": f("w_out")[0], "w_q": f("peer_w_q")[0], "w_upP": w_upP, "w_dnP": w_dnP,
    }
    fm_shared = np.zeros((128, NFM), np.float32)

    def put(a, name, arr):
        o0, o1 = FM[name]
        a[:, o0:o1] = arr
    put(fm_shared, "bada", _fmT(f("b_ada")[0], 96))
    put(fm_shared, "g1", _fmT(f("norm1_g")[0], 16))
    put(fm_shared, "g2", _fmT(f("norm2_g")[0], 16))
    dw = f("conv_dw")[0]
    put(fm_shared, "dw", np.ascontiguousarray(dw.reshape(31, 8, 128).transpose(2, 1, 0).reshape(128, 248)))
    put(fm_shared, "db", _fmT(f("conv_db")[0], 8))
    put(fm_shared, "lng", _fmT(f("conv_ln_g")[0], 8))
    put(fm_shared, "lnb", _fmT(f("conv_ln_b")[0], 8))
    put(fm_shared, "bco", _fmT(f("b_conv_out")[0], 16))
    put(fm_shared, "qg", np.ascontiguousarray(f("q_norm_g")[0].T))
    put(fm_shared, "kg", np.ascontiguousarray(f("k_norm_g")[0].T))
    in_maps = []
    for core in range(8):
        b, q = core // 4, core % 4
        lo = 1024 * q - 2048
        xhh = np.zeros((LT, D), np.float32)
        s0 = max(lo, 0)
        xhh[s0 - lo:] = x[b, s0:1024 * q + 1024]
        valid = (lo + np.arange(LT)) >= 0
        kbias = np.where(valid, 0.0, NEG).astype(np.float32)
        fmc_ = fm_shared.copy()
        put(fmc_, "cT", _fmT(c[b], 16))
        put(fmc_, "hval", np.full((128, 1), 1.0 if q > 0 else 0.0, np.float32))
        p = np.arange(128)
        put(fmc_, "kb1", np.stack([kbias[1920 + 128 * i + p] for i in range(9)], axis=1))
        put(fmc_, "kb2", np.stack([kbias[1536 + 4 * (128 * kt + p) + r] for r in range(4) for kt in range(3)], axis=1))
        put(fmc_, "kb3", np.stack([kbias[16 * p + r] for r in range(16)], axis=1))
        m = dict(shared)
        m["xh"] = xhh
        m["fm"] = fmc_
        in_maps.append(m)
    res = run_bass_kernel_spmd(nc, in_maps, core_ids=list(range(8)))
    out = np.zeros((2, 4096, D), np.float32)
    for core in range(8):
        b, q = core // 4, core % 4
        out[b, 1024 * q:1024 * q + 1024] = res.results[core]["y"]
    return out
```

```python
import contextlib
import numpy as np
import concourse.bass as bass
import concourse.mybir as mybir
from concourse.bass_utils import run_bass_kernel_spmd

F32 = mybir.dt.float32
BF16 = mybir.dt.bfloat16
U32 = mybir.dt.uint32
AF = mybir.ActivationFunctionType
ALU = mybir.AluOpType
AX = mybir.AxisListType

D = 2048
KC = 16
NT = 1024
LT = 3072
EPS = 1e-6
NEG = -30000.0
IN_COLS = 10752
CH_Q, CH_K, CH_V, CH_G = 16, 28, 40, 52

FM = {}
_o = 0
for _n, _w in [("cT", 16), ("bada", 96), ("g1", 16), ("g2", 16), ("dw", 248), ("db", 8), ("lng", 8), ("lnb", 8),
               ("bco", 16), ("qg", 12), ("kg", 12), ("hval", 1), ("kb1", 9), ("kb2", 12), ("kb3", 16)]:
    FM[_n] = (_o, _o + _w)
    _o += _w
NFM = _o
CS = {"ident": (0, 128), "mcur": (128, 256), "mprev": (256, 384), "iota": (384, 512), "iota16": (512, 528)}
NCS = 528

NO_SELF_SYNC = ("pe",)
STAGE = "full"


class Sched:
    def __init__(self, nc, es):
        self.nc = nc
        self.es = es
        self.eng = {'pe': nc.tensor, 'act': nc.scalar, 'dve': nc.vector, 'pool': nc.gpsimd, 'sp': nc.sync}
        self.sem = {k: es.enter_context(nc.semaphore('sem_' + k)) for k in self.eng}
        self.cnt = {k: 0 for k in self.eng}
        self.seen = {k: {} for k in self.eng}
        self.last_w = {}
        self.readers = {}
        self.dsem = {}
        self.dcnt = {}
        self.bank_i = 0

    def _semof(self, key):
        return self.sem[key] if key in self.sem else self.dsem[key]

    def _deps(self, reads, writes):
        deps = {}

        def add(k, c):
            if deps.get(k, 0) < c:
                deps[k] = c
        for r in reads:
            ev = self.last_w.get(r)
            if ev is not None:
                add(*ev)
        for w in writes:
            ev = self.last_w.get(w)
            if ev is not None:
                add(*ev)
            for k, c in self.readers.get(w, {}).items():
                add(k, c)
        return deps

    def _wait(self, e, deps):
        for k, c in deps.items():
            if k == e and e in NO_SELF_SYNC:
                continue
            if self.seen[e].get(k, 0) >= c:
                continue
            self.eng[e].wait_ge(self._semof(k), c)
            self.seen[e][k] = c

    def _record(self, ev, reads, writes):
        k, c = ev
        for r in reads:
            self.readers.setdefault(r, {})[k] = c
        for w in writes:
            self.last_w[w] = ev
            self.readers[w] = {}

    def op(self, e, fn, reads=(), writes=()):
        self._wait(e, self._deps(reads, writes))
        ins = fn(self.eng[e])
        self.cnt[e] += 1
        ins.then_inc(self.sem[e], 1)
        self._record((e, self.cnt[e]), reads, writes)
        return ins

    def dma(self, q, semname, reads=(), writes=(), out=None, in_=None, fn=None, **kw):
        if semname not in self.dsem:
            self.dsem[semname] = self.es.enter_context(self.nc.semaphore('d_' + semname))
            self.dcnt[semname] = 0
        self._wait(q, self._deps(reads, writes))
        if fn is not None:
            ins = fn(self.eng[q])
        else:
            ins = self.eng[q].dma_start(out=out, in_=in_, **kw)
        self.dcnt[semname] += 16
        ins.then_inc(self.dsem[semname], 16)
        self._record((semname, self.dcnt[semname]), reads, writes)
        return ins

    def barrier(self):
        evs = {k: c for k, c in self.cnt.items() if c > 0}
        evs.update({k: c for k, c in self.dcnt.items() if c > 0})
        for e in self.eng:
            self._wait(e, dict(evs))

    def finish(self, q='sp'):
        evs = {k: c for k, c in self.dcnt.items() if c > 0}
        evs.update({k: c for k, c in self.cnt.items() if c > 0 and k != q})
        self._wait(q, evs)


class Arena:
    def __init__(self, base_ap, lo, hi):
        self.base = base_ap
        self.lo0, self.hi0 = lo, hi
        self.lo, self.hi = lo, hi

    def reset(self):
        self.lo, self.hi = self.lo0, self.hi0

    def alloc(self, shape, dtype, top=False):
        n = int(np.prod(shape))
        isz = 4 if dtype in (F32, U32) else 2
        words = (n * isz + 3) // 4
        if top:
            self.hi -= words
            off = self.hi
        else:
            off = self.lo
            self.lo += words
        assert self.lo <= self.hi, ("arena overflow", self.lo, self.hi)
        v = self.base[:, off:off + words]
        if dtype != F32:
            v = v.bitcast(dtype)
        v = v[:, 0:n]
        if len(shape) == 2:
            v = v.rearrange("p (a b) -> p a b", a=shape[0], b=shape[1])
        elif len(shape) == 3:
            v = v.rearrange("p (a b c) -> p a b c", a=shape[0], b=shape[1], c=shape[2])
        return v


def bc(ap2, reps_axis, n):
    pat = [list(x) for x in ap2.ap]
    pat.insert(reps_axis, [0, n])
    return bass.AP(tensor=ap2.tensor, offset=ap2.offset, ap=pat)


def build_program(stage=None):
    stage = stage or STAGE
    nc = bass.Bass("TRN2", target_bir_lowering=False)
    early = stage in ("A", "H", "M1")
    def dram(n, s, d=F32, kind="ExternalInput"):
        if early and n in ("w_co", "w_ao", "w_out", "w_q", "w_upP", "w_dnP") or (stage == "A" and n in ("w_in", "xh")):
            return None
        if (stage == "x1" and n in ("w_q", "w_upP", "w_dnP")) or (stage == "R" and n in ("w_upP", "w_dnP")):
            return None
        return nc.dram_tensor(n, s, d, kind=kind).ap()
    dbg = nc.dram_tensor("dbg", [128, 8192], F32, kind="ExternalOutput").ap() if stage != "full" else None
    xh = dram("xh", [LT, D])
    fm_d = dram("fm", [128, NFM])
    cst_d = dram("cst", [128, NCS])
    skT_d = dram("skT", [128, 2048])
    w_ada = dram("w_ada", [D, 6 * D])
    w_in = dram("w_in", [D, IN_COLS])
    w_co = dram("w_co", [1024, D])
    w_ao = dram("w_ao", [512, D])
    w_out = dram("w_out", [D, D])
    w_q = dram("w_q", [D, D])
    w_upP = dram("w_upP", [128, 128, KC * 128])
    w_dnP = dram("w_dnP", [128, 128, D])
    y = dram("y", [NT, D], F32, "ExternalOutput")
    gscr = nc.dram_tensor("gscr", [128, 128, NT], BF16, kind="Internal").ap()

    with contextlib.ExitStack() as es:
        S = Sched(nc, es)
        TOT = 53100
        big = es.enter_context(nc.sbuf_tensor("arena", [128, TOT], F32))
        pbs = [es.enter_context(nc.psum_tensor(f"pb{i}", [128, 512], F32)) for i in range(8)]
        CONST_W = 7900
        X1_W = 16384
        AC = Arena(big, 0, CONST_W)
        AX1 = Arena(big, CONST_W, CONST_W + X1_W)
        AM = Arena(big, CONST_W + X1_W, TOT)

        dbgc = {'c': 0}

        def dump(ap, n, res):
            c0 = dbgc['c']
            dbgc['c'] += n
            S.dma('pool', 'dbg', out=dbg[:, c0:c0 + n], in_=ap, reads=[res])
            return c0

        bank_set = {'s': list(range(8))}

        def bank():
            bs = bank_set['s']
            i = bs[S.bank_i % len(bs)]
            S.bank_i += 1
            return pbs[i], f"pb{i}"

        fm = AC.alloc([NFM], F32)
        cst = AC.alloc([NCS], F32)
        skT = AC.alloc([16, 128], F32)
        idb = AC.alloc([128], BF16)
        mcur_b = AC.alloc([128], BF16)
        mprev_b = AC.alloc([128], BF16)
        ones_f = AC.alloc([128], F32)
        ones_b = AC.alloc([128], BF16)
        sc = AC.alloc([16], F32)
        scb = AC.alloc([16], BF16)
        modT = AC.alloc([96], F32)
        modT2 = modT
        A1T = AC.alloc([16], F32)
        A2T = AC.alloc([16], F32)
        gate1B = AC.alloc([D], F32)
        gate2B = AC.alloc([D], F32)
        nsc = [(AC.alloc([1], F32), AC.alloc([1], F32), AC.alloc([1], F32)) for _ in range(2)]
        nstate = {'i': 0, 'q': 0}
        epsc = AC.alloc([2], F32)
        eps1 = epsc[:, 0:1]
        eps128 = epsc[:, 1:2]
        diagf = AC.alloc([128], F32)

        def fmc(name, a=None, b=None):
            o0, o1 = FM[name]
            if a is None:
                return fm[:, o0:o1]
            return fm[:, o0 + a:o0 + (b if b is not None else a + 1)]
        ident_f = cst[:, 0:128]

        S.dma('sp', 'c0', out=fm, in_=fm_d[:, :], writes=['fm'])
        S.dma('sp', 'c1', out=cst, in_=cst_d[:, :], writes=['cst'])
        S.dma('sp', 'c2', out=skT.rearrange("p a b -> p (a b)"), in_=skT_d[:, :], writes=['skT'])
        S.op('dve', lambda e: e.tensor_copy(out=idb, in_=ident_f), reads=['cst'], writes=['idb'])
        S.op('dve', lambda e: e.tensor_copy(out=mcur_b, in_=cst[:, 128:256]), reads=['cst'], writes=['mcur'])
        S.op('dve', lambda e: e.tensor_copy(out=mprev_b, in_=cst[:, 256:384]), reads=['cst'], writes=['mprev'])
        S.op('dve', lambda e: e.memset(ones_f, 1.0), writes=['ones_f'])
        S.op('dve', lambda e: e.memset(ones_b, 1.0), writes=['ones_b'])
        S.op('dve', lambda e: e.memset(eps1, EPS), writes=['epsc'])
        S.op('dve', lambda e: e.memset(eps128, 128.0 * EPS), writes=['epsc'])

        S.op('act', lambda e: e.activation(out=sc, in_=fmc("cT"), func=AF.Silu), reads=['fm'], writes=['sc'])
        S.op('dve', lambda e: e.tensor_copy(out=scb, in_=sc), reads=['sc'], writes=['scb'])
        wa = [AX1.alloc([16, 512], BF16) for _ in range(3)]
        w_ada_v = w_ada.rearrange("(kc p) n -> p kc n", p=128)
        for nb in range(8):
            s_ = nb % 3
            S.dma('pool', f'wa{s_}', out=wa[s_], in_=w_ada_v[:, :, nb * 512:(nb + 1) * 512], writes=[f'wa{s_}'])
            pb, pn = bank()
            for sub in range(4):
                for kc in range(KC):
                    S.op('pe', lambda e: e.matmul(out=pb[:, sub:sub + 1], lhsT=wa[s_][:, kc, sub * 128:(sub + 1) * 128],
                                                  rhs=scb[:, kc:kc + 1], start=(kc == 0), stop=(kc == KC - 1)),
                         reads=[f'wa{s_}', 'scb'], writes=[pn])
            S.op('dve', lambda e: e.tensor_tensor(out=modT[:, nb * 4:nb * 4 + 4], in0=pb[:, 0:4],
                                                  in1=fmc("bada", nb * 4, nb * 4 + 4), op=ALU.add),
                 reads=[pn, 'fm'], writes=['modT'])
        ada_state = {'col': 32, 'i': 0, 'pend': None}

        def ada_prefetch(wad):
            col = ada_state['col']
            if col >= 96 or ada_state['pend'] is not None:
                return
            ada_state['col'] += 1
            s_ = ada_state['i'] % len(wad)
            ada_state['i'] += 1
            S.dma('pool', f'wad{s_}', out=wad[s_], in_=w_ada_v[:, :, col * 128:(col + 1) * 128], writes=[f'wad{s_}'])
            ada_state['pend'] = (col, s_)

        def ada_deferred(wad, nblk, prefetch_next=True):
            flush_norm()
            for _ in range(nblk):
                if ada_state['pend'] is None:
                    ada_prefetch(wad)
                if ada_state['pend'] is None:
                    return
                col, s_ = ada_state['pend']
                ada_state['pend'] = None
                pb, pn = bank()
                for kc in range(KC):
                    S.op('pe', lambda e: e.matmul(out=pb[:, 0:1], lhsT=wad[s_][:, kc, :], rhs=scb[:, kc:kc + 1],
                                                  start=(kc == 0), stop=(kc == KC - 1)), reads=[f'wad{s_}', 'scb'], writes=[pn])
                S.op('dve', lambda e: e.tensor_tensor(out=modT2[:, col:col + 1], in0=pb[:, 0:1], in1=fmc("bada", col), op=ALU.add),
                     reads=[pn, 'fm'], writes=['modT2'])
                if prefetch_next:
                    ada_prefetch(wad)
        S.op('dve', lambda e: e.scalar_tensor_tensor(out=A1T, in0=modT[:, 16:32], scalar=1.0, in1=fmc("g1"),
                                                     op0=ALU.add, op1=ALU.mult), reads=['modT', 'fm'], writes=['A1T'])
        sh1T = modT[:, 0:16]
        sh2T = modT[:, 48:64]

        def bcast_cols(colsT, dst, dname):
            for g4 in range(4):
                pb, pn = bank()
                for k4 in range(4):
                    kc = g4 * 4 + k4
                    S.op('dve', lambda e: e.tensor_scalar(out=diagf, in0=ident_f, scalar1=colsT[:, kc:kc + 1], scalar2=None,
                                                          op0=ALU.mult), reads=['cst', 'modT2'], writes=['diagf'])
                    S.op('pe', lambda e: e.matmul(out=pb[:, k4 * 128:(k4 + 1) * 128], lhsT=ones_f, rhs=diagf,
                                                  start=True, stop=True), reads=['ones_f', 'diagf'], writes=[pn])
                S.op('act', lambda e: e.activation(out=dst[:, g4 * 512:(g4 + 1) * 512], in_=pb[:, :], func=AF.Copy),
                     reads=[pn], writes=[dname])
        S.barrier()
        AX1.reset()
        if stage == "A":
            dump(modT, 96, 'modT'); dump(A1T, 16, 'A1T')
            S.finish()
            return nc

        def norm_T(src, src_res, dst3, dst_res, AT, ares, shT, junks, xss, shres='modT'):
            ni = nstate['i'] % 2
            nstate['i'] += 1
            ss, rs, rstd = nsc[ni]
            junk = junks[ni % len(junks)]
            xs = xss[ni % len(xss)]
            jn, xn_, sn, rn, rdn = f'junk{ni}', f'xs{ni}', f'ss{ni}', f'rs{ni}', f'rstd{ni}'
            S.op('act', lambda e: e.activation(out=junk, in_=src, func=AF.Square, accum_out=ss),
                 reads=[src_res], writes=[jn, sn])
            S.op('act', lambda e: e.activation(out=rs, in_=ss, func=AF.Sqrt, scale=1.0 / D, bias=EPS),
                 reads=[sn], writes=[rn])
            S.op('dve', lambda e: e.reciprocal(out=rstd, in_=rs), reads=[rn], writes=[rdn])
            S.op('act', lambda e: e.activation(out=xs, in_=src, func=AF.Identity, scale=rstd),
                 reads=[src_res, rdn], writes=[xn_])
            for half in range(2):
                pb, pn = bank()
                pv = pb[:, :].bitcast(BF16)
                for k in range(8):
                    kc = half * 8 + k
                    S.op('pe', lambda e: e.transpose(out=pv[:, k * 128:(k + 1) * 128], in_=xs[:, kc * 128:(kc + 1) * 128],
                                                     identity=idb), reads=[xn_, 'idb'], writes=[pn])
                for k in range(8):
                    kc = half * 8 + k
                    if k % 2 == 0:
                        S.op('dve', lambda e: e.tensor_scalar(out=dst3[:, kc, :], in0=pv[:, k * 128:(k + 1) * 128],
                                                              scalar1=AT[:, kc:kc + 1], scalar2=shT[:, kc:kc + 1],
                                                              op0=ALU.mult, op1=ALU.add),
                             reads=[pn, ares, shres], writes=[dst_res])
                    else:
                        S.op('act', lambda e: e.activation(out=dst3[:, kc, :], in_=pv[:, k * 128:(k + 1) * 128],
                                                           func=AF.Identity, scale=AT[:, kc:kc + 1], bias=shT[:, kc:kc + 1]),
                             reads=[pn, ares, shres], writes=[dst_res])

        w_in_v = w_in.rearrange("(kc p) n -> p kc n", p=128)
        wstate = {'i': 0}

        def load_w(ws, cc):
            s_ = wstate['i'] % len(ws)
            wstate['i'] += 1
            S.dma('pool', f'ws{s_}', out=ws[s_], in_=w_in_v[:, :, cc * 128:(cc + 1) * 128], writes=[f'ws{s_}'])
            return ws[s_], f'ws{s_}'

        def projT(wslot, wres, hT, hres, t0, n, pb, pn, c0=0):
            for kc in range(KC):
                S.op('pe', lambda e: e.matmul(out=pb[:, c0:c0 + n], lhsT=wslot[:, kc, :], rhs=hT[:, kc, t0:t0 + n],
                                              start=(kc == 0), stop=(kc == KC - 1)), reads=[wres, hres], writes=[pn])

        pend = {'f': None}

        def flush_norm():
            if pend['f'] is not None:
                f_ = pend['f']
                pend['f'] = None
                f_()

        def qk_norm(pb, pn, n, gcol, dst, dres, tmp, is_q):
            sqs, rks = tmp
            i = nstate['q'] % 2
            nstate['q'] += 1
            sq, rk = sqs[i], rks[i]
            flush_norm()
            S.op('act', lambda e: e.activation(out=sq[:, 0:n], in_=pb[:, 0:n], func=AF.Square), reads=[pn], writes=[f'sq{i}'])

            def rest():
                pb2, pn2 = bank()
                S.op('pe', lambda e: e.matmul(out=pb2[:, 0:n], lhsT=ones_f, rhs=sq[:, 0:n], start=True, stop=True),
                     reads=['ones_f', f'sq{i}'], writes=[pn2])
                if is_q:
                    S.op('act', lambda e: e.activation(out=rk[:, 0:n], in_=pb2[:, 0:n], func=AF.Sqrt, scale=1.0, bias=128.0 * EPS),
                         reads=[pn2], writes=[f'rk{i}'])
                else:
                    S.op('act', lambda e: e.activation(out=rk[:, 0:n], in_=pb2[:, 0:n], func=AF.Sqrt, scale=1.0 / 128, bias=EPS),
                         reads=[pn2], writes=[f'rk{i}'])
                S.op('dve', lambda e: e.reciprocal(out=rk[:, 0:n], in_=rk[:, 0:n]), reads=[f'rk{i}'], writes=[f'rk{i}'])
                S.op('dve', lambda e: e.scalar_tensor_tensor(out=dst, in0=pb[:, 0:n], scalar=gcol, in1=rk[:, 0:n],
                                                             op0=ALU.mult, op1=ALU.mult), reads=[pn, f'rk{i}', 'fm'], writes=[dres])
            pend['f'] = rest

        hTm = AM.alloc([KC, 1536], BF16)
        K3T = AM.alloc([4, LT], BF16)
        V3T = AM.alloc([4, LT], BF16)
        ws = [AM.alloc([KC, 128], BF16) for _ in range(3)]
        am_mark = (AM.lo, AM.hi)

        xin = [AX1.alloc([D], F32) for _ in range(3)]
        junk = [AX1.alloc([D], BF16)]
        xsb = [AX1.alloc([D], BF16) for _ in range(2)]
        hTh = AX1.alloc([KC, 512], BF16)
        sq = [AX1.alloc([512], F32) for _ in range(2)]
        rk = [AX1.alloc([512], F32) for _ in range(2)]
        xi = 0
        for tile in range(24):
            s_ = xi % 3
            xi += 1
            S.dma('sp', f'xin{s_}', out=xin[s_], in_=xh[tile * 128:(tile + 1) * 128, :], writes=[f'xin{s_}'])
            if tile < 12:
                blk, tt = tile // 4, tile % 4
                norm_T(xin[s_], f'xin{s_}', hTh[:, :, tt * 128:(tt + 1) * 128], 'hTh', A1T, 'A1T', sh1T, junk, xsb)
                if tt == 3:
                    for hd in range(4):
                        for kind in range(2):
                            cc = (CH_K if kind == 0 else CH_V) + 8 + hd
                            wsl, wr = load_w(ws, cc)
                            pb, pn = bank()
                            projT(wsl, wr, hTh, 'hTh', 0, 512, pb, pn)
                            if kind == 0:
                                qk_norm(pb, pn, 512, fmc("kg", 8 + hd), K3T[:, hd, blk * 512:(blk + 1) * 512], 'K3T', (sq, rk), False)
                            else:
                                S.op('act', lambda e: e.activation(out=V3T[:, hd, blk * 512:(blk + 1) * 512], in_=pb[:, :], func=AF.Copy),
                                     reads=[pn], writes=['V3T'])
                    flush_norm()
            else:
                flush_norm()
                m0 = (tile - 12) * 128
                norm_T(xin[s_], f'xin{s_}', hTm[:, :, m0:m0 + 128], 'hTm', A1T, 'A1T', sh1T, junk, xsb)
        S.barrier()
        if stage == "H":
            dump(hTm[:, 0, 0:512], 512, 'hTm'); dump(hTm[:, 5, 512:1024], 512, 'hTm'); dump(K3T[:, 1, 0:512], 512, 'K3T')
            dump(V3T[:, 2, 512:1024], 512, 'V3T'); dump(hTh[:, 3, :], 512, 'hTh')
            S.finish()
            return nc
        AX1.reset()

        attnT = AX1.alloc([4, NT], BF16)
        x1_mark = AX1.lo
        Qs = [AX1.alloc([NT], BF16) for _ in range(3)]
        K2T = AX1.alloc([1536], BF16)
        V2T = AX1.alloc([1536], BF16)
        V2 = AX1.alloc([12, 128], BF16)
        K1T = AX1.alloc([1152], BF16)
        V1T = AX1.alloc([1152], BF16)
        V1 = AX1.alloc([9, 128], BF16)
        V3 = AX1.alloc([32, 128], BF16)
        sq = [AX1.alloc([512], F32) for _ in range(2)]
        rk = [AX1.alloc([512], F32) for _ in range(2)]
        PTs = [AX1.alloc([256], BF16) for _ in range(4)]
        oacc = AX1.alloc([NT], F32)
        dacc = AX1.alloc([NT], F32)
        pti = {'i': 0}
        wad = [AX1.alloc([16, 128], BF16) for _ in range(2)]

        def kv_proj(cc, hT, hres, spans, dstK, dres, gcol, is_k):
            wsl, wr = load_w(ws, cc)
            for (t0, n, d0) in spans:
                pb, pn = bank()
                projT(wsl, wr, hT, hres, t0, n, pb, pn)
                if is_k:
                    qk_norm(pb, pn, n, gcol, dstK[:, d0:d0 + n], dres, (sq, rk), False)
                else:
                    S.op('act', lambda e: e.activation(out=dstK[:, d0:d0 + n], in_=pb[:, 0:n], func=AF.Copy),
                         reads=[pn], writes=[dres])

        def transpose_tiles(srcs, dst, dres, sres):
            for i0 in range(0, len(srcs), 8):
                pb, pn = bank()
                pv = pb[:, :].bitcast(BF16)
                grp = srcs[i0:i0 + 8]
                for k, (sap, n) in enumerate(grp):
                    S.op('pe', lambda e: e.transpose(out=pv[0:n, k * 128:(k + 1) * 128], in_=sap, identity=idb),
                         reads=[sres, 'idb'], writes=[pn])
                nmin = min(n for _, n in grp)
                S.op('dve', lambda e: e.tensor_copy(out=dst[0:nmin, i0:i0 + len(grp), :],
                                                    in_=pv[0:nmin, 0:len(grp) * 128].rearrange("p (k e) -> p k e", k=len(grp))),
                     reads=[pn], writes=[dres])

        def attend(pairs, qap, qn, ob, on, db, dn, c0, first_unused=None):
            pts = []
            for (kap, nk, vap, mask, kb) in pairs:
                pb, pn = bank()
                S.op('pe', lambda e: e.matmul(out=pb[0:nk, 0:qn], lhsT=kap, rhs=qap, start=True, stop=False),
                     reads=['KQ', 'K3T'], writes=[pn])
                S.op('pe', lambda e: e.matmul(out=pb[0:nk, 0:qn], lhsT=idb[0:nk, 0:nk], rhs=mask, start=False, stop=True),
                     reads=['idb', 'mcur', 'mprev'], writes=[pn])
                i = pti['i'] % 4
                pti['i'] += 1
                pt = PTs[i]
                if kb is not None:
                    S.op('act', lambda e: e.activation(out=pt[0:nk, 0:qn], in_=pb[0:nk, 0:qn], func=AF.Exp, bias=kb),
                         reads=[pn, 'fm'], writes=[f'pt{i}'])
                else:
                    S.op('act', lambda e: e.activation(out=pt[0:nk, 0:qn], in_=pb[0:nk, 0:qn], func=AF.Exp),
                         reads=[pn], writes=[f'pt{i}'])
                pts.append((pt, i, nk, vap))
            for j, (pt, i, nk, vap) in enumerate(pts):
                S.op('pe', lambda e: e.matmul(out=ob[:, c0:c0 + qn], lhsT=vap, rhs=pt[0:nk, 0:qn],
                                              start=(j == 0), stop=(j == len(pts) - 1)), reads=[f'pt{i}', 'Vt'], writes=[on])
            for j, (pt, i, nk, vap) in enumerate(pts):
                S.op('pe', lambda e: e.matmul(out=db[:, c0:c0 + qn], lhsT=ones_b[0:nk, :], rhs=pt[0:nk, 0:qn],
                                              start=(j == 0), stop=(j == len(pts) - 1)), reads=[f'pt{i}', 'ones_b'], writes=[dn])

        for j in range(4):
            h1, h2, h3 = j, 4 + j, 8 + j
            ada_deferred(wad, 4)
            for gi, hd in enumerate((h1, h2, h3)):
                wsl, wr = load_w(ws, CH_Q + hd)
                for half in range(2):
                    pb, pn = bank()
                    projT(wsl, wr, hTm, 'hTm', 512 + half * 512, 512, pb, pn)
                    qk_norm(pb, pn, 512, fmc("qg", hd), Qs[gi][:, half * 512:(half + 1) * 512], 'KQ', (sq, rk), True)
                    ada_deferred(wad, 1)
            sp1 = [(384, 512, 0), (896, 512, 512), (1408, 128, 1024)]
            sp2 = [(0, 512, 0), (512, 512, 512), (1024, 512, 1024)]
            sp3 = [(0, 512, 1536), (512, 512, 2048), (1024, 512, 2560)]
            kv_proj(CH_K + h1, hTm, 'hTm', sp1, K1T, 'KQ', fmc("kg", h1), True)
            ada_deferred(wad, 1)
            kv_proj(CH_V + h1, hTm, 'hTm', sp1, V1T, 'VT', None, False)
            ada_deferred(wad, 1)
            kv_proj(CH_K + h2, hTm, 'hTm', sp2, K2T, 'KQ', fmc("kg", h2), True)
            ada_deferred(wad, 1)
            kv_proj(CH_V + h2, hTm, 'hTm', sp2, V2T, 'VT', None, False)
            ada_deferred(wad, 1)
            kv_proj(CH_K + h3, hTm, 'hTm', sp3, K3T[:, j, :], 'K3T', fmc("kg", h3), True)
            ada_deferred(wad, 1)
            kv_proj(CH_V + h3, hTm, 'hTm', sp3, V3T[:, j, :], 'V3T', None, False)
            ada_deferred(wad, 1, prefetch_next=False)
            flush_norm()
            transpose_tiles([(V1T[:, i * 128:(i + 1) * 128], 128) for i in range(9)], V1, 'Vt', 'VT')
            transpose_tiles([(V2T[:, 512 * kt + r:512 * kt + 512:4], 128) for r in range(4) for kt in range(3)], V2, 'Vt', 'VT')
            transpose_tiles([(V3T[:, j, r:2048:16], 128) for r in range(16)], V3[:, 0:16, :], 'Vt', 'V3T')
            transpose_tiles([(V3T[:, j, 2048 + r:LT:16], 64) for r in range(16)], V3[:, 16:32, :], 'Vt', 'V3T')
            bank_set['s'] = list(range(6))
            for hb in range(2):
                ob, on = pbs[6], 'pb6'
                db, dn = pbs[7], 'pb7'
                for q4 in range(4):
                    qb = hb * 4 + q4
                    pairs = [(K1T[:, 128 * (qb + 1):128 * (qb + 2)], 128, V1[:, qb + 1, :], mcur_b, fmc("kb1", qb + 1)),
                             (K1T[:, 128 * qb:128 * (qb + 1)], 128, V1[:, qb, :], mprev_b, fmc("kb1", qb))]
                    attend(pairs, Qs[0][:, 128 * qb:128 * (qb + 1)], 128, ob, on, db, dn, q4 * 128)
                S.op('act', lambda e: e.activation(out=oacc[:, hb * 512:(hb + 1) * 512], in_=ob[:, :], func=AF.Copy),
                     reads=[on], writes=['oacc'])
                S.op('dve', lambda e: e.tensor_copy(out=dacc[:, hb * 512:(hb + 1) * 512], in_=db[:, :]),
                     reads=[dn], writes=['dacc'])
            for qb in range(2):
                ob, on = pbs[6], 'pb6'
                db, dn = pbs[7], 'pb7'
                for r in range(4):
                    pairs = [(K2T[:, 512 * (qb + 1) + r:512 * (qb + 2):4], 128, V2[:, r * 3 + qb + 1, :], mcur_b, fmc("kb2", r * 3 + qb + 1)),
                             (K2T[:, 512 * qb + r:512 * (qb + 1):4], 128, V2[:, r * 3 + qb, :], mprev_b, fmc("kb2", r * 3 + qb))]
                    attend(pairs, Qs[1][:, 512 * qb + r:512 * (qb + 1):4], 128, ob, on, db, dn, r * 128)
                ov = oacc[:, 512 * qb:512 * (qb + 1)].rearrange("p (m r) -> p r m", r=4)
                dv = dacc[:, 512 * qb:512 * (qb + 1)].rearrange("p (m r) -> p r m", r=4)
                S.op('dve', lambda e: e.tensor_tensor(out=ov, in0=ob[:, :].rearrange("p (r m) -> p r m", r=4), in1=ov, op=ALU.add),
                     reads=[on, 'oacc'], writes=['oacc'])
                S.op('dve', lambda e: e.tensor_tensor(out=dv, in0=db[:, :].rearrange("p (r m) -> p r m", r=4), in1=dv, op=ALU.add),
                     reads=[dn, 'dacc'], writes=['dacc'])
            for hb in range(2):
                ob, on = pbs[6], 'pb6'
                db, dn = pbs[7], 'pb7'
                for r8 in range(8):
                    r = hb * 8 + r8
                    pairs = [(K3T[:, j, r:2048:16], 128, V3[:, r, :], mprev_b[:, 0:64], fmc("kb3", r)),
                             (K3T[:, j, 2048 + r:LT:16], 64, V3[0:64, 16 + r, :], mcur_b[0:64, 0:64], None)]
                    attend(pairs, Qs[2][:, r:NT:16], 64, ob, on, db, dn, r8 * 64)
                ov = oacc[:, :].rearrange("p (m r) -> p r m", r=16)[:, hb * 8:(hb + 1) * 8, :]
                dv = dacc[:, :].rearrange("p (m r) -> p r m", r=16)[:, hb * 8:(hb + 1) * 8, :]
                S.op('dve', lambda e: e.tensor_tensor(out=ov, in0=ob[:, :].rearrange("p (r m) -> p r m", r=8), in1=ov, op=ALU.add),
                     reads=[on, 'oacc'], writes=['oacc'])
                S.op('dve', lambda e: e.tensor_tensor(out=dv, in0=db[:, :].rearrange("p (r m) -> p r m", r=8), in1=dv, op=ALU.add),
                     reads=[dn, 'dacc'], writes=['dacc'])
            bank_set['s'] = list(range(8))
            S.op('dve', lambda e: e.reciprocal(out=dacc, in_=dacc), reads=['dacc'], writes=['dacc'])
            S.op('dve', lambda e: e.tensor_tensor(out=attnT[:, j, :], in0=oacc, in1=dacc, op=ALU.mult),
                 reads=['oacc', 'dacc'], writes=['attnT'])
            if stage == "M1":
                dump(Qs[0][:, 0:512], 512, 'KQ'); dump(K1T[:, 0:512], 512, 'KQ'); dump(V1[:, 1, :], 128, 'Vt')
                dump(attnT[:, 0, :], 1024, 'attnT'); dump(dacc, 1024, 'dacc'); dump(V3[:, 5, :], 128, 'Vt'); dump(V3[:, 21, :], 128, 'Vt')
                dump(V2[:, 4, :], 128, 'Vt')
                S.finish()
                return nc
        assert ada_state['col'] == 96 or stage == "M1"
        S.op('dve', lambda e: e.scalar_tensor_tensor(out=A2T, in0=modT2[:, 64:80], scalar=1.0, in1=fmc("g2"),
                                                     op0=ALU.add, op1=ALU.mult), reads=['modT2', 'fm'], writes=['A2T'])
        bcast_cols(modT2[:, 32:48], gate1B, 'gate1B')
        bcast_cols(modT2[:, 80:96], gate2B, 'gate2B')
        S.barrier()
        AX1.lo = x1_mark
        BM = CONST_W + X1_W

        FA = Arena(big, BM + 12288, BM + 24576)
        FB = Arena(big, BM + 27648, TOT)
        U = AX1.alloc([8, 1152], BF16)
        Vc = AX1.alloc([8, NT], F32)
        uact = FA.alloc([8, NT], BF16)
        sig = FA.alloc([512], F32)
        sqv = FA.alloc([NT], F32)
        mean = FA.alloc([NT], F32)
        rstdv = FA.alloc([NT], F32)
        t1 = FA.alloc([NT], F32)
        diags = [FB.alloc([128], BF16) for _ in range(4)]
        spU = [(384, 512, 0), (896, 512, 512), (1408, 128, 1024)]
        for cc in range(8):
            wa_, wra = load_w(ws, cc)
            wb_, wrb = load_w(ws, 8 + cc)
            for (t0, n, d0) in spU:
                pa, pna = bank()
                pg, png = bank()
                projT(wa_, wra, hTm, 'hTm', t0, n, pa, pna)
                projT(wb_, wrb, hTm, 'hTm', t0, n, pg, png)
                S.op('act', lambda e: e.activation(out=sig[:, 0:n], in_=pg[:, 0:n], func=AF.Sigmoid), reads=[png], writes=['sig'])
                S.op('dve', lambda e: e.tensor_tensor(out=U[:, cc, d0:d0 + n], in0=pa[:, 0:n], in1=sig[:, 0:n], op=ALU.mult),
                     reads=[pna, 'sig'], writes=['U'])
            S.op('dve', lambda e: e.tensor_scalar(out=U[:, cc, 0:128], in0=U[:, cc, 0:128], scalar1=fmc("hval", 0), scalar2=None,
                                                  op0=ALU.mult), reads=['U', 'fm'], writes=['U'])
        di = 0
        for cc in range(8):
            pbs2 = [bank(), bank()]
            for k in range(31):
                dg = diags[di % 4]
                dn_ = f'diag{di % 4}'
                di += 1
                S.op('dve', lambda e: e.tensor_scalar(out=dg, in0=ident_f, scalar1=fmc("dw", cc * 31 + k), scalar2=None, op0=ALU.mult),
                     reads=['cst', 'fm'], writes=[dn_])
                for half in range(2):
                    pb, pn = pbs2[half]
                    o0 = 128 - 30 + k + half * 512
                    S.op('pe', lambda e: e.matmul(out=pb[:, :], lhsT=dg, rhs=U[:, cc, o0:o0 + 512], start=(k == 0), stop=(k == 30)),
                         reads=[dn_, 'U'], writes=[pn])
            for half in range(2):
                pb, pn = pbs2[half]
                S.op('act', lambda e: e.activation(out=Vc[:, cc, half * 512:(half + 1) * 512], in_=pb[:, :], func=AF.Identity,
                                                   bias=fmc("db", cc)), reads=[pn, 'fm'], writes=['Vc'])
        for half in range(2):
            pm, pmn = bank()
            pq, pqn = bank()
            hs = slice(half * 512, (half + 1) * 512)
            for cc in range(8):
                S.op('pe', lambda e: e.matmul(out=pm[:, :], lhsT=ones_f, rhs=Vc[:, cc, hs], start=(cc == 0), stop=(cc == 7)),
                     reads=['ones_f', 'Vc'], writes=[pmn])
            for cc in range(8):
                S.op('act', lambda e: e.activation(out=sqv[:, 0:512], in_=Vc[:, cc, hs], func=AF.Square), reads=['Vc'], writes=['sqv'])
                S.op('pe', lambda e: e.matmul(out=pq[:, :], lhsT=ones_f, rhs=sqv[:, 0:512], start=(cc == 0), stop=(cc == 7)),
                     reads=['ones_f', 'sqv'], writes=[pqn])
            S.op('dve', lambda e: e.tensor_scalar(out=mean[:, hs], in0=pm[:, :], scalar1=1.0 / 1024, scalar2=None, op0=ALU.mult),
                 reads=[pmn], writes=['mean'])
            S.op('dve', lambda e: e.tensor_tensor(out=t1[:, hs], in0=mean[:, hs], in1=mean[:, hs], op=ALU.mult),
                 reads=['mean'], writes=['t1'])
            S.op('dve', lambda e: e.scalar_tensor_tensor(out=rstdv[:, hs], in0=pq[:, :], scalar=1.0 / 1024, in1=t1[:, hs],
                                                         op0=ALU.mult, op1=ALU.subtract), reads=[pqn, 't1'], writes=['rstdv'])
            S.op('act', lambda e: e.activation(out=rstdv[:, hs], in_=rstdv[:, hs], func=AF.Sqrt, scale=1.0, bias=EPS),
                 reads=['rstdv'], writes=['rstdv'])
            S.op('dve', lambda e: e.reciprocal(out=rstdv[:, hs], in_=rstdv[:, hs]), reads=['rstdv'], writes=['rstdv'])
        for cc in range(8):
            S.op('dve', lambda e: e.tensor_tensor(out=t1, in0=Vc[:, cc, :], in1=mean, op=ALU.subtract), reads=['Vc', 'mean'], writes=['t1'])
            S.op('dve', lambda e: e.tensor_tensor(out=t1, in0=t1, in1=rstdv, op=ALU.mult), reads=['t1', 'rstdv'], writes=['t1'])
            S.op('act', lambda e: e.activation(out=uact[:, cc, :], in_=t1, func=AF.Silu, scale=fmc("lng", cc), bias=fmc("lnb", cc)),
                 reads=['t1', 'fm'], writes=['uact'])
        S.barrier()
        AX1.lo = x1_mark

        mergedT = Arena(big, BM + 16384, BM + 24576).alloc([KC, NT], BF16)
        wco = [AX1.alloc([8, 128], BF16) for _ in range(2)]
        wao = [AX1.alloc([4, 128], BF16) for _ in range(2)]
        sA = [AX1.alloc([512], F32) for _ in range(2)]
        sB = [AX1.alloc([512], F32) for _ in range(2)]
        tA = [AX1.alloc([512], F32) for _ in range(2)]
        tB = [AX1.alloc([512], F32) for _ in range(2)]
        w_co_v = w_co.rearrange("(cc p) n -> p cc n", p=128)
        w_ao_v = w_ao.rearrange("(j p) n -> p j n", p=128)
        it = 0
        for dc in range(KC):
            s_ = dc % 2
            S.dma('pool', f'wco{s_}', out=wco[s_], in_=w_co_v[:, :, dc * 128:(dc + 1) * 128], writes=[f'wco{s_}'])
            S.dma('pool', f'wao{s_}', out=wao[s_], in_=w_ao_v[:, :, dc * 128:(dc + 1) * 128], writes=[f'wao{s_}'])
            wga, wgar = load_w(ws, CH_G + dc)
            wgb, wgbr = load_w(ws, CH_G + 16 + dc)
            for half in range(2):
                hs = slice(half * 512, (half + 1) * 512)
                b_ = it % 2
                it += 1
                pA, pAn = bank()
                pB, pBn = bank()
                pC, pCn = bank()
                pY, pYn = bank()
                projT(wga, wgar, hTm, 'hTm', 512 + half * 512, 512, pA, pAn)
                projT(wgb, wgbr, hTm, 'hTm', 512 + half * 512, 512, pB, pBn)
                for cc in range(8):
                    S.op('pe', lambda e: e.matmul(out=pC[:, :], lhsT=wco[s_][:, cc, :], rhs=uact[:, cc, hs], start=(cc == 0), stop=(cc == 7)),
                         reads=[f'wco{s_}', 'uact'], writes=[pCn])
                for jj in range(4):
                    S.op('pe', lambda e: e.matmul(out=pY[:, :], lhsT=wao[s_][:, jj, :], rhs=attnT[:, jj, hs], start=(jj == 0), stop=(jj == 3)),
                         reads=[f'wao{s_}', 'attnT'], writes=[pYn])
                S.op('act', lambda e: e.activation(out=sA[b_], in_=pA[:, :], func=AF.Sigmoid), reads=[pAn], writes=[f'sA{b_}'])
                S.op('act', lambda e: e.activation(out=sB[b_], in_=pB[:, :], func=AF.Sigmoid), reads=[pBn], writes=[f'sB{b_}'])
                S.op('dve', lambda e: e.scalar_tensor_tensor(out=tA[b_], in0=pC[:, :], scalar=fmc("bco", dc), in1=sA[b_],
                                                             op0=ALU.add, op1=ALU.mult), reads=[pCn, f'sA{b_}', 'fm'], writes=[f'tA{b_}'])
                S.op('dve', lambda e: e.tensor_tensor(out=tB[b_], in0=pY[:, :], in1=sB[b_], op=ALU.mult),
                     reads=[pYn, f'sB{b_}'], writes=[f'tB{b_}'])
                S.op('dve', lambda e: e.tensor_tensor(out=mergedT[:, dc, hs], in0=tA[b_], in1=tB[b_], op=ALU.add),
                     reads=[f'tA{b_}', f'tB{b_}'], writes=['mergedT'])
        S.barrier()
        AX1.reset()

        x1 = AX1.alloc([8, D], F32)
        AM4 = Arena(big, BM, BM + 16384)
        wos = [AM4.alloc([KC, 512], BF16) for _ in range(2)]
        xr = [AM4.alloc([512], F32) for _ in range(2)]
        tg = [AM4.alloc([512], F32) for _ in range(2)]
        w_out_v = w_out.rearrange("(kc p) n -> p kc n", p=128)
        it = 0
        for nb in range(4):
            ns = slice(nb * 512, (nb + 1) * 512)
            wo = wos[nb % 2]
            wn = f'wo{nb % 2}'
            S.dma('pool', wn, out=wo, in_=w_out_v[:, :, ns], writes=[wn])
            for tt in range(8):
                b_ = it % 2
                it += 1
                S.dma('sp', f'xr{b_}', out=xr[b_], in_=xh[2048 + tt * 128:2048 + (tt + 1) * 128, ns], writes=[f'xr{b_}'])
                pb, pn = bank()
                for kc in range(KC):
                    S.op('pe', lambda e: e.matmul(out=pb[:, :], lhsT=mergedT[:, kc, tt * 128:(tt + 1) * 128], rhs=wo[:, kc, :],
                                                  start=(kc == 0), stop=(kc == KC - 1)), reads=['mergedT', wn], writes=[pn])
                S.op('dve', lambda e: e.tensor_tensor(out=tg[b_], in0=pb[:, :], in1=gate1B[:, ns], op=ALU.mult),
                     reads=[pn, 'gate1B'], writes=[f'tg{b_}'])
                S.op('dve', lambda e: e.tensor_tensor(out=x1[:, tt, ns], in0=tg[b_], in1=xr[b_], op=ALU.add),
                     reads=[f'tg{b_}', f'xr{b_}'], writes=['x1'])
        S.barrier()

        if stage == "x1":
            for tt in range(8):
                S.dma('sp', 'out', out=y[tt * 128:(tt + 1) * 128, :], in_=x1[:, tt, :], reads=['x1'])
            S.finish()
            return nc

        BM = CONST_W + X1_W
        P = Arena(big, BM, TOT)
        h2T = P.alloc([KC, NT], BF16)
        p_mark = P.lo
        junk = [P.alloc([D], BF16)]
        xsb = [P.alloc([D], BF16) for _ in range(2)]
        for tt in range(8):
            norm_T(x1[:, tt, :], 'x1', h2T[:, :, tt * 128:(tt + 1) * 128], 'h2T', A2T, 'A2T', sh2T, junk, xsb, 'modT2')
        S.barrier()
        P.lo = p_mark
        e1T = P.alloc([NT], F32)
        e2T = P.alloc([NT], F32)
        gT = P.alloc([NT], F32)
        g_mark = P.lo
        wqs = [P.alloc([KC, 128], BF16) for _ in range(2)]
        qpT = P.alloc([16, 512], F32)
        s_sb = P.alloc([16, 128], F32)
        v1 = P.alloc([16, 16], F32)
        i1 = P.alloc([16, 16], U32)
        i1f = P.alloc([16, 16], F32)
        wks = [P.alloc([128], F32) for _ in range(4)]
        cand = P.alloc([8, 256], F32)
        wk2s = [P.alloc([256], F32) for _ in range(2)]
        top = P.alloc([8, 16], F32)
        ci = P.alloc([8, 16], U32)
        cu = P.alloc([8, 16], U32)
        af = P.alloc([8, 16], F32)
        bf_ = P.alloc([8, 16], F32)
        ex = P.alloc([8, 16], F32)
        zs = P.alloc([8], F32)
        gg = P.alloc([8, 16], F32)
        oh = cand
        e1f = P.alloc([8, 16], F32)
        e2f = P.alloc([8, 16], F32)
        w_q_v = w_q.rearrange("(kc p) n -> p kc n", p=128)
        iota16 = cst[:, 512:528]
        iota128 = cst[:, 384:512]
        wqi = 0
        for half in range(2):
            for cc in range(16):
                s_ = wqi % 2
                wqi += 1
                S.dma('pool', f'wq{s_}', out=wqs[s_], in_=w_q_v[:, :, cc * 128:(cc + 1) * 128], writes=[f'wq{s_}'])
                pb, pn = bank()
                projT(wqs[s_], f'wq{s_}', h2T, 'h2T', half * 512, 512, pb, pn)
                S.op('act', lambda e: e.activation(out=qpT[:, cc, :], in_=pb[:, :], func=AF.Copy), reads=[pn], writes=['qpT'])
            for t4 in range(4):
                tt = half * 4 + t4
                for b4 in range(4):
                    pb, pn = bank()
                    for k4 in range(4):
                        hp = b4 * 4 + k4
                        S.op('pe', lambda e: e.matmul(out=pb[:, k4 * 128:(k4 + 1) * 128], lhsT=qpT[:, hp, t4 * 128:(t4 + 1) * 128],
                                                      rhs=skT[:, hp, :], start=True, stop=True), reads=['qpT', 'skT'], writes=[pn])
                    S.op('act', lambda e: e.activation(out=s_sb[:, b4 * 4:(b4 + 1) * 4, :].rearrange("p a b -> p (a b)"), in_=pb[:, :], func=AF.Copy),
                         reads=[pn], writes=['s_sb'])
                NCH = 4
                for hp0 in range(0, 16, NCH):
                    hps = list(range(hp0, hp0 + NCH))
                    for hp in hps:
                        S.op('dve', lambda e: e.max(out=v1[:, hp, 0:8], in_=s_sb[:, hp, :]), reads=['s_sb'], writes=[f'v1a{hp}'])
                    for hp in hps:
                        S.op('dve', lambda e: e.max_index(out=i1[:, hp, 0:8], in_max=v1[:, hp, 0:8], in_values=s_sb[:, hp, :]),
                             reads=['s_sb', f'v1a{hp}'], writes=[f'i1a{hp}'])
                    for hp in hps:
                        S.op('dve', lambda e: e.match_replace(out=wks[hp % NCH], in_to_replace=v1[:, hp, 0:8], in_values=s_sb[:, hp, :], imm_value=-1e30),
                             reads=['s_sb', f'v1a{hp}'], writes=[f'wk{hp % NCH}'])
                    for hp in hps:
                        S.op('dve', lambda e: e.max(out=v1[:, hp, 8:16], in_=wks[hp % NCH]), reads=[f'wk{hp % NCH}'], writes=[f'v1b{hp}'])
                    for hp in hps:
                        S.op('dve', lambda e: e.max_index(out=i1[:, hp, 8:16], in_max=v1[:, hp, 8:16], in_values=wks[hp % NCH]),
                             reads=[f'wk{hp % NCH}', f'v1b{hp}'], writes=[f'i1b{hp}'])
                v1all = [f'v1a{hp}' for hp in range(16)] + [f'v1b{hp}' for hp in range(16)]
                i1all = [f'i1a{hp}' for hp in range(16)] + [f'i1b{hp}' for hp in range(16)]
                v1v = v1.rearrange("p (h q) a -> p h q a", q=2)
                i1fv = i1f.rearrange("p (h q) a -> p h q a", q=2)
                cand4 = cand.rearrange("p h (a b) -> p h a b", a=16)
                S.op('dve', lambda e: e.tensor_tensor(out=cand4, in0=bc(v1v[:, :, 0, :], 3, 16), in1=bc(v1v[:, :, 1, :], 2, 16), op=ALU.add),
                     reads=v1all, writes=['cand'])
                for h0 in range(0, 8, 2):
                    hs2 = (h0, h0 + 1)
                    for h in hs2:
                        S.op('dve', lambda e: e.max(out=top[:, h, 0:8], in_=cand[:, h, :]), reads=['cand'], writes=[f'topa{h}'])
                    for h in hs2:
                        S.op('dve', lambda e: e.max_index(out=ci[:, h, 0:8], in_max=top[:, h, 0:8], in_values=cand[:, h, :]),
                             reads=['cand', f'topa{h}'], writes=[f'cia{h}'])
                    for h in hs2:
                        S.op('dve', lambda e: e.match_replace(out=wk2s[h % 2], in_to_replace=top[:, h, 0:8], in_values=cand[:, h, :], imm_value=-1e30),
                             reads=['cand', f'topa{h}'], writes=[f'wk2{h % 2}'])
                    for h in hs2:
                        S.op('dve', lambda e: e.max(out=top[:, h, 8:16], in_=wk2s[h % 2]), reads=[f'wk2{h % 2}'], writes=[f'topb{h}'])
                    for h in hs2:
                        S.op('dve', lambda e: e.max_index(out=ci[:, h, 8:16], in_max=top[:, h, 8:16], in_values=wk2s[h % 2]),
                             reads=[f'wk2{h % 2}', f'topb{h}'], writes=[f'cib{h}'])
                topall = [f'topa{h}' for h in range(8)] + [f'topb{h}' for h in range(8)]
                ciall = [f'cia{h}' for h in range(8)] + [f'cib{h}' for h in range(8)]
                S.op('dve', lambda e: e.tensor_tensor(out=ex, in0=top, in1=bc(top[:, :, 0], 2, 16), op=ALU.subtract), reads=topall, writes=['ex'])
                S.op('act', lambda e: e.activation(out=ex, in_=ex, func=AF.Exp), reads=['ex'], writes=['ex'])
                S.op('dve', lambda e: e.tensor_reduce(out=zs, in_=ex, axis=AX.X, op=ALU.add), reads=['ex'], writes=['zs'])
                S.op('dve', lambda e: e.reciprocal(out=zs, in_=zs), reads=['zs'], writes=['zs'])
                S.op('dve', lambda e: e.tensor_tensor(out=gg, in0=ex, in1=bc(zs, 2, 16), op=ALU.mult), reads=['ex', 'zs'], writes=['gg'])
                S.op('dve', lambda e: e.tensor_copy(out=i1f, in_=i1), reads=i1all, writes=['i1f'])
                S.op('dve', lambda e: e.tensor_scalar(out=cu, in0=ci, scalar1=4, scalar2=None, op0=ALU.logical_shift_right), reads=ciall, writes=['cu'])
                S.op('dve', lambda e: e.tensor_copy(out=af, in_=cu), reads=['cu'], writes=['af'])
                S.op('dve', lambda e: e.tensor_scalar(out=cu, in0=ci, scalar1=15, scalar2=None, op0=ALU.bitwise_and), reads=ciall, writes=['cu'])
                S.op('dve', lambda e: e.tensor_copy(out=bf_, in_=cu), reads=['cu'], writes=['bf'])
                oh4 = oh.rearrange("p h (k a) -> p h k a", k=16)
                io4 = bc(bc(iota16, 1, 16), 1, 8)
                for (src, q_, dst, dn_) in ((af, 0, e1f, 'e1f'), (bf_, 1, e2f, 'e2f')):
                    S.op('dve', lambda e: e.tensor_tensor(out=oh4, in0=io4, in1=bc(src, 3, 16), op=ALU.is_equal), reads=['af', 'bf', 'cst'], writes=['cand'])
                    S.op('dve', lambda e: e.tensor_tensor(out=oh4, in0=oh4, in1=bc(i1fv[:, :, q_, :], 2, 16), op=ALU.mult), reads=['cand', 'i1f'], writes=['cand'])
                    S.op('dve', lambda e: e.tensor_reduce(out=dst, in_=oh4, axis=AX.X, op=ALU.add), reads=['cand'], writes=[dn_])
                pb, pn = bank()
                for k3, (src, sn) in enumerate(((e1f, 'e1f'), (e2f, 'e2f'), (gg, 'gg'))):
                    S.op('pe', lambda e: e.transpose(out=pb[:, k3 * 128:(k3 + 1) * 128], in_=src.rearrange("p h k -> p (h k)"), identity=ident_f),
                         reads=[sn, 'cst'], writes=[pn])
                for k3, (dst, dn_) in enumerate(((e1T, 'e1T'), (e2T, 'e2T'), (gT, 'gT'))):
                    S.op('act', lambda e: e.activation(out=dst[:, tt * 128:(tt + 1) * 128], in_=pb[:, k3 * 128:(k3 + 1) * 128], func=AF.Copy),
                         reads=[pn], writes=[dn_])
        S.barrier()
        if stage == "R":
            dump(e1T, 1024, 'e1T'); dump(e2T, 1024, 'e2T'); dump(gT, 1024, 'gT')
            S.finish()
            return nc
        P.lo = g_mark
        iob = P.alloc([128], BF16)
        e1b = P.alloc([NT], BF16)
        e2b = P.alloc([NT], BF16)
        gb16 = P.alloc([NT], BF16)
        S.op('dve', lambda e: e.tensor_copy(out=iob, in_=iota128), reads=['cst'], writes=['iob'])
        S.op('dve', lambda e: e.tensor_copy(out=e1b, in_=e1T), reads=['e1T'], writes=['e1b'])
        S.op('dve', lambda e: e.tensor_copy(out=e2b, in_=e2T), reads=['e2T'], writes=['e2b'])
        S.op('dve', lambda e: e.tensor_copy(out=gb16, in_=gT), reads=['gT'], writes=['gb16'])
        P1s = [P.alloc([16, 128], BF16) for _ in range(2)]
        P2s = [P.alloc([16, 128], BF16) for _ in range(2)]
        P2g = [P.alloc([16, 128], BF16) for _ in range(2)]
        Gs = P.alloc([128, 128], BF16)
        gscr_v = gscr.rearrange("j i t -> i j t")
        gi = 0
        ev_i = 0
        for tb in range(8):
            for g16 in range(8):
                t0 = tb * 128 + g16 * 16
                b_ = gi % 2
                gi += 1
                for tl in range(16):
                    S.op('dve', lambda e: e.tensor_scalar(out=P1s[b_][:, tl, :], in0=iob, scalar1=e1T[:, t0 + tl:t0 + tl + 1], scalar2=None,
                                                          op0=ALU.is_equal), reads=['iob', 'e1T'], writes=[f'P1{b_}'])
                    S.op('dve', lambda e: e.tensor_scalar(out=P2g[b_][:, tl, :], in0=iob, scalar1=e2T[:, t0 + tl:t0 + tl + 1],
                                                          scalar2=gT[:, t0 + tl:t0 + tl + 1], op0=ALU.is_equal, op1=ALU.mult),
                         reads=['iob', 'e2T', 'gT'], writes=[f'P2g{b_}'])
                for q4 in range(4):
                    pb, pn = bank()
                    for k4 in range(4):
                        tl = q4 * 4 + k4
                        S.op('pe', lambda e: e.matmul(out=pb[:, k4 * 128:(k4 + 1) * 128], lhsT=P1s[b_][:, tl, :], rhs=P2g[b_][:, tl, :],
                                                      start=True, stop=True), reads=[f'P1{b_}', f'P2g{b_}'], writes=[pn])
                    c0 = g16 * 16 + q4 * 4
                    src = pb[:, :].rearrange("p (t j) -> p j t", t=4)
                    S.op('act', lambda e: e.activation(out=Gs[:, :, c0:c0 + 4], in_=src, func=AF.Copy), reads=[pn], writes=['Gs'])
                    ev_i += 1
            for jq in range(4):
                S.dma('sp', 'gsw', out=gscr_v[:, jq * 32:(jq + 1) * 32, tb * 128:(tb + 1) * 128], in_=Gs[:, jq * 32:(jq + 1) * 32, :],
                      reads=['Gs'], writes=['gscr'])
        S.barrier()
        P.lo = p_mark
        JG = 4
        NG = 128 // JG
        Gt = [P.alloc([JG, NT], BF16) for _ in range(2)]
        NWU = 4
        wup = [P.alloc([KC, 128], BF16) for _ in range(NWU)]
        NWD = 7
        wdn = [P.alloc([D], BF16) for _ in range(NWD)]
        GaT = P.alloc([JG, NT], BF16)
        gel = [P.alloc([512], F32) for _ in range(2)]
        evb = [P.alloc([512], F32) for _ in range(4)]
        st = {'u': 0, 'd': 0}
        uslot = {}
        dslot = {}

        def load_G(g):
            S.dma('sp', f'Gt{g % 2}', out=Gt[g % 2], in_=gscr_v[:, g * JG:(g + 1) * JG, :], reads=['gscr'], writes=[f'Gt{g % 2}'])

        def load_up(j):
            us = st['u'] % NWU
            st['u'] += 1
            uslot[j] = us
            S.dma('pool', f'wup{us}', out=wup[us], in_=w_upP[j].rearrange("p (kc i) -> p kc i", kc=KC), writes=[f'wup{us}'])

        def load_dn(j):
            ds = st['d'] % NWD
            st['d'] += 1
            dslot[j] = ds
            S.dma('pool', f'wdn{ds}', out=wdn[ds].rearrange("p (a b) -> p a b", a=4), in_=w_dnP[j].rearrange("p (a b) -> p a b", a=4),
                  writes=[f'wdn{ds}'])

        load_G(0)
        for jj in range(JG):
            load_up(jj)
            load_dn(jj)
        gl = 0
        ei = 0
        for g in range(NG):
            b_ = g % 2
            j0 = g * JG
            for jj in range(JG):
                us = uslot[j0 + jj]
                for half in range(2):
                    hs = slice(half * 512, (half + 1) * 512)
                    pb, pn = bank()
                    projT(wup[us], f'wup{us}', h2T, 'h2T', half * 512, 512, pb, pn)
                    gb = gl % 2
                    gl += 1
                    S.op('act', lambda e: e.activation(out=gel[gb], in_=pb[:, :], func=AF.Gelu), reads=[pn], writes=[f'gel{gb}'])
                    S.op('dve', lambda e: e.tensor_tensor(out=GaT[:, jj, hs], in0=gel[gb], in1=Gt[b_][:, jj, hs], op=ALU.mult),
                         reads=[f'gel{gb}', f'Gt{b_}'], writes=['GaT'])
            if g + 1 < NG:
                load_G(g + 1)
                for jj in range(JG):
                    load_up(j0 + JG + jj)
                for jj in range(JG - 1):
                    load_dn(j0 + JG + jj)
            for tt in range(8):
                for dq in range(4):
                    ds_ = slice(dq * 512, (dq + 1) * 512)
                    pb, pn = bank()
                    for jj in range(JG):
                        dsl = dslot[j0 + jj]
                        S.op('pe', lambda e: e.matmul(out=pb[:, :], lhsT=GaT[:, jj, tt * 128:(tt + 1) * 128], rhs=wdn[dsl][:, ds_],
                                                      start=(jj == 0), stop=(jj == JG - 1)), reads=['GaT', f'wdn{dsl}'], writes=[pn])
                    eb = ei % 4
                    ei += 1
                    S.op('dve', lambda e: e.tensor_tensor(out=evb[eb], in0=pb[:, :], in1=gate2B[:, ds_], op=ALU.mult),
                         reads=[pn, 'gate2B'], writes=[f'ev{eb}'])
                    xres = f'x1_{tt}_{dq}'
                    if eb % 2 == 0:
                        S.op('pool', lambda e: e.tensor_tensor(out=x1[:, tt, ds_], in0=x1[:, tt, ds_], in1=evb[eb], op=ALU.add),
                             reads=[f'ev{eb}', xres, 'x1'], writes=[xres])
                    else:
                        S.op('dve', lambda e: e.tensor_tensor(out=x1[:, tt, ds_], in0=x1[:, tt, ds_], in1=evb[eb], op=ALU.add),
                             reads=[f'ev{eb}', xres, 'x1'], writes=[xres])
            if g + 1 < NG:
                load_dn(j0 + JG + JG - 1)
        for tt in range(8):
            S.dma('sp', 'out', out=y[tt * 128:(tt + 1) * 128, :], in_=x1[:, tt, :], reads=['x1'] + [f'x1_{tt}_{dq}' for dq in range(4)])
        S.finish()
    return nc


_CACHE = {}


def _host_consts():
    cst = np.zeros((128, NCS), np.float32)
    cst[:, 0:128] = np.eye(128, dtype=np.float32)
    mk = np.arange(128)[:, None]
    mq = np.arange(128)[None, :]
    cst[:, 128:256] = np.where(mq >= mk, 0.0, NEG)
    cst[:, 256:384] = np.where(mq <= mk, 0.0, NEG)
    cst[:, 384:512] = np.arange(128, dtype=np.float32)[None, :]
    cst[:, 512:528] = np.arange(16, dtype=np.float32)[None, :]
    return cst


def _fmT(v, n):
    return np.ascontiguousarray(np.asarray(v, np.float32).reshape(n, 128).T)


def kernel(**inputs):
    f = lambda k: np.asarray(inputs[k], np.float32)
    x, c = f("x"), f("c")
    if "nc" not in _CACHE:
        _CACHE["nc"] = build_program()
    nc = _CACHE["nc"]
    cst = _host_consts()
    sk = f("peer_sub_keys")[0]
    skT = np.ascontiguousarray(sk.reshape(16, 128, 128).transpose(2, 0, 1).reshape(128, 2048))
    w_up = f("peer_w_up")[0]
    w_dn = f("peer_w_down")[0]
    w_upP = np.ascontiguousarray(w_up.reshape(128, 128, KC, 128).transpose(1, 3, 2, 0).reshape(128, 128, KC * 128))
    w_dnP = np.ascontiguousarray(w_dn.reshape(128, 128, D).transpose(1, 0, 2))
    shared = {
        "cst": cst, "skT": skT, "w_ada": f("w_ada")[0], "w_in": f("w_in")[0], "w_co": f("w_conv_out")[0],
        "w_ao": f("w_attn_o")[0], "w_out": f("w_out")[0], "w_q": f("peer_w_q")[0], "w_upP": w_upP, "w_dnP": w_dnP,
    }
    fm_shared = np.zeros((128, NFM), np.float32)

    def put(a, name, arr):
        o0, o1 = FM[name]
        a[:, o0:o1] = arr
    put(fm_shared, "bada", _fmT(f("b_ada")[0], 96))
    put(fm_shared, "g1", _fmT(f("norm1_g")[0], 16))
    put(fm_shared, "g2", _fmT(f("norm2_g")[0], 16))
    dw = f("conv_dw")[0]
    put(fm_shared, "dw", np.ascontiguousarray(dw.reshape(31, 8, 128).transpose(2, 1, 0).reshape(128, 248)))
    put(fm_shared, "db", _fmT(f("conv_db")[0], 8))
    put(fm_shared, "lng", _fmT(f("conv_ln_g")[0], 8))
    put(fm_shared, "lnb", _fmT(f("conv_ln_b")[0], 8))
    put(fm_shared, "bco", _fmT(f("b_conv_out")[0], 16))
    put(fm_shared, "qg", np.ascontiguousarray(f("q_norm_g")[0].T))
    put(fm_shared, "kg", np.ascontiguousarray(f("k_norm_g")[0].T))
    in_maps = []
    for core in range(8):
        b, q = core // 4, core % 4
        lo = 1024 * q - 2048
        xhh = np.zeros((LT, D), np.float32)
        s0 = max(lo, 0)
        xhh[s0 - lo:] = x[b, s0:1024 * q + 1024]
        valid = (lo + np.arange(LT)) >= 0
        kbias = np.where(valid, 0.0, NEG).astype(np.float32)
        fmc_ = fm_shared.copy()
        put(fmc_, "cT", _fmT(c[b], 16))
        put(fmc_, "hval", np.full((128, 1), 1.0 if q > 0 else 0.0, np.float32))
        p = np.arange(128)
        put(fmc_, "kb1", np.stack([kbias[1920 + 128 * i + p] for i in range(9)], axis=1))
        put(fmc_, "kb2", np.stack([kbias[1536 + 4 * (128 * kt + p) + r] for r in range(4) for kt in range(3)], axis=1))
        put(fmc_, "kb3", np.stack([kbias[16 * p + r] for r in range(16)], axis=1))
        m = dict(shared)
        m["xh"] = xhh
        m["fm"] = fmc_
        in_maps.append(m)
    res = run_bass_kernel_spmd(nc, in_maps, core_ids=list(range(8)))
    out = np.zeros((2, 4096, D), np.float32)
    for core in range(8):
        b, q = core // 4, core % 4
        out[b, 1024 * q:1024 * q + 1024] = res.results[core]["y"]
    return out
```

```python
import contextlib
import numpy as np
import concourse.bass as bass
import concourse.mybir as mybir
from concourse.bass_utils import run_bass_kernel_spmd

F32 = mybir.dt.float32
BF16 = mybir.dt.bfloat16
U32 = mybir.dt.uint32
AF = mybir.ActivationFunctionType
ALU = mybir.AluOpType
AX = mybir.AxisListType

D = 2048
KC = 16
NT = 1024
LT = 3072
EPS = 1e-6
NEG = -30000.0
IN_COLS = 10752
CH_Q, CH_K, CH_V, CH_G = 16, 28, 40, 52

FM = {}
_o = 0
for _n, _w in [("cT", 16), ("bada", 96), ("g1", 16), ("g2", 16), ("dw", 248), ("db", 8), ("lng", 8), ("lnb", 8),
               ("bco", 16), ("qg", 12), ("kg", 12), ("hval", 1), ("kb1", 9), ("kb2", 12), ("kb3", 16)]:
    FM[_n] = (_o, _o + _w)
    _o += _w
NFM = _o
CS = {"ident": (0, 128), "mcur": (128, 256), "mprev": (256, 384), "iota": (384, 512), "iota16": (512, 528)}
NCS = 528

NO_SELF_SYNC = ("pe",)
STAGE = "full"


class Sched:
    def __init__(self, nc, es):
        self.nc = nc
        self.es = es
        self.eng = {'pe': nc.tensor, 'act': nc.scalar, 'dve': nc.vector, 'pool': nc.gpsimd, 'sp': nc.sync}
        self.sem = {k: es.enter_context(nc.semaphore('sem_' + k)) for k in self.eng}
        self.cnt = {k: 0 for k in self.eng}
        self.seen = {k: {} for k in self.eng}
        self.last_w = {}
        self.readers = {}
        self.dsem = {}
        self.dcnt = {}
        self.bank_i = 0

    def _semof(self, key):
        return self.sem[key] if key in self.sem else self.dsem[key]

    def _deps(self, reads, writes):
        deps = {}

        def add(k, c):
            if deps.get(k, 0) < c:
                deps[k] = c
        for r in reads:
            ev = self.last_w.get(r)
            if ev is not None:
                add(*ev)
        for w in writes:
            ev = self.last_w.get(w)
            if ev is not None:
                add(*ev)
            for k, c in self.readers.get(w, {}).items():
                add(k, c)
        return deps

    def _wait(self, e, deps):
        for k, c in deps.items():
            if k == e and e in NO_SELF_SYNC:
                continue
            if self.seen[e].get(k, 0) >= c:
                continue
            self.eng[e].wait_ge(self._semof(k), c)
            self.seen[e][k] = c

    def _record(self, ev, reads, writes):
        k, c = ev
        for r in reads:
            self.readers.setdefault(r, {})[k] = c
        for w in writes:
            self.last_w[w] = ev
            self.readers[w] = {}

    def op(self, e, fn, reads=(), writes=()):
        self._wait(e, self._deps(reads, writes))
        ins = fn(self.eng[e])
        self.cnt[e] += 1
        ins.then_inc(self.sem[e], 1)
        self._record((e, self.cnt[e]), reads, writes)
        return ins

    def dma(self, q, semname, reads=(), writes=(), out=None, in_=None, fn=None, **kw):
        if semname not in self.dsem:
            self.dsem[semname] = self.es.enter_context(self.nc.semaphore('d_' + semname))
            self.dcnt[semname] = 0
        self._wait(q, self._deps(reads, writes))
        if fn is not None:
            ins = fn(self.eng[q])
        else:
            ins = self.eng[q].dma_start(out=out, in_=in_, **kw)
        self.dcnt[semname] += 16
        ins.then_inc(self.dsem[semname], 16)
        self._record((semname, self.dcnt[semname]), reads, writes)
        return ins

    def barrier(self):
        evs = {k: c for k, c in self.cnt.items() if c > 0}
        evs.update({k: c for k, c in self.dcnt.items() if c > 0})
        for e in self.eng:
            self._wait(e, dict(evs))

    def finish(self, q='sp'):
        evs = {k: c for k, c in self.dcnt.items() if c > 0}
        evs.update({k: c for k, c in self.cnt.items() if c > 0 and k != q})
        self._wait(q, evs)


class Arena:
    def __init__(self, base_ap, lo, hi):
        self.base = base_ap
        self.lo0, self.hi0 = lo, hi
        self.lo, self.hi = lo, hi

    def reset(self):
        self.lo, self.hi = self.lo0, self.hi0

    def alloc(self, shape, dtype, top=False):
        n = int(np.prod(shape))
        isz = 4 if dtype in (F32, U32) else 2
        words = (n * isz + 3) // 4
        if top:
            self.hi -= words
            off = self.hi
        else:
            off = self.lo
            self.lo += words
        assert self.lo <= self.hi, ("arena overflow", self.lo, self.hi)
        v = self.base[:, off:off + words]
        if dtype != F32:
            v = v.bitcast(dtype)
        v = v[:, 0:n]
        if len(shape) == 2:
            v = v.rearrange("p (a b) -> p a b", a=shape[0], b=shape[1])
        elif len(shape) == 3:
            v = v.rearrange("p (a b c) -> p a b c", a=shape[0], b=shape[1], c=shape[2])
        return v


def bc(ap2, reps_axis, n):
    pat = [list(x) for x in ap2.ap]
    pat.insert(reps_axis, [0, n])
    return bass.AP(tensor=ap2.tensor, offset=ap2.offset, ap=pat)


def build_program(stage=None):
    stage = stage or STAGE
    nc = bass.Bass("TRN2", target_bir_lowering=False)
    early = stage in ("A", "H", "M1")
    def dram(n, s, d=F32, kind="ExternalInput"):
        if early and n in ("w_co", "w_ao", "w_out", "w_q", "w_upP", "w_dnP") or (stage == "A" and n in ("w_in", "xh")):
            return None
        if (stage == "x1" and n in ("w_q", "w_upP", "w_dnP")) or (stage == "R" and n in ("w_upP", "w_dnP")):
            return None
        return nc.dram_tensor(n, s, d, kind=kind).ap()
    dbg = nc.dram_tensor("dbg", [128, 8192], F32, kind="ExternalOutput").ap() if stage != "full" else None
    xh = dram("xh", [LT, D])
    fm_d = dram("fm", [128, NFM])
    cst_d = dram("cst", [128, NCS])
    skT_d = dram("skT", [128, 2048])
    w_ada = dram("w_ada", [96, 128, D])
    w_in = dram("w_in", [84, 128, D])
    w_co = dram("w_co", [1024, D])
    w_ao = dram("w_ao", [512, D])
    w_out = dram("w_out", [D, D])
    w_q = dram("w_q", [D, D])
    w_upP = dram("w_upP", [128, 128, KC * 128])
    w_dnP = dram("w_dnP", [128, 128, D])
    y = dram("y", [NT, D], F32, "ExternalOutput")
    gscr = nc.dram_tensor("gscr", [128, 128, NT], BF16, kind="Internal").ap()

    with contextlib.ExitStack() as es:
        S = Sched(nc, es)
        TOT = 53100
        big = es.enter_context(nc.sbuf_tensor("arena", [128, TOT], F32))
        pbs = [es.enter_context(nc.psum_tensor(f"pb{i}", [128, 512], F32)) for i in range(8)]
        CONST_W = 7900
        X1_W = 16384
        AC = Arena(big, 0, CONST_W)
        AX1 = Arena(big, CONST_W, CONST_W + X1_W)
        AM = Arena(big, CONST_W + X1_W, TOT)

        dbgc = {'c': 0}

        def dump(ap, n, res):
            c0 = dbgc['c']
            dbgc['c'] += n
            S.dma('pool', 'dbg', out=dbg[:, c0:c0 + n], in_=ap, reads=[res])
            return c0

        bank_set = {'s': list(range(8))}

        def bank():
            bs = bank_set['s']
            i = bs[S.bank_i % len(bs)]
            S.bank_i += 1
            return pbs[i], f"pb{i}"

        fm = AC.alloc([NFM], F32)
        cst = AC.alloc([NCS], F32)
        skT = AC.alloc([16, 128], F32)
        idb = AC.alloc([128], BF16)
        mcur_b = AC.alloc([128], BF16)
        mprev_b = AC.alloc([128], BF16)
        ones_f = AC.alloc([128], F32)
        ones_b = AC.alloc([128], BF16)
        sc = AC.alloc([16], F32)
        scb = AC.alloc([16], BF16)
        modT = AC.alloc([96], F32)
        modT2 = modT
        A1T = AC.alloc([16], F32)
        A2T = AC.alloc([16], F32)
        gate1B = AC.alloc([D], F32)
        gate2B = AC.alloc([D], F32)
        nsc = [(AC.alloc([1], F32), AC.alloc([1], F32), AC.alloc([1], F32)) for _ in range(2)]
        nstate = {'i': 0, 'q': 0}
        epsc = AC.alloc([2], F32)
        eps1 = epsc[:, 0:1]
        eps128 = epsc[:, 1:2]
        diagf = AC.alloc([128], F32)

        def fmc(name, a=None, b=None):
            o0, o1 = FM[name]
            if a is None:
                return fm[:, o0:o1]
            return fm[:, o0 + a:o0 + (b if b is not None else a + 1)]
        ident_f = cst[:, 0:128]

        S.dma('sp', 'c0', out=fm, in_=fm_d[:, :], writes=['fm'])
        S.dma('sp', 'c1', out=cst, in_=cst_d[:, :], writes=['cst'])
        S.dma('sp', 'c2', out=skT.rearrange("p a b -> p (a b)"), in_=skT_d[:, :], writes=['skT'])
        S.op('dve', lambda e: e.tensor_copy(out=idb, in_=ident_f), reads=['cst'], writes=['idb'])
        S.op('dve', lambda e: e.tensor_copy(out=mcur_b, in_=cst[:, 128:256]), reads=['cst'], writes=['mcur'])
        S.op('dve', lambda e: e.tensor_copy(out=mprev_b, in_=cst[:, 256:384]), reads=['cst'], writes=['mprev'])
        S.op('dve', lambda e: e.memset(ones_f, 1.0), writes=['ones_f'])
        S.op('dve', lambda e: e.memset(ones_b, 1.0), writes=['ones_b'])
        S.op('dve', lambda e: e.memset(eps1, EPS), writes=['epsc'])
        S.op('dve', lambda e: e.memset(eps128, 128.0 * EPS), writes=['epsc'])

        S.op('act', lambda e: e.activation(out=sc, in_=fmc("cT"), func=AF.Silu), reads=['fm'], writes=['sc'])
        S.op('dve', lambda e: e.tensor_copy(out=scb, in_=sc), reads=['sc'], writes=['scb'])
        wa = [AX1.alloc([4, 16, 128], BF16) for _ in range(3)]
        for nb in range(8):
            s_ = nb % 3
            S.dma('pool', f'wa{s_}', out=wa[s_].rearrange("p s k c -> p s (k c)"), in_=w_ada[nb * 4:(nb + 1) * 4].rearrange("s p f -> p s f"), writes=[f'wa{s_}'])
            pb, pn = bank()
            for sub in range(4):
                for kc in range(KC):
                    S.op('pe', lambda e: e.matmul(out=pb[:, sub:sub + 1], lhsT=wa[s_][:, sub, kc, :],
                                                  rhs=scb[:, kc:kc + 1], start=(kc == 0), stop=(kc == KC - 1)),
                         reads=[f'wa{s_}', 'scb'], writes=[pn])
            S.op('dve', lambda e: e.tensor_tensor(out=modT[:, nb * 4:nb * 4 + 4], in0=pb[:, 0:4],
                                                  in1=fmc("bada", nb * 4, nb * 4 + 4), op=ALU.add),
                 reads=[pn, 'fm'], writes=['modT'])
        ada_state = {'col': 32, 'i': 0, 'pend': None}

        def ada_prefetch(wad):
            col = ada_state['col']
            if col >= 96 or ada_state['pend'] is not None:
                return
            ada_state['col'] += 1
            s_ = ada_state['i'] % len(wad)
            ada_state['i'] += 1
            S.dma('pool', f'wad{s_}', out=wad[s_].rearrange("p k c -> p (k c)"), in_=w_ada[col], writes=[f'wad{s_}'])
            ada_state['pend'] = (col, s_)

        def ada_deferred(wad, nblk, prefetch_next=True):
            flush_norm()
            for _ in range(nblk):
                if ada_state['pend'] is None:
                    ada_prefetch(wad)
                if ada_state['pend'] is None:
                    return
                col, s_ = ada_state['pend']
                ada_state['pend'] = None
                pb, pn = bank()
                for kc in range(KC):
                    S.op('pe', lambda e: e.matmul(out=pb[:, 0:1], lhsT=wad[s_][:, kc, :], rhs=scb[:, kc:kc + 1],
                                                  start=(kc == 0), stop=(kc == KC - 1)), reads=[f'wad{s_}', 'scb'], writes=[pn])
                S.op('dve', lambda e: e.tensor_tensor(out=modT2[:, col:col + 1], in0=pb[:, 0:1], in1=fmc("bada", col), op=ALU.add),
                     reads=[pn, 'fm'], writes=['modT2'])
                if prefetch_next:
                    ada_prefetch(wad)
        S.op('dve', lambda e: e.scalar_tensor_tensor(out=A1T, in0=modT[:, 16:32], scalar=1.0, in1=fmc("g1"),
                                                     op0=ALU.add, op1=ALU.mult), reads=['modT', 'fm'], writes=['A1T'])
        sh1T = modT[:, 0:16]
        sh2T = modT[:, 48:64]

        def bcast_cols(colsT, dst, dname):
            for g4 in range(4):
                pb, pn = bank()
                for k4 in range(4):
                    kc = g4 * 4 + k4
                    S.op('dve', lambda e: e.tensor_scalar(out=diagf, in0=ident_f, scalar1=colsT[:, kc:kc + 1], scalar2=None,
                                                          op0=ALU.mult), reads=['cst', 'modT2'], writes=['diagf'])
                    S.op('pe', lambda e: e.matmul(out=pb[:, k4 * 128:(k4 + 1) * 128], lhsT=ones_f, rhs=diagf,
                                                  start=True, stop=True), reads=['ones_f', 'diagf'], writes=[pn])
                S.op('act', lambda e: e.activation(out=dst[:, g4 * 512:(g4 + 1) * 512], in_=pb[:, :], func=AF.Copy),
                     reads=[pn], writes=[dname])
        S.barrier()
        AX1.reset()
        if stage == "A":
            dump(modT, 96, 'modT'); dump(A1T, 16, 'A1T')
            S.finish()
            return nc

        def norm_T(src, src_res, dst3, dst_res, AT, ares, shT, junks, xss, shres='modT'):
            ni = nstate['i'] % 2
            nstate['i'] += 1
            ss, rs, rstd = nsc[ni]
            junk = junks[ni % len(junks)]
            xs = xss[ni % len(xss)]
            jn, xn_, sn, rn, rdn = f'junk{ni}', f'xs{ni}', f'ss{ni}', f'rs{ni}', f'rstd{ni}'
            S.op('act', lambda e: e.activation(out=junk, in_=src, func=AF.Square, accum_out=ss),
                 reads=[src_res], writes=[jn, sn])
            S.op('act', lambda e: e.activation(out=rs, in_=ss, func=AF.Sqrt, scale=1.0 / D, bias=EPS),
                 reads=[sn], writes=[rn])
            S.op('dve', lambda e: e.reciprocal(out=rstd, in_=rs), reads=[rn], writes=[rdn])
            S.op('act', lambda e: e.activation(out=xs, in_=src, func=AF.Identity, scale=rstd),
                 reads=[src_res, rdn], writes=[xn_])
            for half in range(2):
                pb, pn = bank()
                pv = pb[:, :].bitcast(BF16)
                for k in range(8):
                    kc = half * 8 + k
                    S.op('pe', lambda e: e.transpose(out=pv[:, k * 128:(k + 1) * 128], in_=xs[:, kc * 128:(kc + 1) * 128],
                                                     identity=idb), reads=[xn_, 'idb'], writes=[pn])
                for k in range(8):
                    kc = half * 8 + k
                    if k % 2 == 0:
                        S.op('dve', lambda e: e.tensor_scalar(out=dst3[:, kc, :], in0=pv[:, k * 128:(k + 1) * 128],
                                                              scalar1=AT[:, kc:kc + 1], scalar2=shT[:, kc:kc + 1],
                                                              op0=ALU.mult, op1=ALU.add),
                             reads=[pn, ares, shres], writes=[dst_res])
                    else:
                        S.op('act', lambda e: e.activation(out=dst3[:, kc, :], in_=pv[:, k * 128:(k + 1) * 128],
                                                           func=AF.Identity, scale=AT[:, kc:kc + 1], bias=shT[:, kc:kc + 1]),
                             reads=[pn, ares, shres], writes=[dst_res])

        wstate = {'i': 0}

        def load_w(ws, cc):
            s_ = wstate['i'] % len(ws)
            wstate['i'] += 1
            S.dma('pool', f'ws{s_}', out=ws[s_].rearrange("p k c -> p (k c)"), in_=w_in[cc], writes=[f'ws{s_}'])
            return ws[s_], f'ws{s_}'

        def projT(wslot, wres, hT, hres, t0, n, pb, pn, c0=0):
            for kc in range(KC):
                S.op('pe', lambda e: e.matmul(out=pb[:, c0:c0 + n], lhsT=wslot[:, kc, :], rhs=hT[:, kc, t0:t0 + n],
                                              start=(kc == 0), stop=(kc == KC - 1)), reads=[wres, hres], writes=[pn])

        pend = {'f': None}

        def flush_norm():
            if pend['f'] is not None:
                f_ = pend['f']
                pend['f'] = None
                f_()

        def qk_norm(pb, pn, n, gcol, dst, dres, tmp, is_q):
            sqs, rks = tmp
            i = nstate['q'] % 2
            nstate['q'] += 1
            sq, rk = sqs[i], rks[i]
            flush_norm()
            S.op('act', lambda e: e.activation(out=sq[:, 0:n], in_=pb[:, 0:n], func=AF.Square), reads=[pn], writes=[f'sq{i}'])

            def rest():
                pb2, pn2 = bank()
                S.op('pe', lambda e: e.matmul(out=pb2[:, 0:n], lhsT=ones_f, rhs=sq[:, 0:n], start=True, stop=True),
                     reads=['ones_f', f'sq{i}'], writes=[pn2])
                if is_q:
                    S.op('act', lambda e: e.activation(out=rk[:, 0:n], in_=pb2[:, 0:n], func=AF.Sqrt, scale=1.0, bias=128.0 * EPS),
                         reads=[pn2], writes=[f'rk{i}'])
                else:
                    S.op('act', lambda e: e.activation(out=rk[:, 0:n], in_=pb2[:, 0:n], func=AF.Sqrt, scale=1.0 / 128, bias=EPS),
                         reads=[pn2], writes=[f'rk{i}'])
                S.op('dve', lambda e: e.reciprocal(out=rk[:, 0:n], in_=rk[:, 0:n]), reads=[f'rk{i}'], writes=[f'rk{i}'])
                S.op('dve', lambda e: e.scalar_tensor_tensor(out=dst, in0=pb[:, 0:n], scalar=gcol, in1=rk[:, 0:n],
                                                             op0=ALU.mult, op1=ALU.mult), reads=[pn, f'rk{i}', 'fm'], writes=[dres])
            pend['f'] = rest

        hTm = AM.alloc([KC, 1536], BF16)
        K3T = AM.alloc([4, LT], BF16)
        V3T = AM.alloc([4, LT], BF16)
        ws = [AM.alloc([KC, 128], BF16) for _ in range(3)]
        am_mark = (AM.lo, AM.hi)

        xin = [AX1.alloc([D], F32) for _ in range(3)]
        junk = [AX1.alloc([D], BF16)]
        xsb = [AX1.alloc([D], BF16) for _ in range(2)]
        hTh = AX1.alloc([KC, 512], BF16)
        sq = [AX1.alloc([512], F32) for _ in range(2)]
        rk = [AX1.alloc([512], F32) for _ in range(2)]
        xi = 0
        for tile in range(24):
            s_ = xi % 3
            xi += 1
            S.dma('sp', f'xin{s_}', out=xin[s_], in_=xh[tile * 128:(tile + 1) * 128, :], writes=[f'xin{s_}'])
            if tile < 12:
                blk, tt = tile // 4, tile % 4
                norm_T(xin[s_], f'xin{s_}', hTh[:, :, tt * 128:(tt + 1) * 128], 'hTh', A1T, 'A1T', sh1T, junk, xsb)
                if tt == 3:
                    for hd in range(4):
                        for kind in range(2):
                            cc = (CH_K if kind == 0 else CH_V) + 8 + hd
                            wsl, wr = load_w(ws, cc)
                            pb, pn = bank()
                            projT(wsl, wr, hTh, 'hTh', 0, 512, pb, pn)
                            if kind == 0:
                                qk_norm(pb, pn, 512, fmc("kg", 8 + hd), K3T[:, hd, blk * 512:(blk + 1) * 512], 'K3T', (sq, rk), False)
                            else:
                                S.op('act', lambda e: e.activation(out=V3T[:, hd, blk * 512:(blk + 1) * 512], in_=pb[:, :], func=AF.Copy),
                                     reads=[pn], writes=['V3T'])
                    flush_norm()
            else:
                flush_norm()
                m0 = (tile - 12) * 128
                norm_T(xin[s_], f'xin{s_}', hTm[:, :, m0:m0 + 128], 'hTm', A1T, 'A1T', sh1T, junk, xsb)
        S.barrier()
        if stage == "H":
            dump(hTm[:, 0, 0:512], 512, 'hTm'); dump(hTm[:, 5, 512:1024], 512, 'hTm'); dump(K3T[:, 1, 0:512], 512, 'K3T')
            dump(V3T[:, 2, 512:1024], 512, 'V3T'); dump(hTh[:, 3, :], 512, 'hTh')
            S.finish()
            return nc
        AX1.reset()

        attnT = AX1.alloc([4, NT], BF16)
        x1_mark = AX1.lo
        Qs = [AX1.alloc([NT], BF16) for _ in range(3)]
        K2T = AX1.alloc([1536], BF16)
        V2T = AX1.alloc([1536], BF16)
        V2 = AX1.alloc([12, 128], BF16)
        K1T = AX1.alloc([1152], BF16)
        V1T = AX1.alloc([1152], BF16)
        V1 = AX1.alloc([9, 128], BF16)
        V3 = AX1.alloc([32, 128], BF16)
        sq = [AX1.alloc([512], F32) for _ in range(2)]
        rk = [AX1.alloc([512], F32) for _ in range(2)]
        PTs = [AX1.alloc([256], BF16) for _ in range(4)]
        oacc = AX1.alloc([NT], F32)
        dacc = AX1.alloc([NT], F32)
        pti = {'i': 0}
        wad = [AX1.alloc([16, 128], BF16) for _ in range(2)]

        def kv_proj(cc, hT, hres, spans, dstK, dres, gcol, is_k):
            wsl, wr = load_w(ws, cc)
            for (t0, n, d0) in spans:
                pb, pn = bank()
                projT(wsl, wr, hT, hres, t0, n, pb, pn)
                if is_k:
                    qk_norm(pb, pn, n, gcol, dstK[:, d0:d0 + n], dres, (sq, rk), False)
                else:
                    S.op('act', lambda e: e.activation(out=dstK[:, d0:d0 + n], in_=pb[:, 0:n], func=AF.Copy),
                         reads=[pn], writes=[dres])

        def transpose_tiles(srcs, dst, dres, sres):
            for i0 in range(0, len(srcs), 8):
                pb, pn = bank()
                pv = pb[:, :].bitcast(BF16)
                grp = srcs[i0:i0 + 8]
                for k, (sap, n) in enumerate(grp):
                    S.op('pe', lambda e: e.transpose(out=pv[0:n, k * 128:(k + 1) * 128], in_=sap, identity=idb),
                         reads=[sres, 'idb'], writes=[pn])
                nmin = min(n for _, n in grp)
                S.op('dve', lambda e: e.tensor_copy(out=dst[0:nmin, i0:i0 + len(grp), :],
                                                    in_=pv[0:nmin, 0:len(grp) * 128].rearrange("p (k e) -> p k e", k=len(grp))),
                     reads=[pn], writes=[dres])

        def attend(pairs, qap, qn, ob, on, db, dn, c0, first_unused=None):
            pts = []
            for (kap, nk, vap, mask, kb) in pairs:
                pb, pn = bank()
                S.op('pe', lambda e: e.matmul(out=pb[0:nk, 0:qn], lhsT=kap, rhs=qap, start=True, stop=False),
                     reads=['KQ', 'K3T'], writes=[pn])
                S.op('pe', lambda e: e.matmul(out=pb[0:nk, 0:qn], lhsT=idb[0:nk, 0:nk], rhs=mask, start=False, stop=True),
                     reads=['idb', 'mcur', 'mprev'], writes=[pn])
                i = pti['i'] % 4
                pti['i'] += 1
                pt = PTs[i]
                if kb is not None:
                    S.op('act', lambda e: e.activation(out=pt[0:nk, 0:qn], in_=pb[0:nk, 0:qn], func=AF.Exp, bias=kb),
                         reads=[pn, 'fm'], writes=[f'pt{i}'])
                else:
                    S.op('act', lambda e: e.activation(out=pt[0:nk, 0:qn], in_=pb[0:nk, 0:qn], func=AF.Exp),
                         reads=[pn], writes=[f'pt{i}'])
                pts.append((pt, i, nk, vap))
            for j, (pt, i, nk, vap) in enumerate(pts):
                S.op('pe', lambda e: e.matmul(out=ob[:, c0:c0 + qn], lhsT=vap, rhs=pt[0:nk, 0:qn],
                                              start=(j == 0), stop=(j == len(pts) - 1)), reads=[f'pt{i}', 'Vt'], writes=[on])
            for j, (pt, i, nk, vap) in enumerate(pts):
                S.op('pe', lambda e: e.matmul(out=db[:, c0:c0 + qn], lhsT=ones_b[0:nk, :], rhs=pt[0:nk, 0:qn],
                                              start=(j == 0), stop=(j == len(pts) - 1)), reads=[f'pt{i}', 'ones_b'], writes=[dn])

        for j in range(4):
            h1, h2, h3 = j, 4 + j, 8 + j
            ada_deferred(wad, 4)
            for gi, hd in enumerate((h1, h2, h3)):
                wsl, wr = load_w(ws, CH_Q + hd)
                for half in range(2):
                    pb, pn = bank()
                    projT(wsl, wr, hTm, 'hTm', 512 + half * 512, 512, pb, pn)
                    qk_norm(pb, pn, 512, fmc("qg", hd), Qs[gi][:, half * 512:(half + 1) * 512], 'KQ', (sq, rk), True)
                    ada_deferred(wad, 1)
            sp1 = [(384, 512, 0), (896, 512, 512), (1408, 128, 1024)]
            sp2 = [(0, 512, 0), (512, 512, 512), (1024, 512, 1024)]
            sp3 = [(0, 512, 1536), (512, 512, 2048), (1024, 512, 2560)]
            kv_proj(CH_K + h1, hTm, 'hTm', sp1, K1T, 'KQ', fmc("kg", h1), True)
            ada_deferred(wad, 1)
            kv_proj(CH_V + h1, hTm, 'hTm', sp1, V1T, 'VT', None, False)
            ada_deferred(wad, 1)
            kv_proj(CH_K + h2, hTm, 'hTm', sp2, K2T, 'KQ', fmc("kg", h2), True)
            ada_deferred(wad, 1)
            kv_proj(CH_V + h2, hTm, 'hTm', sp2, V2T, 'VT', None, False)
            ada_deferred(wad, 1)
            kv_proj(CH_K + h3, hTm, 'hTm', sp3, K3T[:, j, :], 'K3T', fmc("kg", h3), True)
            ada_deferred(wad, 1)
            kv_proj(CH_V + h3, hTm, 'hTm', sp3, V3T[:, j, :], 'V3T', None, False)
            ada_deferred(wad, 1, prefetch_next=False)
            flush_norm()
            transpose_tiles([(V1T[:, i * 128:(i + 1) * 128], 128) for i in range(9)], V1, 'Vt', 'VT')
            transpose_tiles([(V2T[:, 512 * kt + r:512 * kt + 512:4], 128) for r in range(4) for kt in range(3)], V2, 'Vt', 'VT')
            transpose_tiles([(V3T[:, j, r:2048:16], 128) for r in range(16)], V3[:, 0:16, :], 'Vt', 'V3T')
            transpose_tiles([(V3T[:, j, 2048 + r:LT:16], 64) for r in range(16)], V3[:, 16:32, :], 'Vt', 'V3T')
            bank_set['s'] = list(range(6))
            for hb in range(2):
                ob, on = pbs[6], 'pb6'
                db, dn = pbs[7], 'pb7'
                for q4 in range(4):
                    qb = hb * 4 + q4
                    pairs = [(K1T[:, 128 * (qb + 1):128 * (qb + 2)], 128, V1[:, qb + 1, :], mcur_b, fmc("kb1", qb + 1)),
                             (K1T[:, 128 * qb:128 * (qb + 1)], 128, V1[:, qb, :], mprev_b, fmc("kb1", qb))]
                    attend(pairs, Qs[0][:, 128 * qb:128 * (qb + 1)], 128, ob, on, db, dn, q4 * 128)
                S.op('act', lambda e: e.activation(out=oacc[:, hb * 512:(hb + 1) * 512], in_=ob[:, :], func=AF.Copy),
                     reads=[on], writes=['oacc'])
                S.op('dve', lambda e: e.tensor_copy(out=dacc[:, hb * 512:(hb + 1) * 512], in_=db[:, :]),
                     reads=[dn], writes=['dacc'])
            for qb in range(2):
                ob, on = pbs[6], 'pb6'
                db, dn = pbs[7], 'pb7'
                for r in range(4):
                    pairs = [(K2T[:, 512 * (qb + 1) + r:512 * (qb + 2):4], 128, V2[:, r * 3 + qb + 1, :], mcur_b, fmc("kb2", r * 3 + qb + 1)),
                             (K2T[:, 512 * qb + r:512 * (qb + 1):4], 128, V2[:, r * 3 + qb, :], mprev_b, fmc("kb2", r * 3 + qb))]
                    attend(pairs, Qs[1][:, 512 * qb + r:512 * (qb + 1):4], 128, ob, on, db, dn, r * 128)
                ov = oacc[:, 512 * qb:512 * (qb + 1)].rearrange("p (m r) -> p r m", r=4)
                dv = dacc[:, 512 * qb:512 * (qb + 1)].rearrange("p (m r) -> p r m", r=4)
                S.op('dve', lambda e: e.tensor_tensor(out=ov, in0=ob[:, :].rearrange("p (r m) -> p r m", r=4), in1=ov, op=ALU.add),
                     reads=[on, 'oacc'], writes=['oacc'])
                S.op('dve', lambda e: e.tensor_tensor(out=dv, in0=db[:, :].rearrange("p (r m) -> p r m", r=4), in1=dv, op=ALU.add),
                     reads=[dn, 'dacc'], writes=['dacc'])
            for hb in range(2):
                ob, on = pbs[6], 'pb6'
                db, dn = pbs[7], 'pb7'
                for r8 in range(8):
                    r = hb * 8 + r8
                    pairs = [(K3T[:, j, r:2048:16], 128, V3[:, r, :], mprev_b[:, 0:64], fmc("kb3", r)),
                             (K3T[:, j, 2048 + r:LT:16], 64, V3[0:64, 16 + r, :], mcur_b[0:64, 0:64], None)]
                    attend(pairs, Qs[2][:, r:NT:16], 64, ob, on, db, dn, r8 * 64)
                ov = oacc[:, :].rearrange("p (m r) -> p r m", r=16)[:, hb * 8:(hb + 1) * 8, :]
                dv = dacc[:, :].rearrange("p (m r) -> p r m", r=16)[:, hb * 8:(hb + 1) * 8, :]
                S.op('dve', lambda e: e.tensor_tensor(out=ov, in0=ob[:, :].rearrange("p (r m) -> p r m", r=8), in1=ov, op=ALU.add),
                     reads=[on, 'oacc'], writes=['oacc'])
                S.op('dve', lambda e: e.tensor_tensor(out=dv, in0=db[:, :].rearrange("p (r m) -> p r m", r=8), in1=dv, op=ALU.add),
                     reads=[dn, 'dacc'], writes=['dacc'])
            bank_set['s'] = list(range(8))
            S.op('dve', lambda e: e.reciprocal(out=dacc, in_=dacc), reads=['dacc'], writes=['dacc'])
            S.op('dve', lambda e: e.tensor_tensor(out=attnT[:, j, :], in0=oacc, in1=dacc, op=ALU.mult),
                 reads=['oacc', 'dacc'], writes=['attnT'])
            if stage == "M1":
                dump(Qs[0][:, 0:512], 512, 'KQ'); dump(K1T[:, 0:512], 512, 'KQ'); dump(V1[:, 1, :], 128, 'Vt')
                dump(attnT[:, 0, :], 1024, 'attnT'); dump(dacc, 1024, 'dacc'); dump(V3[:, 5, :], 128, 'Vt'); dump(V3[:, 21, :], 128, 'Vt')
                dump(V2[:, 4, :], 128, 'Vt')
                S.finish()
                return nc
        assert ada_state['col'] == 96 or stage == "M1"
        S.op('dve', lambda e: e.scalar_tensor_tensor(out=A2T, in0=modT2[:, 64:80], scalar=1.0, in1=fmc("g2"),
                                                     op0=ALU.add, op1=ALU.mult), reads=['modT2', 'fm'], writes=['A2T'])
        bcast_cols(modT2[:, 32:48], gate1B, 'gate1B')
        bcast_cols(modT2[:, 80:96], gate2B, 'gate2B')
        S.barrier()
        AX1.lo = x1_mark
        BM = CONST_W + X1_W

        FA = Arena(big, BM + 12288, BM + 24576)
        FB = Arena(big, BM + 27648, TOT)
        U = AX1.alloc([8, 1152], BF16)
        Vc = AX1.alloc([8, NT], F32)
        uact = FA.alloc([8, NT], BF16)
        sig = FA.alloc([512], F32)
        sqv = FA.alloc([NT], F32)
        mean = FA.alloc([NT], F32)
        rstdv = FA.alloc([NT], F32)
        t1 = FA.alloc([NT], F32)
        diags = [FB.alloc([128], BF16) for _ in range(4)]
        spU = [(384, 512, 0), (896, 512, 512), (1408, 128, 1024)]
        for cc in range(8):
            wa_, wra = load_w(ws, cc)
            wb_, wrb = load_w(ws, 8 + cc)
            for (t0, n, d0) in spU:
                pa, pna = bank()
                pg, png = bank()
                projT(wa_, wra, hTm, 'hTm', t0, n, pa, pna)
                projT(wb_, wrb, hTm, 'hTm', t0, n, pg, png)
                S.op('act', lambda e: e.activation(out=sig[:, 0:n], in_=pg[:, 0:n], func=AF.Sigmoid), reads=[png], writes=['sig'])
                S.op('dve', lambda e: e.tensor_tensor(out=U[:, cc, d0:d0 + n], in0=pa[:, 0:n], in1=sig[:, 0:n], op=ALU.mult),
                     reads=[pna, 'sig'], writes=['U'])
            S.op('dve', lambda e: e.tensor_scalar(out=U[:, cc, 0:128], in0=U[:, cc, 0:128], scalar1=fmc("hval", 0), scalar2=None,
                                                  op0=ALU.mult), reads=['U', 'fm'], writes=['U'])
        di = 0
        for cc in range(8):
            pbs2 = [bank(), bank()]
            for k in range(31):
                dg = diags[di % 4]
                dn_ = f'diag{di % 4}'
                di += 1
                S.op('dve', lambda e: e.tensor_scalar(out=dg, in0=ident_f, scalar1=fmc("dw", cc * 31 + k), scalar2=None, op0=ALU.mult),
                     reads=['cst', 'fm'], writes=[dn_])
                for half in range(2):
                    pb, pn = pbs2[half]
                    o0 = 128 - 30 + k + half * 512
                    S.op('pe', lambda e: e.matmul(out=pb[:, :], lhsT=dg, rhs=U[:, cc, o0:o0 + 512], start=(k == 0), stop=(k == 30)),
                         reads=[dn_, 'U'], writes=[pn])
            for half in range(2):
                pb, pn = pbs2[half]
                S.op('act', lambda e: e.activation(out=Vc[:, cc, half * 512:(half + 1) * 512], in_=pb[:, :], func=AF.Identity,
                                                   bias=fmc("db", cc)), reads=[pn, 'fm'], writes=['Vc'])
        for half in range(2):
            pm, pmn = bank()
            pq, pqn = bank()
            hs = slice(half * 512, (half + 1) * 512)
            for cc in range(8):
                S.op('pe', lambda e: e.matmul(out=pm[:, :], lhsT=ones_f, rhs=Vc[:, cc, hs], start=(cc == 0), stop=(cc == 7)),
                     reads=['ones_f', 'Vc'], writes=[pmn])
            for cc in range(8):
                S.op('act', lambda e: e.activation(out=sqv[:, 0:512], in_=Vc[:, cc, hs], func=AF.Square), reads=['Vc'], writes=['sqv'])
                S.op('pe', lambda e: e.matmul(out=pq[:, :], lhsT=ones_f, rhs=sqv[:, 0:512], start=(cc == 0), stop=(cc == 7)),
                     reads=['ones_f', 'sqv'], writes=[pqn])
            S.op('dve', lambda e: e.tensor_scalar(out=mean[:, hs], in0=pm[:, :], scalar1=1.0 / 1024, scalar2=None, op0=ALU.mult),
                 reads=[pmn], writes=['mean'])
            S.op('dve', lambda e: e.tensor_tensor(out=t1[:, hs], in0=mean[:, hs], in1=mean[:, hs], op=ALU.mult),
                 reads=['mean'], writes=['t1'])
            S.op('dve', lambda e: e.scalar_tensor_tensor(out=rstdv[:, hs], in0=pq[:, :], scalar=1.0 / 1024, in1=t1[:, hs],
                                                         op0=ALU.mult, op1=ALU.subtract), reads=[pqn, 't1'], writes=['rstdv'])
            S.op('act', lambda e: e.activation(out=rstdv[:, hs], in_=rstdv[:, hs], func=AF.Sqrt, scale=1.0, bias=EPS),
                 reads=['rstdv'], writes=['rstdv'])
            S.op('dve', lambda e: e.reciprocal(out=rstdv[:, hs], in_=rstdv[:, hs]), reads=['rstdv'], writes=['rstdv'])
        for cc in range(8):
            S.op('dve', lambda e: e.tensor_tensor(out=t1, in0=Vc[:, cc, :], in1=mean, op=ALU.subtract), reads=['Vc', 'mean'], writes=['t1'])
            S.op('dve', lambda e: e.tensor_tensor(out=t1, in0=t1, in1=rstdv, op=ALU.mult), reads=['t1', 'rstdv'], writes=['t1'])
            S.op('act', lambda e: e.activation(out=uact[:, cc, :], in_=t1, func=AF.Silu, scale=fmc("lng", cc), bias=fmc("lnb", cc)),
                 reads=['t1', 'fm'], writes=['uact'])
        S.barrier()
        AX1.lo = x1_mark

        mergedT = Arena(big, BM + 16384, BM + 24576).alloc([KC, NT], BF16)
        wco = [AX1.alloc([8, 128], BF16) for _ in range(2)]
        wao = [AX1.alloc([4, 128], BF16) for _ in range(2)]
        sA = [AX1.alloc([512], F32) for _ in range(2)]
        sB = [AX1.alloc([512], F32) for _ in range(2)]
        tA = [AX1.alloc([512], F32) for _ in range(2)]
        tB = [AX1.alloc([512], F32) for _ in range(2)]
        w_co_v = w_co.rearrange("(cc p) n -> p cc n", p=128)
        w_ao_v = w_ao.rearrange("(j p) n -> p j n", p=128)
        it = 0
        for dc in range(KC):
            s_ = dc % 2
            S.dma('pool', f'wco{s_}', out=wco[s_], in_=w_co_v[:, :, dc * 128:(dc + 1) * 128], writes=[f'wco{s_}'])
            S.dma('pool', f'wao{s_}', out=wao[s_], in_=w_ao_v[:, :, dc * 128:(dc + 1) * 128], writes=[f'wao{s_}'])
            wga, wgar = load_w(ws, CH_G + dc)
            wgb, wgbr = load_w(ws, CH_G + 16 + dc)
            for half in range(2):
                hs = slice(half * 512, (half + 1) * 512)
                b_ = it % 2
                it += 1
                pA, pAn = bank()
                pB, pBn = bank()
                pC, pCn = bank()
                pY, pYn = bank()
                projT(wga, wgar, hTm, 'hTm', 512 + half * 512, 512, pA, pAn)
                projT(wgb, wgbr, hTm, 'hTm', 512 + half * 512, 512, pB, pBn)
                for cc in range(8):
                    S.op('pe', lambda e: e.matmul(out=pC[:, :], lhsT=wco[s_][:, cc, :], rhs=uact[:, cc, hs], start=(cc == 0), stop=(cc == 7)),
                         reads=[f'wco{s_}', 'uact'], writes=[pCn])
                for jj in range(4):
                    S.op('pe', lambda e: e.matmul(out=pY[:, :], lhsT=wao[s_][:, jj, :], rhs=attnT[:, jj, hs], start=(jj == 0), stop=(jj == 3)),
                         reads=[f'wao{s_}', 'attnT'], writes=[pYn])
                S.op('act', lambda e: e.activation(out=sA[b_], in_=pA[:, :], func=AF.Sigmoid), reads=[pAn], writes=[f'sA{b_}'])
                S.op('act', lambda e: e.activation(out=sB[b_], in_=pB[:, :], func=AF.Sigmoid), reads=[pBn], writes=[f'sB{b_}'])
                S.op('dve', lambda e: e.scalar_tensor_tensor(out=tA[b_], in0=pC[:, :], scalar=fmc("bco", dc), in1=sA[b_],
                                                             op0=ALU.add, op1=ALU.mult), reads=[pCn, f'sA{b_}', 'fm'], writes=[f'tA{b_}'])
                S.op('dve', lambda e: e.tensor_tensor(out=tB[b_], in0=pY[:, :], in1=sB[b_], op=ALU.mult),
                     reads=[pYn, f'sB{b_}'], writes=[f'tB{b_}'])
                S.op('dve', lambda e: e.tensor_tensor(out=mergedT[:, dc, hs], in0=tA[b_], in1=tB[b_], op=ALU.add),
                     reads=[f'tA{b_}', f'tB{b_}'], writes=['mergedT'])
        S.barrier()
        AX1.reset()

        x1 = AX1.alloc([8, D], F32)
        AM4 = Arena(big, BM, BM + 16384)
        wos = [AM4.alloc([KC, 512], BF16) for _ in range(2)]
        xr = [AM4.alloc([512], F32) for _ in range(2)]
        tg = [AM4.alloc([512], F32) for _ in range(2)]
        w_out_v = w_out.rearrange("(kc p) n -> p kc n", p=128)
        it = 0
        for nb in range(4):
            ns = slice(nb * 512, (nb + 1) * 512)
            wo = wos[nb % 2]
            wn = f'wo{nb % 2}'
            S.dma('pool', wn, out=wo, in_=w_out_v[:, :, ns], writes=[wn])
            for tt in range(8):
                b_ = it % 2
                it += 1
                S.dma('sp', f'xr{b_}', out=xr[b_], in_=xh[2048 + tt * 128:2048 + (tt + 1) * 128, ns], writes=[f'xr{b_}'])
                pb, pn = bank()
                for kc in range(KC):
                    S.op('pe', lambda e: e.matmul(out=pb[:, :], lhsT=mergedT[:, kc, tt * 128:(tt + 1) * 128], rhs=wo[:, kc, :],
                                                  start=(kc == 0), stop=(kc == KC - 1)), reads=['mergedT', wn], writes=[pn])
                S.op('dve', lambda e: e.tensor_tensor(out=tg[b_], in0=pb[:, :], in1=gate1B[:, ns], op=ALU.mult),
                     reads=[pn, 'gate1B'], writes=[f'tg{b_}'])
                S.op('dve', lambda e: e.tensor_tensor(out=x1[:, tt, ns], in0=tg[b_], in1=xr[b_], op=ALU.add),
                     reads=[f'tg{b_}', f'xr{b_}'], writes=['x1'])
        S.barrier()

        if stage == "x1":
            for tt in range(8):
                S.dma('sp', 'out', out=y[tt * 128:(tt + 1) * 128, :], in_=x1[:, tt, :], reads=['x1'])
            S.finish()
            return nc

        BM = CONST_W + X1_W
        P = Arena(big, BM, TOT)
        h2T = P.alloc([KC, NT], BF16)
        p_mark = P.lo
        junk = [P.alloc([D], BF16)]
        xsb = [P.alloc([D], BF16) for _ in range(2)]
        for tt in range(8):
            norm_T(x1[:, tt, :], 'x1', h2T[:, :, tt * 128:(tt + 1) * 128], 'h2T', A2T, 'A2T', sh2T, junk, xsb, 'modT2')
        S.barrier()
        P.lo = p_mark
        e1T = P.alloc([NT], F32)
        e2T = P.alloc([NT], F32)
        gT = P.alloc([NT], F32)
        g_mark = P.lo
        wqs = [P.alloc([KC, 128], BF16) for _ in range(2)]
        qpT = P.alloc([16, 512], F32)
        s_sb = P.alloc([16, 128], F32)
        v1 = P.alloc([16, 16], F32)
        i1 = P.alloc([16, 16], U32)
        i1f = P.alloc([16, 16], F32)
        wks = [P.alloc([128], F32) for _ in range(4)]
        cand = P.alloc([8, 256], F32)
        wk2s = [P.alloc([256], F32) for _ in range(2)]
        top = P.alloc([8, 16], F32)
        ci = P.alloc([8, 16], U32)
        cu = P.alloc([8, 16], U32)
        af = P.alloc([8, 16], F32)
        bf_ = P.alloc([8, 16], F32)
        ex = P.alloc([8, 16], F32)
        zs = P.alloc([8], F32)
        gg = P.alloc([8, 16], F32)
        oh = cand
        e1f = P.alloc([8, 16], F32)
        e2f = P.alloc([8, 16], F32)
        w_q_v = w_q.rearrange("(kc p) n -> p kc n", p=128)
        iota16 = cst[:, 512:528]
        iota128 = cst[:, 384:512]
        wqi = 0
        for half in range(2):
            for cc in range(16):
                s_ = wqi % 2
                wqi += 1
                S.dma('pool', f'wq{s_}', out=wqs[s_], in_=w_q_v[:, :, cc * 128:(cc + 1) * 128], writes=[f'wq{s_}'])
                pb, pn = bank()
                projT(wqs[s_], f'wq{s_}', h2T, 'h2T', half * 512, 512, pb, pn)
                S.op('act', lambda e: e.activation(out=qpT[:, cc, :], in_=pb[:, :], func=AF.Copy), reads=[pn], writes=['qpT'])
            for t4 in range(4):
                tt = half * 4 + t4
                for b4 in range(4):
                    pb, pn = bank()
                    for k4 in range(4):
                        hp = b4 * 4 + k4
                        S.op('pe', lambda e: e.matmul(out=pb[:, k4 * 128:(k4 + 1) * 128], lhsT=qpT[:, hp, t4 * 128:(t4 + 1) * 128],
                                                      rhs=skT[:, hp, :], start=True, stop=True), reads=['qpT', 'skT'], writes=[pn])
                    S.op('act', lambda e: e.activation(out=s_sb[:, b4 * 4:(b4 + 1) * 4, :].rearrange("p a b -> p (a b)"), in_=pb[:, :], func=AF.Copy),
                         reads=[pn], writes=['s_sb'])
                NCH = 4
                for hp0 in range(0, 16, NCH):
                    hps = list(range(hp0, hp0 + NCH))
                    for hp in hps:
                        S.op('dve', lambda e: e.max(out=v1[:, hp, 0:8], in_=s_sb[:, hp, :]), reads=['s_sb'], writes=[f'v1a{hp}'])
                    for hp in hps:
                        S.op('dve', lambda e: e.max_index(out=i1[:, hp, 0:8], in_max=v1[:, hp, 0:8], in_values=s_sb[:, hp, :]),
                             reads=['s_sb', f'v1a{hp}'], writes=[f'i1a{hp}'])
                    for hp in hps:
                        S.op('dve', lambda e: e.match_replace(out=wks[hp % NCH], in_to_replace=v1[:, hp, 0:8], in_values=s_sb[:, hp, :], imm_value=-1e30),
                             reads=['s_sb', f'v1a{hp}'], writes=[f'wk{hp % NCH}'])
                    for hp in hps:
                        S.op('dve', lambda e: e.max(out=v1[:, hp, 8:16], in_=wks[hp % NCH]), reads=[f'wk{hp % NCH}'], writes=[f'v1b{hp}'])
                    for hp in hps:
                        S.op('dve', lambda e: e.max_index(out=i1[:, hp, 8:16], in_max=v1[:, hp, 8:16], in_values=wks[hp % NCH]),
                             reads=[f'wk{hp % NCH}', f'v1b{hp}'], writes=[f'i1b{hp}'])
                v1all = [f'v1a{hp}' for hp in range(16)] + [f'v1b{hp}' for hp in range(16)]
                i1all = [f'i1a{hp}' for hp in range(16)] + [f'i1b{hp}' for hp in range(16)]
                v1v = v1.rearrange("p (h q) a -> p h q a", q=2)
                i1fv = i1f.rearrange("p (h q) a -> p h q a", q=2)
                cand4 = cand.rearrange("p h (a b) -> p h a b", a=16)
                S.op('dve', lambda e: e.tensor_tensor(out=cand4, in0=bc(v1v[:, :, 0, :], 3, 16), in1=bc(v1v[:, :, 1, :], 2, 16), op=ALU.add),
                     reads=v1all, writes=['cand'])
                for h0 in range(0, 8, 2):
                    hs2 = (h0, h0 + 1)
                    for h in hs2:
                        S.op('dve', lambda e: e.max(out=top[:, h, 0:8], in_=cand[:, h, :]), reads=['cand'], writes=[f'topa{h}'])
                    for h in hs2:
                        S.op('dve', lambda e: e.max_index(out=ci[:, h, 0:8], in_max=top[:, h, 0:8], in_values=cand[:, h, :]),
                             reads=['cand', f'topa{h}'], writes=[f'cia{h}'])
                    for h in hs2:
                        S.op('dve', lambda e: e.match_replace(out=wk2s[h % 2], in_to_replace=top[:, h, 0:8], in_values=cand[:, h, :], imm_value=-1e30),
                             reads=['cand', f'topa{h}'], writes=[f'wk2{h % 2}'])
                    for h in hs2:
                        S.op('dve', lambda e: e.max(out=top[:, h, 8:16], in_=wk2s[h % 2]), reads=[f'wk2{h % 2}'], writes=[f'topb{h}'])
                    for h in hs2:
                        S.op('dve', lambda e: e.max_index(out=ci[:, h, 8:16], in_max=top[:, h, 8:16], in_values=wk2s[h % 2]),
                             reads=[f'wk2{h % 2}', f'topb{h}'], writes=[f'cib{h}'])
                topall = [f'topa{h}' for h in range(8)] + [f'topb{h}' for h in range(8)]
                ciall = [f'cia{h}' for h in range(8)] + [f'cib{h}' for h in range(8)]
                S.op('dve', lambda e: e.tensor_tensor(out=ex, in0=top, in1=bc(top[:, :, 0], 2, 16), op=ALU.subtract), reads=topall, writes=['ex'])
                S.op('act', lambda e: e.activation(out=ex, in_=ex, func=AF.Exp), reads=['ex'], writes=['ex'])
                S.op('dve', lambda e: e.tensor_reduce(out=zs, in_=ex, axis=AX.X, op=ALU.add), reads=['ex'], writes=['zs'])
                S.op('dve', lambda e: e.reciprocal(out=zs, in_=zs), reads=['zs'], writes=['zs'])
                S.op('dve', lambda e: e.tensor_tensor(out=gg, in0=ex, in1=bc(zs, 2, 16), op=ALU.mult), reads=['ex', 'zs'], writes=['gg'])
                S.op('dve', lambda e: e.tensor_copy(out=i1f, in_=i1), reads=i1all, writes=['i1f'])
                S.op('dve', lambda e: e.tensor_scalar(out=cu, in0=ci, scalar1=4, scalar2=None, op0=ALU.logical_shift_right), reads=ciall, writes=['cu'])
                S.op('dve', lambda e: e.tensor_copy(out=af, in_=cu), reads=['cu'], writes=['af'])
                S.op('dve', lambda e: e.tensor_scalar(out=cu, in0=ci, scalar1=15, scalar2=None, op0=ALU.bitwise_and), reads=ciall, writes=['cu'])
                S.op('dve', lambda e: e.tensor_copy(out=bf_, in_=cu), reads=['cu'], writes=['bf'])
                oh4 = oh.rearrange("p h (k a) -> p h k a", k=16)
                io4 = bc(bc(iota16, 1, 16), 1, 8)
                for (src, q_, dst, dn_) in ((af, 0, e1f, 'e1f'), (bf_, 1, e2f, 'e2f')):
                    S.op('dve', lambda e: e.tensor_tensor(out=oh4, in0=io4, in1=bc(src, 3, 16), op=ALU.is_equal), reads=['af', 'bf', 'cst'], writes=['cand'])
                    S.op('dve', lambda e: e.tensor_tensor(out=oh4, in0=oh4, in1=bc(i1fv[:, :, q_, :], 2, 16), op=ALU.mult), reads=['cand', 'i1f'], writes=['cand'])
                    S.op('dve', lambda e: e.tensor_reduce(out=dst, in_=oh4, axis=AX.X, op=ALU.add), reads=['cand'], writes=[dn_])
                pb, pn = bank()
                for k3, (src, sn) in enumerate(((e1f, 'e1f'), (e2f, 'e2f'), (gg, 'gg'))):
                    S.op('pe', lambda e: e.transpose(out=pb[:, k3 * 128:(k3 + 1) * 128], in_=src.rearrange("p h k -> p (h k)"), identity=ident_f),
                         reads=[sn, 'cst'], writes=[pn])
                for k3, (dst, dn_) in enumerate(((e1T, 'e1T'), (e2T, 'e2T'), (gT, 'gT'))):
                    S.op('act', lambda e: e.activation(out=dst[:, tt * 128:(tt + 1) * 128], in_=pb[:, k3 * 128:(k3 + 1) * 128], func=AF.Copy),
                         reads=[pn], writes=[dn_])
        S.barrier()
        if stage == "R":
            dump(e1T, 1024, 'e1T'); dump(e2T, 1024, 'e2T'); dump(gT, 1024, 'gT')
            S.finish()
            return nc
        P.lo = g_mark
        iob = P.alloc([128], BF16)
        e1b = P.alloc([NT], BF16)
        e2b = P.alloc([NT], BF16)
        gb16 = P.alloc([NT], BF16)
        S.op('dve', lambda e: e.tensor_copy(out=iob, in_=iota128), reads=['cst'], writes=['iob'])
        S.op('dve', lambda e: e.tensor_copy(out=e1b, in_=e1T), reads=['e1T'], writes=['e1b'])
        S.op('dve', lambda e: e.tensor_copy(out=e2b, in_=e2T), reads=['e2T'], writes=['e2b'])
        S.op('dve', lambda e: e.tensor_copy(out=gb16, in_=gT), reads=['gT'], writes=['gb16'])
        P1s = [P.alloc([16, 128], BF16) for _ in range(2)]
        P2s = [P.alloc([16, 128], BF16) for _ in range(2)]
        P2g = [P.alloc([16, 128], BF16) for _ in range(2)]
        Gs = P.alloc([128, 128], BF16)
        gscr_v = gscr.rearrange("j i t -> i j t")
        gi = 0
        ev_i = 0
        for tb in range(8):
            for g16 in range(8):
                t0 = tb * 128 + g16 * 16
                b_ = gi % 2
                gi += 1
                for tl in range(16):
                    S.op('dve', lambda e: e.tensor_scalar(out=P1s[b_][:, tl, :], in0=iob, scalar1=e1T[:, t0 + tl:t0 + tl + 1], scalar2=None,
                                                          op0=ALU.is_equal), reads=['iob', 'e1T'], writes=[f'P1{b_}'])
                    S.op('dve', lambda e: e.tensor_scalar(out=P2g[b_][:, tl, :], in0=iob, scalar1=e2T[:, t0 + tl:t0 + tl + 1],
                                                          scalar2=gT[:, t0 + tl:t0 + tl + 1], op0=ALU.is_equal, op1=ALU.mult),
                         reads=['iob', 'e2T', 'gT'], writes=[f'P2g{b_}'])
                for q4 in range(4):
                    pb, pn = bank()
                    for k4 in range(4):
                        tl = q4 * 4 + k4
                        S.op('pe', lambda e: e.matmul(out=pb[:, k4 * 128:(k4 + 1) * 128], lhsT=P1s[b_][:, tl, :], rhs=P2g[b_][:, tl, :],
                                                      start=True, stop=True), reads=[f'P1{b_}', f'P2g{b_}'], writes=[pn])
                    c0 = g16 * 16 + q4 * 4
                    src = pb[:, :].rearrange("p (t j) -> p j t", t=4)
                    S.op('act', lambda e: e.activation(out=Gs[:, :, c0:c0 + 4], in_=src, func=AF.Copy), reads=[pn], writes=['Gs'])
                    ev_i += 1
            for jq in range(4):
                S.dma('sp', 'gsw', out=gscr_v[:, jq * 32:(jq + 1) * 32, tb * 128:(tb + 1) * 128], in_=Gs[:, jq * 32:(jq + 1) * 32, :],
                      reads=['Gs'], writes=['gscr'])
        S.barrier()
        P.lo = p_mark
        JG = 4
        NG = 128 // JG
        Gt = [P.alloc([JG, NT], BF16) for _ in range(2)]
        NWU = 4
        wup = [P.alloc([KC, 128], BF16) for _ in range(NWU)]
        NWD = 7
        wdn = [P.alloc([D], BF16) for _ in range(NWD)]
        GaT = P.alloc([JG, NT], BF16)
        gel = [P.alloc([512], F32) for _ in range(2)]
        evb = [P.alloc([512], F32) for _ in range(4)]
        st = {'u': 0, 'd': 0}
        uslot = {}
        dslot = {}

        def load_G(g):
            S.dma('sp', f'Gt{g % 2}', out=Gt[g % 2], in_=gscr_v[:, g * JG:(g + 1) * JG, :], reads=['gscr'], writes=[f'Gt{g % 2}'])

        def load_up(j):
            us = st['u'] % NWU
            st['u'] += 1
            uslot[j] = us
            S.dma('pool', f'wup{us}', out=wup[us], in_=w_upP[j].rearrange("p (kc i) -> p kc i", kc=KC), writes=[f'wup{us}'])

        def load_dn(j):
            ds = st['d'] % NWD
            st['d'] += 1
            dslot[j] = ds
            S.dma('pool', f'wdn{ds}', out=wdn[ds].rearrange("p (a b) -> p a b", a=4), in_=w_dnP[j].rearrange("p (a b) -> p a b", a=4),
                  writes=[f'wdn{ds}'])

        load_G(0)
        for jj in range(JG):
            load_up(jj)
            load_dn(jj)
        gl = 0
        ei = 0
        for g in range(NG):
            b_ = g % 2
            j0 = g * JG
            for jj in range(JG):
                us = uslot[j0 + jj]
                for half in range(2):
                    hs = slice(half * 512, (half + 1) * 512)
                    pb, pn = bank()
                    projT(wup[us], f'wup{us}', h2T, 'h2T', half * 512, 512, pb, pn)
                    gb = gl % 2
                    gl += 1
                    S.op('act', lambda e: e.activation(out=gel[gb], in_=pb[:, :], func=AF.Gelu), reads=[pn], writes=[f'gel{gb}'])
                    S.op('dve', lambda e: e.tensor_tensor(out=GaT[:, jj, hs], in0=gel[gb], in1=Gt[b_][:, jj, hs], op=ALU.mult),
                         reads=[f'gel{gb}', f'Gt{b_}'], writes=['GaT'])
            if g + 1 < NG:
                load_G(g + 1)
                for jj in range(JG):
                    load_up(j0 + JG + jj)
                for jj in range(JG - 1):
                    load_dn(j0 + JG + jj)
            for tt in range(8):
                for dq in range(4):
                    ds_ = slice(dq * 512, (dq + 1) * 512)
                    pb, pn = bank()
                    for jj in range(JG):
                        dsl = dslot[j0 + jj]
                        S.op('pe', lambda e: e.matmul(out=pb[:, :], lhsT=GaT[:, jj, tt * 128:(tt + 1) * 128], rhs=wdn[dsl][:, ds_],
                                                      start=(jj == 0), stop=(jj == JG - 1)), reads=['GaT', f'wdn{dsl}'], writes=[pn])
                    eb = ei % 4
                    ei += 1
                    S.op('dve', lambda e: e.tensor_tensor(out=evb[eb], in0=pb[:, :], in1=gate2B[:, ds_], op=ALU.mult),
                         reads=[pn, 'gate2B'], writes=[f'ev{eb}'])
                    xres = f'x1_{tt}_{dq}'
                    if eb % 2 == 0:
                        S.op('pool', lambda e: e.tensor_tensor(out=x1[:, tt, ds_], in0=x1[:, tt, ds_], in1=evb[eb], op=ALU.add),
                             reads=[f'ev{eb}', xres, 'x1'], writes=[xres])
                    else:
                        S.op('dve', lambda e: e.tensor_tensor(out=x1[:, tt, ds_], in0=x1[:, tt, ds_], in1=evb[eb], op=ALU.add),
                             reads=[f'ev{eb}', xres, 'x1'], writes=[xres])
            if g + 1 < NG:
                load_dn(j0 + JG + JG - 1)
        for tt in range(8):
            S.dma('sp', 'out', out=y[tt * 128:(tt + 1) * 128, :], in_=x1[:, tt, :], reads=['x1'] + [f'x1_{tt}_{dq}' for dq in range(4)])
        S.finish()
    return nc


_CACHE = {}


def _host_consts():
    cst = np.zeros((128, NCS), np.float32)
    cst[:, 0:128] = np.eye(128, dtype=np.float32)
    mk = np.arange(128)[:, None]
    mq = np.arange(128)[None, :]
    cst[:, 128:256] = np.where(mq >= mk, 0.0, NEG)
    cst[:, 256:384] = np.where(mq <= mk, 0.0, NEG)
    cst[:, 384:512] = np.arange(128, dtype=np.float32)[None, :]
    cst[:, 512:528] = np.arange(16, dtype=np.float32)[None, :]
    return cst


def _fmT(v, n):
    return np.ascontiguousarray(np.asarray(v, np.float32).reshape(n, 128).T)


def _chunk_major(w):
    n = w.shape[1] // 128
    return np.ascontiguousarray(w.reshape(KC, 128, n, 128).transpose(2, 1, 0, 3).reshape(n, 128, KC * 128))


def kernel(**inputs):
    f = lambda k: np.asarray(inputs[k], np.float32)
    x, c = f("x"), f("c")
    if "nc" not in _CACHE:
        _CACHE["nc"] = build_program()
    nc = _CACHE["nc"]
    cst = _host_consts()
    sk = f("peer_sub_keys")[0]
    skT = np.ascontiguousarray(sk.reshape(16, 128, 128).transpose(2, 0, 1).reshape(128, 2048))
    w_up = f("peer_w_up")[0]
    w_dn = f("peer_w_down")[0]
    w_upP = np.ascontiguousarray(w_up.reshape(128, 128, KC, 128).transpose(1, 3, 2, 0).reshape(128, 128, KC * 128))
    w_dnP = np.ascontiguousarray(w_dn.reshape(128, 128, D).transpose(1, 0, 2))
    shared = {
        "cst": cst, "skT": skT, "w_ada": _chunk_major(f("w_ada")[0]), "w_in": _chunk_major(f("w_in")[0]), "w_co": f("w_conv_out")[0],
        "w_ao": f("w_attn_o")[0], "w_out": f("w_out")[0], "w_q": f("peer_w_q")[0], "w_upP": w_upP, "w_dnP": w_dnP,
    }
    fm_shared = np.zeros((128, NFM), np.float32)

    def put(a, name, arr):
        o0, o1 = FM[name]
        a[:, o0:o1] = arr
    put(fm_shared, "bada", _fmT(f("b_ada")[0], 96))
    put(fm_shared, "g1", _fmT(f("norm1_g")[0], 16))
    put(fm_shared, "g2", _fmT(f("norm2_g")[0], 16))
    dw = f("conv_dw")[0]
    put(fm_shared, "dw", np.ascontiguousarray(dw.reshape(31, 8, 128).transpose(2, 1, 0).reshape(128, 248)))
    put(fm_shared, "db", _fmT(f("conv_db")[0], 8))
    put(fm_shared, "lng", _fmT(f("conv_ln_g")[0], 8))
    put(fm_shared, "lnb", _fmT(f("conv_ln_b")[0], 8))
    put(fm_shared, "bco", _fmT(f("b_conv_out")[0], 16))
    put(fm_shared, "qg", np.ascontiguousarray(f("q_norm_g")[0].T))
    put(fm_shared, "kg", np.ascontiguousarray(f("k_norm_g")[0].T))
    in_maps = []
    for core in range(8):
        b, q = core // 4, core % 4
        lo = 1024 * q - 2048
        xhh = np.zeros((LT, D), np.float32)
        s0 = max(lo, 0)
        xhh[s0 - lo:] = x[b, s0:1024 * q + 1024]
        valid = (lo + np.arange(LT)) >= 0
        kbias = np.where(valid, 0.0, NEG).astype(np.float32)
        fmc_ = fm_shared.copy()
        put(fmc_, "cT", _fmT(c[b], 16))
        put(fmc_, "hval", np.full((128, 1), 1.0 if q > 0 else 0.0, np.float32))
        p = np.arange(128)
        put(fmc_, "kb1", np.stack([kbias[1920 + 128 * i + p] for i in range(9)], axis=1))
        put(fmc_, "kb2", np.stack([kbias[1536 + 4 * (128 * kt + p) + r] for r in range(4) for kt in range(3)], axis=1))
        put(fmc_, "kb3", np.stack([kbias[16 * p + r] for r in range(16)], axis=1))
        m = dict(shared)
        m["xh"] = xhh
        m["fm"] = fmc_
        in_maps.append(m)
    res = run_bass_kernel_spmd(nc, in_maps, core_ids=list(range(8)))
    out = np.zeros((2, 4096, D), np.float32)
    for core in range(8):
        b, q = core // 4, core % 4
        out[b, 1024 * q:1024 * q + 1024] = res.results[core]["y"]
    return out
```
